# Optimizing a Trainium2 kernel written in Bass

```python
import math
import jax, jax.numpy as jnp
from jax import lax
import numpy as np


D_MODEL = 1024
BATCH = 2
SEQ = 8192
DEPTH = 2
DEC_BATCH = 32
DEC_SEQ = 1
PAST_LEN = 16384
PAGE_SIZE = 128

N_BRANCH = 4
BRANCH_WIDTH = D_MODEL // 4
IN_WIDTH = 10 * BRANCH_WIDTH
CHUNK = 128
A_HEADS = 4
A_HEAD_DIM = BRANCH_WIDTH // A_HEADS
B_CONV = 3
C_HEADS = 4
C_HEAD_DIM = BRANCH_WIDTH // C_HEADS
MOBA_BLOCK = 256
MOBA_TOPK = 3
Q_BLOCK = 128
D_CONV = 31
D_FF = 2816
N_EXPERTS = 8
TOP_K = 2
D_EXPERT = 3584
N_DENSE = (DEPTH + 1) // 2
N_MOE = DEPTH // 2
PLE_DIM = 256
ALPHA = (2 * DEPTH) ** 0.25
BETA = (8 * DEPTH) ** -0.25
LN_EPS = 1e-5

kernel_name = 'hybrid_gated_branch_decoder_step'


def layer_norm(x, g, b):
    xf = x.astype(jnp.float32)
    mu = jnp.mean(xf, axis=-1, keepdims=True)
    var = jnp.mean(jnp.square(xf - mu), axis=-1, keepdims=True)
    y = (xf - mu) * lax.rsqrt(var + LN_EPS) * g.astype(jnp.float32) + b.astype(jnp.float32)
    return y.astype(x.dtype)


def causal_dwconv(xp, w):
    c = xp.shape[-1]
    return lax.conv_general_dilated(xp, w[:, None, :].astype(xp.dtype), window_strides=(1,), padding='VALID',
                                    dimension_numbers=('NWC', 'WIO', 'NWC'), feature_group_count=c)


def chunk_spatial_mix(vn, w_s, b_s):
    n, t, _ = vn.shape
    nc = -(-t // CHUNK)
    vp = jnp.pad(vn, ((0, 0), (0, nc * CHUNK - t), (0, 0))).reshape(n, nc, CHUNK, A_HEADS, A_HEAD_DIM)
    w = jnp.tril(w_s).astype(vn.dtype)
    s = jnp.einsum('hts,ncshd->ncthd', w, vp) + b_s.T[None, None, :, :, None]
    return s.reshape(n, nc * CHUNK, BRANCH_WIDTH)[:, :t]


def moba_attention(q, k, v, pos0):
    n, tq, h, dh = q.shape
    lk = k.shape[1]
    nb = -(-lk // MOBA_BLOCK)
    pad = nb * MOBA_BLOCK - lk
    kb = jnp.pad(k, ((0, 0), (0, pad), (0, 0), (0, 0))).reshape(n, nb, MOBA_BLOCK, h, dh).transpose(0, 3, 1, 2, 4)
    vb = jnp.pad(v, ((0, 0), (0, pad), (0, 0), (0, 0))).reshape(n, nb, MOBA_BLOCK, h, dh).transpose(0, 3, 1, 2, 4)
    kmean = jnp.mean(kb.astype(jnp.float32), axis=3)
    qb = min(Q_BLOCK, tq)
    nq = -(-tq // qb)
    qp = jnp.pad(q, ((0, 0), (0, nq * qb - tq), (0, 0), (0, 0)))
    q_chunks = qp.reshape(n, nq, qb, h, dh).transpose(1, 0, 3, 2, 4)
    pos = jnp.minimum(pos0 + jnp.arange(nq * qb, dtype=jnp.int32), pos0 + tq - 1).reshape(nq, qb)
    k_sel = min(MOBA_TOPK, nb)
    n_ix = jnp.arange(n)[:, None, None, None]
    h_ix = jnp.arange(h)[None, :, None, None]
    blk_ar = jnp.arange(nb, dtype=jnp.int32)
    key_ar = jnp.arange(MOBA_BLOCK, dtype=jnp.int32)
    scale = 1.0 / math.sqrt(dh)

    def attend(args):
        qc, pc = args
        own = pc // MOBA_BLOCK
        gate = jnp.einsum('nhqd,nhbd->nhqb', qc.astype(jnp.float32), kmean)
        gate = jnp.where(blk_ar[None, :] < own[:, None], gate, -jnp.inf)
        _, sel = lax.top_k(gate, k_sel)
        sel_ok = sel < own[:, None]
        blocks = jnp.concatenate([sel, jnp.broadcast_to(own[:, None], (n, h, qb, 1))], axis=-1)
        blk_ok = jnp.concatenate([sel_ok, jnp.ones((n, h, qb, 1), bool)], axis=-1)
        kg = kb[n_ix, h_ix, blocks]
        vg = vb[n_ix, h_ix, blocks]
        s = jnp.einsum('nhqd,nhqjkd->nhqjk', qc, kg).astype(jnp.float32) * scale
        key_pos = blocks[..., None] * MOBA_BLOCK + key_ar
        ok = blk_ok[..., None] & (key_pos <= pc[:, None, None])
        s = jnp.where(ok, s, -jnp.inf)
        pr = jax.nn.softmax(s.reshape(n, h, qb, -1), axis=-1).reshape(s.shape).astype(vg.dtype)
        return jnp.einsum('nhqjk,nhqjkd->nhqd', pr, vg)

    out = lax.map(attend, (q_chunks, pos))
    return out.transpose(1, 0, 3, 2, 4).reshape(n, nq * qb, h, dh)[:, :tq]


def token_mixers(h, hist_b, hist_d, k_past, v_past, pos0, w_in, w_gate, b_gate, a_ln_g, a_ln_b, a_w_s, a_b_s,
                 b_conv_w, d_conv_w, d_conv_b, d_ln_g, d_ln_b, w_branch, w_out):
    n, t, _ = h.shape
    a_u, a_v, b_b, b_c, b_x, c_q, c_k, c_v, d_a, d_g = jnp.split(h @ w_in, 10, axis=-1)
    a_vn = layer_norm(jax.nn.gelu(a_v), a_ln_g, a_ln_b)
    y_a = jax.nn.gelu(a_u) * chunk_spatial_mix(a_vn, a_w_s, a_b_s)
    cb = jnp.concatenate([hist_b.astype(h.dtype), b_c * b_x], axis=1)
    y_b = b_b * causal_dwconv(cb, b_conv_w)
    tail_b = cb[:, -(B_CONV - 1):]
    q = c_q.reshape(n, t, C_HEADS, C_HEAD_DIM)
    k = c_k.reshape(n, t, C_HEADS, C_HEAD_DIM)
    v = c_v.reshape(n, t, C_HEADS, C_HEAD_DIM)
    k_full = jnp.concatenate([k_past.astype(h.dtype), k], axis=1)
    v_full = jnp.concatenate([v_past.astype(h.dtype), v], axis=1)
    y_c = moba_attention(q, k_full, v_full, pos0).reshape(n, t, BRANCH_WIDTH)
    cd = jnp.concatenate([hist_d.astype(h.dtype), d_a * jax.nn.sigmoid(d_g)], axis=1)
    y_d = jax.nn.silu(layer_norm(causal_dwconv(cd, d_conv_w) + d_conv_b, d_ln_g, d_ln_b))
    tail_d = cd[:, -(D_CONV - 1):]
    branches = jnp.einsum('ntjc,jcd->ntjd', jnp.stack([y_a, y_b, y_c, y_d], axis=2), w_branch)
    gates = jax.nn.sigmoid(h @ w_gate + b_gate).reshape(n, t, N_BRANCH, D_MODEL)
    out = jnp.sum(gates * branches, axis=2) @ w_out
    return out, a_vn, tail_b, tail_d, k, v


def swiglu(x, w_gate, w_up, w_down):
    return (jax.nn.silu(x @ w_gate) * (x @ w_up)) @ w_down


def moe_swiglu(x, w_router, b_router, w_gate, w_up, w_down):
    logits = (x @ w_router).astype(jnp.float32) + b_router.astype(jnp.float32)
    top_val, top_idx = lax.top_k(logits, TOP_K)
    weights = jax.nn.softmax(top_val, axis=-1)
    combine = jnp.sum(jax.nn.one_hot(top_idx, N_EXPERTS, dtype=jnp.float32) * weights[..., None], axis=-2)
    out = jnp.zeros_like(x)
    for e in range(N_EXPERTS):
        out = out + combine[..., e:e + 1].astype(x.dtype) * swiglu(x, w_gate[e], w_up[e], w_down[e])
    return out


def run_group(x, p_emb, hist_b, hist_d, paged_kv, pos0, W):
    n = x.shape[0]
    states = []
    for i in range(DEPTH):
        if hist_b is None:
            hb = jnp.zeros((n, B_CONV - 1, BRANCH_WIDTH), x.dtype)
            hd = jnp.zeros((n, D_CONV - 1, BRANCH_WIDTH), x.dtype)
            kp = jnp.zeros((n, 0, C_HEADS, C_HEAD_DIM), x.dtype)
            vp = jnp.zeros((n, 0, C_HEADS, C_HEAD_DIM), x.dtype)
        else:
            hb = hist_b[i]
            hd = hist_d[i]
            cache_k, cache_v, page_table = paged_kv
            kp = cache_k[i][page_table].reshape(n, -1, C_HEADS, C_HEAD_DIM)
            vp = cache_v[i][page_table].reshape(n, -1, C_HEADS, C_HEAD_DIM)
        mix, va, tb, td, kn, vn = token_mixers(
            x, hb, hd, kp, vp, pos0, W['w_in'][i], W['w_gate'][i], W['b_gate'][i], W['a_ln_g'][i], W['a_ln_b'][i],
            W['a_w_s'][i], W['a_b_s'][i], W['b_conv_w'][i], W['d_conv_w'][i], W['d_conv_b'][i], W['d_ln_g'][i],
            W['d_ln_b'][i], W['w_branch'][i], W['w_out'][i])
        x = layer_norm(ALPHA * x + mix, W['ln_g'][i, 0], W['ln_b'][i, 0])
        if i % 2 == 0:
            j = i // 2
            f = swiglu(x, W['ffn_w_gate'][j], W['ffn_w_up'][j], W['ffn_w_down'][j])
        else:
            j = i // 2
            f = moe_swiglu(x, W['moe_w_router'][j], W['moe_b_router'][j], W['moe_w_gate'][j],
                           W['moe_w_up'][j], W['moe_w_down'][j])
        x = layer_norm(ALPHA * x + f, W['ln_g'][i, 1], W['ln_b'][i, 1])
        ple = jax.nn.sigmoid(x @ W['ple_w_gate'][i]) * (p_emb[i] @ W['ple_w_proj'][i])
        x = layer_norm(ALPHA * x + ple, W['ln_g'][i, 2], W['ln_b'][i, 2])
        states.append((va, tb, td, kn, vn))
    va, tb, td, kn, vn = [jnp.stack(z) for z in zip(*states)]
    return x, va, tb, td, kn, vn


def setup_inputs(seed: int = 0) -> dict:
    key = jax.random.key(seed)
    keys = iter(jax.random.split(key, 48))

    def normal(shape, scale):
        return jax.random.normal(next(keys), shape, jnp.float32) * scale

    n_pages = PAST_LEN // PAGE_SIZE
    n_used = DEC_BATCH * n_pages
    n_pool = n_used + max(1, n_used // 4)
    bw = BRANCH_WIDTH
    d = D_MODEL
    return {
        'x_prompt': normal((BATCH, SEQ, d), 1.0),
        'x_sample': normal((DEC_BATCH, DEC_SEQ, d), 1.0),
        'p_prompt': normal((DEPTH, BATCH, SEQ, PLE_DIM), 1.0),
        'p_sample': normal((DEPTH, DEC_BATCH, DEC_SEQ, PLE_DIM), 1.0),
        'cache_k': normal((DEPTH, n_pool, PAGE_SIZE, C_HEADS, C_HEAD_DIM), 1.0),
        'cache_v': normal((DEPTH, n_pool, PAGE_SIZE, C_HEADS, C_HEAD_DIM), 1.0),
        'state_conv_b': normal((DEPTH, DEC_BATCH, B_CONV - 1, bw), 1.0),
        'state_conv_d': normal((DEPTH, DEC_BATCH, D_CONV - 1, bw), 1.0),
        'page_table': jax.random.permutation(next(keys), n_pool)[:n_used].reshape(DEC_BATCH, n_pages).astype(jnp.int32),
        'w_in': normal((DEPTH, d, IN_WIDTH), d ** -0.5),
        'w_gate': normal((DEPTH, d, N_BRANCH * d), d ** -0.5),
        'b_gate': normal((DEPTH, N_BRANCH * d), 0.02),
        'a_ln_g': 1.0 + normal((DEPTH, bw), 0.02),
        'a_ln_b': normal((DEPTH, bw), 0.02),
        'a_w_s': normal((DEPTH, A_HEADS, CHUNK, CHUNK), CHUNK ** -0.5),
        'a_b_s': 1.0 + normal((DEPTH, A_HEADS, CHUNK), 0.1),
        'b_conv_w': normal((DEPTH, B_CONV, bw), B_CONV ** -0.5),
        'd_conv_w': normal((DEPTH, D_CONV, bw), D_CONV ** -0.5),
        'd_conv_b': normal((DEPTH, bw), 0.02),
        'd_ln_g': 1.0 + normal((DEPTH, bw), 0.02),
        'd_ln_b': normal((DEPTH, bw), 0.02),
        'w_branch': normal((DEPTH, N_BRANCH, bw, d), bw ** -0.5 * BETA),
        'w_out': normal((DEPTH, d, d), d ** -0.5 * BETA),
        'ln_g': 1.0 + normal((DEPTH, 3, d), 0.02),
        'ln_b': normal((DEPTH, 3, d), 0.02),
        'ffn_w_gate': normal((N_DENSE, d, D_FF), d ** -0.5),
        'ffn_w_up': normal((N_DENSE, d, D_FF), d ** -0.5),
        'ffn_w_down': normal((N_DENSE, D_FF, d), D_FF ** -0.5 * BETA),
        'moe_w_router': normal((N_MOE, d, N_EXPERTS), d ** -0.5),
        'moe_b_router': normal((N_MOE, N_EXPERTS), 0.01),
        'moe_w_gate': normal((N_MOE, N_EXPERTS, d, D_EXPERT), d ** -0.5),
        'moe_w_up': normal((N_MOE, N_EXPERTS, d, D_EXPERT), d ** -0.5),
        'moe_w_down': normal((N_MOE, N_EXPERTS, D_EXPERT, d), D_EXPERT ** -0.5 * BETA),
        'ple_w_gate': normal((DEPTH, d, d), d ** -0.5),
        'ple_w_proj': normal((DEPTH, PLE_DIM, d), PLE_DIM ** -0.5 * BETA),
    }


def reference(x_prompt, x_sample, p_prompt, p_sample, cache_k, cache_v, state_conv_b, state_conv_d, page_table,
              w_in, w_gate, b_gate, a_ln_g, a_ln_b, a_w_s, a_b_s, b_conv_w, d_conv_w, d_conv_b, d_ln_g, d_ln_b,
              w_branch, w_out, ln_g, ln_b, ffn_w_gate, ffn_w_up, ffn_w_down, moe_w_router, moe_b_router,
              moe_w_gate, moe_w_up, moe_w_down, ple_w_gate, ple_w_proj):
    W = dict(w_in=w_in, w_gate=w_gate, b_gate=b_gate, a_ln_g=a_ln_g, a_ln_b=a_ln_b, a_w_s=a_w_s, a_b_s=a_b_s,
             b_conv_w=b_conv_w, d_conv_w=d_conv_w, d_conv_b=d_conv_b, d_ln_g=d_ln_g, d_ln_b=d_ln_b,
             w_branch=w_branch, w_out=w_out, ln_g=ln_g, ln_b=ln_b, ffn_w_gate=ffn_w_gate, ffn_w_up=ffn_w_up,
             ffn_w_down=ffn_w_down, moe_w_router=moe_w_router, moe_b_router=moe_b_router, moe_w_gate=moe_w_gate,
             moe_w_up=moe_w_up, moe_w_down=moe_w_down, ple_w_gate=ple_w_gate, ple_w_proj=ple_w_proj)
    y_prompt, _, conv_b_prompt, conv_d_prompt, k_prompt, v_prompt = run_group(
        x_prompt, p_prompt, None, None, None, 0, W)
    past_len = page_table.shape[1] * cache_k.shape[2]
    y_sample, chunk_v_sample, conv_b_sample, conv_d_sample, k_sample, v_sample = run_group(
        x_sample, p_sample, state_conv_b, state_conv_d, (cache_k, cache_v, page_table), past_len, W)
    return (y_prompt, y_sample, k_prompt, v_prompt, k_sample, v_sample, conv_b_prompt, conv_b_sample,
            conv_d_prompt, conv_d_sample, chunk_v_sample)
```

```python
import numpy as np
import concourse.bass as bass
import concourse.mybir as mybir
from concourse.bass_utils import run_bass_kernel_spmd

F32 = mybir.dt.float32
BF16 = mybir.dt.bfloat16
I32 = mybir.dt.int32
AF = mybir.ActivationFunctionType
ALU = mybir.AluOpType
AX = mybir.AxisListType

NPR = 2048
NS = 4
T = NPR + NS
MT = [(i * 256, 256) for i in range(8)] + [(NPR, NS)]
FT = [(0, 512), (512, 512), (1024, 512), (1536, 512), (2048, 4)]
DEPTH = 2
ALPHA = (2 * DEPTH) ** 0.25
EPS = 1e-5
NEG = -30000.0
NPOOL = 5120
GROUPS = [[0, 1, 2, 3], [4, 5, 6, 7]]
NFFN_G = 11
NMOE_G = 14
PCH = 16
import os
DEBUG = bool(os.environ.get('KDBG'))
NCHK = 128 // PCH

SP_BGATE = 0
SP_LNG = 32
SP_LNB = 56
SP_ALNG = 80
SP_ALNB = 82
SP_DLNG = 84
SP_DLNB = 86
SP_DCB = 88
SP_BCW = 90
SP_DCW = 96
SP_AW00 = 158
SP_ABS0 = 162
NSP = 166
C_ID = 0
C_ONE = 128
C_TRI = 256
C_PAIR = 384
C_PAIRT = 448
C_SEL8 = 576
C_PB = 1600
C_PV = 1920
C_PO = 2240
C_SELR = 2560
NCST = 2564
CB_ID = 0
CB_CAUS = 128
NCSTB = 640


class Buf:
    __slots__ = ("w", "r", "name")

    def __init__(self, name=""):
        self.w = None
        self.r = {}
        self.name = name


class Op:
    __slots__ = ("eng", "fn", "deps", "kind", "sem", "val", "signal", "idx")


class Prog:
    def __init__(self):
        self.ops = []
        self.bufs = {}
        self.always = []

    def B(self, *key):
        b = self.bufs.get(key)
        if b is None:
            b = self.bufs[key] = Buf(str(key))
        return b

    def add(self, eng, fn, reads=(), writes=(), kind="c"):
        op = Op()
        op.eng, op.fn, op.kind = eng, fn, kind
        op.idx = len(self.ops)
        op.signal = False
        op.sem = None
        op.val = 0
        deps = {}
        reads = list(reads) + [b for b in self.always if b not in writes]
        for b in reads:
            if b.w is not None:
                deps[b.w.idx] = b.w
        for b in writes:
            for o in b.r.values():
                deps[o.idx] = o
            if b.w is not None:
                deps[b.w.idx] = b.w
        rkey = eng if kind == "c" else ("d", op.idx)
        for b in reads:
            b.r[rkey] = op
        for b in writes:
            b.w = op
            b.r = {}
        deps.pop(op.idx, None)
        dl = []
        for o in deps.values():
            if o.kind == "c" and kind == "c" and o.eng == "pe" and eng == "pe":
                continue
            dl.append(o)
        op.deps = dl
        self.ops.append(op)
        return op


def emit(nc, P):
    ops = P.ops
    for op in ops:
        for d in op.deps:
            d.signal = True
    NRING = 12
    csem = {e: nc.alloc_semaphore(name=f"c_{e}") for e in ("pe", "act", "dve", "pool")}
    rings = {q: [nc.alloc_semaphore(name=f"d_{q}{i}") for i in range(NRING)] for q in ("sp", "pool")}
    ccsem = nc.alloc_semaphore(name="ccs")
    ccount = {e: 0 for e in csem}
    rcount = {}
    rlast = {}
    rpos = {"sp": 0, "pool": 0}
    cccount = 0
    finals = {}
    for op in ops:
        if op.kind == "c":
            if op.signal:
                ccount[op.eng] += 1
                op.sem = csem[op.eng]
                op.val = ccount[op.eng]
        elif op.kind == "d":
            s = rings[op.eng][rpos[op.eng] % NRING]
            rpos[op.eng] += 1
            key = id(s)
            prev = rlast.get(key)
            if prev is not None:
                op.deps.append(prev)
            rcount[key] = rcount.get(key, 0) + 1
            rlast[key] = op
            op.sem = s
            op.val = 16 * rcount[key]
            finals[key] = (s, op.val)
        else:
            cccount += 1
            op.sem = ccsem
            op.val = cccount
            finals[id(ccsem)] = (ccsem, cccount)
    assert max(ccount.values()) < 60000, ccount
    queues = {"pe": [], "act": [], "dve": [], "pool": [], "sp": []}
    for op in ops:
        queues[op.eng].append(op)

    def run(eng_name, e):
        waited = {}
        for op in queues[eng_name]:
            need = {}
            for d in op.deps:
                k = id(d.sem)
                if d.val > need.get(k, (None, 0))[1]:
                    need[k] = (d.sem, d.val)
            for k, (s, v) in need.items():
                if waited.get(k, 0) >= v:
                    continue
                e.wait_ge(s, v)
                waited[k] = v
            ins = op.fn(e)
            if op.kind == "c":
                if op.signal:
                    ins.then_inc(op.sem, 1)
            elif op.kind == "d":
                ins.then_inc(op.sem, 16)
            else:
                ins.then_inc(op.sem, 1)
        if eng_name == "sp":
            for k, (s, v) in finals.items():
                e.wait_ge(s, v)

    with nc.Block() as block:
        @block.tensor
        def _(e):
            run("pe", e)

        @block.scalar
        def _(e):
            run("act", e)

        @block.vector
        def _(e):
            run("dve", e)

        @block.gpsimd
        def _(e):
            run("pool", e)

        @block.sync
        def _(e):
            run("sp", e)


def build_program():
    nc = bass.Bass("TRN2", target_bir_lowering=False)
    P = Prog()
    B = P.B

    def din(name, shape, dt=F32):
        return nc.dram_tensor(name, list(shape), dt, kind="ExternalInput").ap()

    def dout(name, shape, dt=F32):
        return nc.dram_tensor(name, list(shape), dt, kind="ExternalOutput").ap()

    def dint(name, shape, dt):
        return nc.dram_tensor(name, list(shape), dt, kind="Internal").ap()

    x_in = din("x_in", [128, 8, T])
    p_in = din("p_in", [2, 128, 2, T])
    scb_in = din("scb_in", [2, 128, 2, NS, 2])
    scd_in = din("scd_in", [2, 128, 2, NS, 30])
    pt_in = din("pt_in", [128, NS], I32)
    ck_in = din("ck_in", [2 * NPOOL * NCHK, PCH * 256])
    cv_in = din("cv_in", [2 * NPOOL * NCHK, PCH * 256])
    win = din("win", [2, 128, 8, 2560])
    wgate = din("wgate", [2, 128, 8, 4096])
    wbrA = din("wbrA", [2, 64, 4, 1024])
    wbrB = din("wbrB", [2, 128, 2, 1024])
    wbrC = din("wbrC", [2, 64, 4, 1024])
    wbrD = din("wbrD", [2, 128, 2, 1024])
    wout = din("wout", [2, 128, 8, 1024])
    awsT = din("awsT", [2, 128, 4, 128])
    absr = din("absr", [2, 1, 4, 128])
    sp_in = din("sp_in", [2, 128, NSP])
    ffnw = din("ffnw", [NFFN_G, 128, 6144])
    moew = din("moew", [8 * NMOE_G, 128, 6144])
    wr_in = din("wr_in", [128, 8, 8])
    br_in = din("br_in", [1, 8])
    pleg = din("pleg", [2, 128, 8, 1024])
    plep = din("plep", [2, 128, 2, 1024])
    cst_in = din("cst_in", [128, NCST])
    cstb_in = din("cstb_in", [128, NCSTB])
    koh_in = din("koh_in", [40, 10240])

    o_y = dout("o_y", [128, 8, T])
    o_k = dout("o_k", [2, 64, 4, T])
    o_v = dout("o_v", [2, 128, 2, T])
    o_cb = dout("o_cb", [2, 128, 2, 2])
    o_cd = dout("o_cd", [2, 128, 2, 30])
    o_cbs = dout("o_cbs", [2, 128, 2, NS, 2])
    o_cds = dout("o_cds", [2, 128, 2, NS, 30])
    o_cv = dout("o_cv", [2, 128, 2, NS])
    if DEBUG:
        d_ya = dout('d_ya', [64, 4, T], BF16)
        d_yb = dout('d_yb', [128, 2, T], BF16)
        d_yc = dout('d_yc', [64, 4, T], BF16)
        d_yd = dout('d_yd', [128, 2, T], BF16)
        d_mg = dout('d_mg', [128, 8, T], BF16)
        d_ln1 = dout('d_ln1', [128, 8, T])
        d_ln2 = dout('d_ln2', [128, 8, T])
        d_ln3 = dout('d_ln3', [128, 8, T])

    cc_kt = [dint(f"cc_kt{l}", [256, NPR], BF16) for l in range(2)]
    cc_ktg = [dint(f"cc_ktg{l}", [4 * 256, NPR], BF16) for l in range(2)]
    cc_v = [dint(f"cc_v{l}", [128, 4096], BF16) for l in range(2)]
    cc_vg = [dint(f"cc_vg{l}", [4 * 128, 4096], BF16) for l in range(2)]
    cc_t = [dint(f"cc_t{l}", [128, 64], F32) for l in range(2)]
    cc_tg = [dint(f"cc_tg{l}", [4 * 128, 64], F32) for l in range(2)]
    xs1 = dint("xs1", [128, 8, T], F32)
    xs2 = dint("xs2", [128, 8, T], F32)

    ARENA_B = 206000
    arena = nc.alloc_sbuf_tensor("arena", [128, ARENA_B // 2], BF16)
    cur = [0]

    def carve(nbytes):
        off = (cur[0] + 63) // 64 * 64
        cur[0] = off + nbytes
        assert cur[0] <= ARENA_B, f"SBUF arena overflow {cur[0]}"
        return off

    def tile(shape, dt):
        esz = 4 if dt in (F32, I32) else 2
        n = 1
        for s in shape[1:]:
            n *= s
        off = carve(n * esz)
        v = arena[0:shape[0], off // 2: off // 2 + n * esz // 2]
        if dt != BF16:
            v = v.bitcast(dt)
        if len(shape) == 3:
            v = v.rearrange("p (a b) -> p a b", a=shape[1])
        elif len(shape) == 4:
            v = v.rearrange("p (a b c) -> p a b c", a=shape[1], b=shape[2])
        return v

    xb = tile([128, 8, T], BF16)
    WSLOT = 8192
    NW = 2
    wslots = [tile([128, WSLOT], BF16) for _ in range(NW)]
    cst = tile([128, NCST], F32)
    cstb = tile([128, NCSTB], BF16)
    spt = [tile([128, NSP], F32) for _ in range(2)]
    awsb = tile([128, 4, 128], BF16)
    awsf = tile([128, 4, 128], F32)
    absf = tile([1, 4, 128], F32)
    wrt = tile([128, 8, 8], F32)
    brt = tile([1, 8], F32)
    cbprev = tile([128, 2, 2], F32)
    cdprev = tile([128, 2, 30], F32)
    tails = tile([128, 64], F32)
    tailg = tile([128, 4, 64], F32)
    pti = tile([128, NS], I32)
    idx = tile([128, 2], I32)
    QTs = tile([64, 4, NS], F32)
    KTs = tile([64, 4, NS], F32)
    VTs = tile([64, 4, NS], F32)
    kmT = tile([64, 4, 40], BF16)
    ycs = tile([64, 4, NS], BF16)
    m8s = tile([4, 8], F32)
    fsc = tile([128, 4], F32)
    phase_base = cur[0]

    xt = tile([128, 8, 256], F32)
    ya = tile([64, 4, 256], BF16)
    yb = tile([128, 2, 256], BF16)
    yc = tile([64, 4, 256], BF16)
    yd = tile([128, 2, 256], BF16)
    mg32 = tile([128, 8, 256], F32)
    mgb = tile([128, 8, 256], BF16)
    QTa = tile([104, 4, 256], BF16)
    Kh = tile([104, 10240], BF16)
    Vh = tile([128, 80, 65], BF16)
    vtm = tile([128, 4, 16, 64], BF16)
    cbT = tile([128, 2, 2 + 256], F32)
    cdT = tile([128, 2, 30 + 256], F32)
    NTMP = 4
    mtmp = [tile([128, 256], F32) for _ in range(NTMP)]
    mlnm = tile([128, 256], F32)
    mlnr = tile([128, 256], F32)
    dacc = [tile([128, 256], F32) for _ in range(2)]
    gv = tile([128, 2, 256], F32)
    vn = tile([128, 2, 256], F32)
    vtA = tile([128, 256], BF16)
    ptile = [tile([128, 256], BF16) for _ in range(2)]
    accS = tile([65, 256], F32)
    small = tile([128, 4, 40], F32)
    small2 = tile([128, 4, 104], F32)
    m8 = tile([128, 8], F32)
    kTb = tile([64, 4, 256], BF16)
    cbs = tile([128, 2, NS, 3], F32)
    cds = tile([128, 2, NS, 31], F32)
    sm3 = tile([128, 2, NS, 31], F32)
    Kc = tile([128, PCH * 256], F32)
    Ssc = tile([128, 128, 4], F32)
    Psc = tile([128, 128, 4], F32)
    qbc = tile([128, 256], F32)
    pvp = tile([128, 256], F32)
    pvc = tile([128, 256], F32)
    sdg = tile([64, 64], F32)
    ssm = tile([128, 16], F32)
    mix_end = cur[0]

    cur[0] = phase_base
    xf = tile([128, 8, T], F32)
    cmbT = tile([8, T], F32)
    cmbbc = [tile([128, T], F32) for _ in range(1)]
    hb = [tile([128, 512], BF16) for _ in range(4)]
    sgt = [tile([128, 512], F32) for _ in range(3)]
    ftmp = [tile([128, 512], F32) for _ in range(NTMP)]
    flnm = tile([128, 512], F32)
    flnr = tile([128, 512], F32)
    pTb = tile([128, 2, T], BF16)
    wppt = tile([128, 2048], BF16)
    lg = tile([128, 8], F32)
    ex8 = tile([128, 8], F32)
    fm8 = tile([128, 8], F32)
    ffn_end = cur[0]
    cur[0] = max(mix_end, ffn_end)
    REG = B("region")
    P.always = [REG]

    psum = [nc.alloc_psum_tensor(f"ps{i}", [128, 512], F32) for i in range(8)]
    psb = [B("ps", i) for i in range(8)]
    psrr = [0]

    def nextps():
        i = psrr[0] % 6
        psrr[0] += 1
        return psum[i], psb[i]

    def MM(out, lhsT, rhs, start, stop, R, W):
        P.add("pe", lambda e, o=out, a=lhsT, b=rhs, s=start, t=stop: e.matmul(o, a, b, start=s, stop=t), R, W)

    def TR(out, in_, ident, R, W):
        P.add("pe", lambda e, o=out, a=in_, i=ident: e.transpose(o, a, i), R, W)

    def ACT(out, in_, func, R, W, bias=0.0, scale=1.0):
        P.add("act", lambda e, o=out, a=in_, f=func, b=bias, s=scale: e.activation(o, a, f, bias=b, scale=s), R, W)

    def TT(out, a, b, op, R, W, eng="dve"):
        P.add(eng, lambda e, o=out, x=a, y=b, p=op: e.tensor_tensor(o, x, y, p), R, W)

    def TS(out, a, s1, s2, op0, op1, R, W, eng="dve"):
        if s2 is None:
            P.add(eng, lambda e, o=out, x=a, q=s1, p=op0: e.tensor_scalar(o, x, q, None, p), R, W)
        else:
            P.add(eng, lambda e, o=out, x=a, q=s1, r=s2, p=op0, p1=op1: e.tensor_scalar(o, x, q, r, p, p1), R, W)

    def STT(out, a, sc, b, op0, op1, R, W, eng="dve"):
        P.add(eng, lambda e, o=out, x=a, s=sc, y=b, p=op0, q=op1: e.scalar_tensor_tensor(o, x, s, y, p, q), R, W)

    def CP(out, in_, R, W, eng="dve"):
        if eng == "act":
            P.add("act", lambda e, o=out, a=in_: e.activation(o, a, AF.Copy), R, W)
        else:
            P.add(eng, lambda e, o=out, a=in_: e.tensor_copy(o, a), R, W)

    def RED(out, in_, op, R, W, eng="dve"):
        P.add(eng, lambda e, o=out, a=in_, p=op: e.tensor_reduce(o, a, AX.X, p), R, W)

    def MAX8(out, in_, R, W):
        P.add("dve", lambda e, o=out, a=in_: e.max(o, a), R, W)

    def RCP(out, in_, R, W):
        P.add("dve", lambda e, o=out, a=in_: e.reciprocal(o, a), R, W)

    def DMA(q, out, in_, R, W):
        P.add(q, lambda e, o=out, a=in_: e.dma_start(out=o, in_=a), R, W, kind="d")

    def FENCE():
        P.add("dve", lambda e: e.tensor_copy(fsc[0:1, 3:4], fsc[0:1, 3:4]), [], [REG])

    def w3(ap2d, a):
        return ap2d.rearrange("p (a b) -> p a b", a=a)

    wrr = [0]
    wsb = [B("wslot", i) for i in range(NW)]

    def loadw(src, parts, n, a=None):
        i = wrr[0] % NW
        wrr[0] += 1
        dst = wslots[i][0:parts, 0:n]
        DMA("pool", w3(dst, a) if a else dst, src, [], [wsb[i]])
        return dst, wsb[i]

    cs = lambda off, n, p=128: cst[0:p, off:off + n]
    ident_f = cs(C_ID, 128)
    ones_f = cs(C_ONE, 128)
    CSTB = B("cst")
    ident_b = cstb[:, CB_ID:CB_ID + 128]

    class TmpPool:
        def __init__(self, tiles, lnm, lnr, name):
            self.tiles = tiles
            self.bufs = [B(name, i) for i in range(len(tiles))]
            self.lnm, self.lnr = lnm, lnr
            self.lnmb, self.lnrb = B(name, "lnm"), B(name, "lnr")
            self.i = 0

        def next(self):
            i = self.i % len(self.tiles)
            self.i += 1
            return self.tiles[i], self.bufs[i]

    MTP = TmpPool(mtmp, mlnm, mlnr, "mtmp")
    FTP = TmpPool(ftmp, flnm, flnr, "ftmp")

    XB = [B("xb", i) for i in range(9)]
    XF = [B("xf", i) for i in range(9)]

    def tok_bufs(lst, c0, n):
        if c0 >= NPR:
            return [lst[8]]
        return [lst[i] for i in range(c0 // 256, (c0 + n - 1) // 256 + 1)]

    DMA("sp", cst[:, :], cst_in, [], [CSTB])
    DMA("pool", cstb[:, :], cstb_in, [], [CSTB])
    for l in range(2):
        DMA("sp", spt[l][:, :], sp_in[l], [], [CSTB])
    DMA("sp", wrt[:, :, :], wr_in, [], [CSTB])
    DMA("sp", brt[:, :], br_in, [], [CSTB])
    DMA("sp", pti[:, :], pt_in, [], [CSTB])
    for ti, (c0, n) in enumerate(MT):
        DMA("pool", xb[:, :, c0:c0 + n], x_in[:, :, c0:c0 + n], [], [XB[ti]])
    KH = B("Kh")
    VH = B("Vh")

    def layernorm(tp, srcs, n, nfeat, gcols, bcols, outs32, outsb, silu=False):
        nch = len(srcs)
        ps1, pb1 = nextps()
        ps2, pb2 = nextps()
        for i, (s, sb) in enumerate(srcs):
            MM(ps1[:, 0:n], ones_f, s, i == 0, i == nch - 1, sb + [CSTB], [pb1])
        for i, (s, sb) in enumerate(srcs):
            t, tb = tp.next()
            ACT(t[:, 0:n], s, AF.Square, sb, [tb])
            MM(ps2[:, 0:n], ones_f, t[:, 0:n], i == 0, i == nch - 1, [tb, CSTB], [pb2])
        mean, mb, rstd, rb = tp.lnm, tp.lnmb, tp.lnr, tp.lnrb
        ACT(mean[:, 0:n], ps1[:, 0:n], AF.Copy, [pb1], [mb], scale=1.0 / nfeat)
        t, tb = tp.next()
        TT(t[:, 0:n], mean[:, 0:n], mean[:, 0:n], ALU.mult, [mb], [tb])
        STT(rstd[:, 0:n], ps2[:, 0:n], 1.0 / nfeat, t[:, 0:n], ALU.mult, ALU.subtract, [pb2, tb], [rb])
        TS(rstd[:, 0:n], rstd[:, 0:n], EPS, None, ALU.add, None, [rb], [rb])
        RCP(rstd[:, 0:n], rstd[:, 0:n], [rb], [rb])
        ACT(rstd[:, 0:n], rstd[:, 0:n], AF.Sqrt, [rb], [rb])
        for i, (s, sb) in enumerate(srcs):
            t, tb = tp.next()
            TT(t[:, 0:n], s, mean[:, 0:n], ALU.subtract, sb + [mb], [tb])
            TT(t[:, 0:n], t[:, 0:n], rstd[:, 0:n], ALU.mult, [tb, rb], [tb])
            if silu:
                ACT(t[:, 0:n], t[:, 0:n], AF.Identity, [tb, CSTB], [tb], bias=bcols[i], scale=gcols[i])
                t2, tb2 = tp.next()
                ACT(t2[:, 0:n], t[:, 0:n], AF.Sigmoid, [tb], [tb2])
                ob, obb = outsb[i]
                TT(ob, t[:, 0:n], t2[:, 0:n], ALU.mult, [tb, tb2], obb)
            else:
                o32, ob32 = outs32[i]
                ACT(o32, t[:, 0:n], AF.Identity, [tb, CSTB], ob32, bias=bcols[i], scale=gcols[i])
                if outsb is not None:
                    ob, obb = outsb[i]
                    CP(ob, o32, ob32, obb)

    def gelu_from(ps_ap, pbuf, out_ap, obufs, n, parts=128):
        x, xbf = MTP.next()
        t, tb = MTP.next()
        ACT(x[0:parts, 0:n], ps_ap, AF.Copy, [pbuf], [xbf])
        TT(t[0:parts, 0:n], x[0:parts, 0:n], x[0:parts, 0:n], ALU.mult, [xbf], [tb])
        TS(t[0:parts, 0:n], t[0:parts, 0:n], 0.044715, 1.0, ALU.mult, ALU.add, [tb], [tb])
        TT(t[0:parts, 0:n], t[0:parts, 0:n], x[0:parts, 0:n], ALU.mult, [tb, xbf], [tb])
        ACT(t[0:parts, 0:n], t[0:parts, 0:n], AF.Sigmoid, [tb], [tb], scale=1.5957691216057308)
        TT(out_ap, x[0:parts, 0:n], t[0:parts, 0:n], ALU.mult, [xbf, tb], obufs)

    def proj(ps_ap, pbuf, wv, wbuf, kcs, cols, c0, n):
        xbufs = tok_bufs(XB, c0, n)
        for kc in range(kcs):
            MM(ps_ap, wv[:, kc, cols[0]:cols[1]], xb[:, kc, c0:c0 + n], kc == 0, kc == kcs - 1,
               [wbuf] + xbufs, [pbuf])

    YA, YB, YC, YD = B("ya"), B("yb"), B("yc"), B("yd")
    MG32, MGB = B("mg32"), B("mgb")
    QTA = B("QTa")
    VTM = B("vtm")
    CBT, CDT = B("cbT"), B("cdT")
    CBP, CDP = B("cbprev"), B("cdprev")
    GV, VN = B("gv"), B("vn")
    SQ = B("sqkv")
    KMT = B("kmT")
    OUTB = B("outs")
    XT = B("xt")
    AWS = B("aws")

    def sample_attention(l):
        KC, SS, PS_, QBC, PVP, PVC, SDG, SSM, IDX = (B("Kc"), B("Ssc"), B("Psc"), B("qbc"), B("pvp"), B("pvc"),
                                                     B("sdg"), B("ssm"), B("idx"))
        for s_ in range(NS):
            psq, pbq = nextps()
            for h in range(4):
                TS(sdg[:, :], ident_f[0:64, 0:64], QTs[:, h, s_:s_ + 1], None, ALU.mult, None, [CSTB, SQ], [SDG])
                MM(psq[:, h * 64:(h + 1) * 64], ones_f[0:64, :], sdg[:, :], True, True, [CSTB, SDG], [pbq])
            CP(qbc[:, :], psq[:, 0:256], [pbq], [QBC])
            for ck in range(NCHK):
                TS(idx[:, 0:1], pti[:, s_:s_ + 1], NCHK, l * NPOOL * NCHK + ck, ALU.mult, ALU.add, [CSTB], [IDX])
                P.add("pool", lambda e: e.indirect_dma_start(
                    out=Kc[:, :], out_offset=None, in_=ck_in,
                    in_offset=bass.IndirectOffsetOnAxis(ap=idx[:, 0:1], axis=0)), [IDX], [KC], kind="d")
                kv = Kc[:, :].rearrange("p (a b) -> p a b", b=256)
                TT(kv, kv, qbc[:, :].rearrange("p (o b) -> p o b", o=1).to_broadcast([128, PCH, 256]), ALU.mult,
                   [KC, QBC], [KC])
                RED(Ssc[:, ck * PCH:(ck + 1) * PCH, :], Kc[:, :].rearrange("p (a h d) -> p a h d", h=4, d=64), ALU.add,
                    [KC], [SS])
                yield
            RED(ssm[:, 0:4], Ssc[:, :, :].rearrange("p a h -> p h a"), ALU.add, [SS], [SSM])
            psg, pbg = nextps()
            MM(psg[0:4, 0:64], ssm[:, 0:4], cs(C_PAIR, 64), True, True, [SSM, CSTB], [pbg])
            t, tb = MTP.next()
            CP(t[0:4, 0:64], psg[0:4, 0:64], [pbg], [tb])
            MAX8(m8s[0:4, :], t[0:4, 0:64], [tb], [B("m8s")])
            TS(t[0:4, 0:64], t[0:4, 0:64], m8s[0:4, 2:3], None, ALU.is_ge, None, [tb, B("m8s")], [tb])
            pst, pbt = nextps()
            TR(pst[0:64, 0:4], t[0:4, 0:64], ident_f[0:4, 0:4], [tb, CSTB], [pbt])
            t2, tb2 = MTP.next()
            CP(t2[0:64, 0:4], pst[0:64, 0:4], [pbt], [tb2])
            psm, pbm = nextps()
            MM(psm[:, 0:4], cs(C_PAIRT, 128, 64), t2[0:64, 0:4], True, True, [CSTB, tb2], [pbm])
            TS(ssm[:, 4:8], psm[:, 0:4], -1.0, -NEG, ALU.add, ALU.mult, [pbm], [SSM])
            for h in range(4):
                ACT(Psc[:, :, h], Ssc[:, :, h], AF.Exp, [SS, SSM], [PS_], bias=ssm[:, 4 + h:5 + h])
            RED(ssm[:, 8:12], Psc[:, :, :].rearrange("p a h -> p h a"), ALU.add, [PS_], [SSM])
            for ck in range(NCHK):
                TS(idx[:, 1:2], pti[:, s_:s_ + 1], NCHK, l * NPOOL * NCHK + ck, ALU.mult, ALU.add, [CSTB], [IDX])
                P.add("pool", lambda e: e.indirect_dma_start(
                    out=Kc[:, :], out_offset=None, in_=cv_in,
                    in_offset=bass.IndirectOffsetOnAxis(ap=idx[:, 1:2], axis=0)), [IDX], [KC], kind="d")
                kv4 = Kc[:, :].rearrange("p (a h d) -> p a h d", h=4, d=64)
                for h in range(4):
                    TT(kv4[:, :, h, :], kv4[:, :, h, :],
                       Psc[:, ck * PCH:(ck + 1) * PCH, h:h + 1].to_broadcast([128, PCH, 64]), ALU.mult, [KC, PS_], [KC])
                dst, dstb = (pvp, PVP) if ck == 0 else (pvc, PVC)
                RED(dst[:, :], Kc[:, :].rearrange("p (a c) -> p c a", c=256), ALU.add, [KC], [dstb])
                if ck > 0:
                    TT(pvp[:, :], pvp[:, :], pvc[:, :], ALU.add, [PVP, PVC], [PVP])
                yield
            psn, pbn = nextps()
            for h in range(4):
                MM(psn[0:64, h:h + 1], pvp[:, h * 64:(h + 1) * 64], ones_f[:, 0:1], True, True, [PVP, CSTB], [pbn])
            psd, pbd = nextps()
            MM(psd[0:64, 0:4], ones_f[:, 0:64], ssm[:, 8:12], True, True, [CSTB, SSM], [pbd])
            t, tb = MTP.next()
            TT(t[0:64, 0:4], QTs[:, :, s_], KTs[:, :, s_], ALU.mult, [SQ], [tb])
            pss, pbs = nextps()
            MM(pss[0:64, 0:4], ones_f[0:64, 0:64], t[0:64, 0:4], True, True, [CSTB, tb], [pbs])
            t2, tb2 = MTP.next()
            ACT(t2[0:64, 0:4], pss[0:64, 0:4], AF.Exp, [pbs], [tb2])
            t3, tb3 = MTP.next()
            TT(t3[0:64, 0:4], t2[0:64, 0:4], VTs[:, :, s_], ALU.mult, [tb2, SQ], [tb3])
            TT(t3[0:64, 0:4], t3[0:64, 0:4], psn[0:64, 0:4], ALU.add, [tb3, pbn], [tb3])
            TT(t2[0:64, 0:4], t2[0:64, 0:4], psd[0:64, 0:4], ALU.add, [tb2, pbd], [tb2])
            RCP(t2[0:64, 0:4], t2[0:64, 0:4], [tb2], [tb2])
            TT(ycs[:, :, s_], t3[0:64, 0:4], t2[0:64, 0:4], ALU.mult, [tb3, tb2], [B("ycs")])
            yield

    def layernorm_x(l, i, c0, n):
        bufs = tok_bufs(XF, c0, n)
        bufsb = tok_bufs(XB, c0, n)
        layernorm(FTP, [(xf[:, m, c0:c0 + n], bufs) for m in range(8)], n, 1024.0,
                  [spt[l][:, SP_LNG + i * 8 + m:SP_LNG + i * 8 + m + 1] for m in range(8)],
                  [spt[l][:, SP_LNB + i * 8 + m:SP_LNB + i * 8 + m + 1] for m in range(8)],
                  [(xf[:, m, c0:c0 + n], bufs) for m in range(8)],
                  [(xb[:, m, c0:c0 + n], bufsb) for m in range(8)])

    for l in range(DEPTH):
        sp = lambda off, n=1, p=128, l=l: spt[l][0:p, off:off + n]
        xsrc = x_in if l == 0 else xs2
        XSRC = [B("xsrc", l, i) for i in range(9)] if l == 0 else [B("xs2", i) for i in range(9)]
        XS1 = [B("xs1", i) for i in range(9)]
        XS2 = [B("xs2", i) for i in range(9)]
        CCB = B("cc", l)
        CG = B("ccg", l)
        DMA("pool", Kh[64:104, :], koh_in, [], [KH])
        P.add("dve", lambda e: e.memset(Vh[:, :, 64:65], 1.0), [], [VH])
        wC, wCb = loadw(win[l][:, :, 1280:2048], 128, 8 * 768, a=8)
        wCv = w3(wC, 8)
        for ti, (c0, n) in enumerate(MT):
            smp = ti == 8
            for h in range(4):
                ps, pb = nextps()
                proj(ps[0:64, 0:n], pb, wCv, wCb, 8, (256 + h * 64, 256 + h * 64 + 64), c0, n)
                t, tb = MTP.next()
                ACT(t[0:64, 0:n], ps[0:64, 0:n], AF.Copy, [pb], [tb])
                DMA("sp", o_k[l][:, h, c0:c0 + n], t[0:64, 0:n], [tb], [OUTB])
                if smp:
                    CP(KTs[:, h, :], t[0:64, 0:n], [tb], [SQ])
                else:
                    CP(kTb[:, h, 0:n], t[0:64, 0:n], [tb], [B("kTb")])
            if not smp:
                for h in range(4):
                    DMA("sp", cc_kt[l][h * 64:(h + 1) * 64, c0:c0 + n], kTb[:, h, 0:n], [B("kTb")], [CCB])
            for vc in range(2):
                ps, pb = nextps()
                proj(ps[:, 0:n], pb, wCv, wCb, 8, (512 + vc * 128, 512 + vc * 128 + 128), c0, n)
                t, tb = MTP.next()
                ACT(t[:, 0:n], ps[:, 0:n], AF.Copy, [pb], [tb])
                DMA("sp", o_v[l][:, vc, c0:c0 + n], t[:, 0:n], [tb], [OUTB])
                if not smp:
                    for c in range(2):
                        pt_, ptb = nextps()
                        TR(pt_[:, 0:128], t[:, c * 128:(c + 1) * 128], ident_f, [tb, CSTB], [ptb])
                        kt = ti * 2 + c
                        CP(vtm[:, 2 * vc:2 * vc + 2, kt, :],
                           pt_[:, 0:128].rearrange("p (a b) -> p a b", a=2), [ptb], [VTM],
                           eng="act" if c % 2 else "dve")
            if smp:
                for h in range(4):
                    ps, pb = nextps()
                    proj(ps[0:64, 0:n], pb, wCv, wCb, 8, (h * 64, h * 64 + 64), c0, n)
                    ACT(QTs[:, h, :], ps[0:64, 0:n], AF.Copy, [pb], [SQ], scale=0.125)
                    ps, pb = nextps()
                    proj(ps[0:64, 0:n], pb, wCv, wCb, 8, (512 + h * 64, 512 + h * 64 + 64), c0, n)
                    ACT(VTs[:, h, :], ps[0:64, 0:n], AF.Copy, [pb], [SQ])
        DMA("sp", cc_v[l], vtm.rearrange("p a b c -> p (a b c)"), [VTM], [CCB])
        wBt, wBtb = loadw(win[l][:, :, 768:1280], 128, 8 * 512, a=8)
        wBtv = w3(wBt, 8)
        wDt, wDtb = loadw(win[l][:, :, 2048:2560], 128, 8 * 512, a=8)
        wDtv = w3(wDt, 8)
        c0t, nt = NPR - 32, 32
        TL = B("tails")
        for cc in range(2):
            psc, pbc = nextps()
            proj(psc[:, 0:nt], pbc, wBtv, wBtb, 8, (cc * 128, cc * 128 + 128), c0t, nt)
            psx, pbx = nextps()
            proj(psx[:, 0:nt], pbx, wBtv, wBtb, 8, (256 + cc * 128, 256 + cc * 128 + 128), c0t, nt)
            t, tb = MTP.next()
            ACT(t[:, 0:nt], psc[:, 0:nt], AF.Copy, [pbc], [tb])
            TT(tails[:, cc * 32:cc * 32 + 2], t[:, 30:32], psx[:, 30:32], ALU.mult, [tb, pbx], [TL])
            psa, pba = nextps()
            proj(psa[:, 0:nt], pba, wDtv, wDtb, 8, (cc * 128, cc * 128 + 128), c0t, nt)
            psg, pbg = nextps()
            proj(psg[:, 0:nt], pbg, wDtv, wDtb, 8, (256 + cc * 128, 256 + cc * 128 + 128), c0t, nt)
            t, tb = MTP.next()
            ACT(t[:, 0:nt], psg[:, 0:nt], AF.Sigmoid, [pbg], [tb])
            TT(tails[:, cc * 32 + 2:cc * 32 + 32], t[:, 2:32], psa[:, 2:32], ALU.mult, [tb, pba], [TL])
        DMA("sp", cc_t[l], tails[:, :], [TL], [CCB])
        for src, dst in ((cc_kt[l], cc_ktg[l]), (cc_v[l], cc_vg[l]), (cc_t[l], cc_tg[l])):
            P.add("pool", lambda e, s=src, d=dst: e.collective_compute(
                "AllGather", ALU.bypass, replica_groups=GROUPS, ins=[s], outs=[d]), [CCB], [CG], kind="cc")
        TG = B("tailg")
        DMA("sp", tailg[:, :, :], cc_tg[l].rearrange("(r p) c -> p r c", p=128), [CG], [TG])
        for rk in range(4):
            selc = cst[:, C_SELR + rk:C_SELR + rk + 1]
            for cc in range(2):
                if rk == 0:
                    TS(cbprev[:, cc, :], tailg[:, rk, cc * 32:cc * 32 + 2], selc, None, ALU.mult, None,
                       [TG, CSTB], [CBP])
                    TS(cdprev[:, cc, :], tailg[:, rk, cc * 32 + 2:cc * 32 + 32], selc, None, ALU.mult, None,
                       [TG, CSTB], [CDP])
                else:
                    STT(cbprev[:, cc, :], tailg[:, rk, cc * 32:cc * 32 + 2], selc, cbprev[:, cc, :],
                        ALU.mult, ALU.add, [TG, CSTB, CBP], [CBP])
                    STT(cdprev[:, cc, :], tailg[:, rk, cc * 32 + 2:cc * 32 + 32], selc, cdprev[:, cc, :],
                        ALU.mult, ALU.add, [TG, CSTB, CDP], [CDP])

        def load_K(h, l=l, CG=CG, CCB=CCB):
            DMA("sp", Kh[0:64, 0:8192].rearrange("p (r t) -> p r t", r=4),
                cc_ktg[l].rearrange("(r hh d) t -> d r hh t", r=4, hh=4)[:, :, h, :], [CG], [KH])
            DMA("sp", Kh[0:64, 8192:10240], cc_kt[l][h * 64:(h + 1) * 64, :], [CCB], [KH])

        def load_V(h, l=l, CG=CG, CCB=CCB):
            for r_ in range(4):
                DMA("sp", Vh[:, r_ * 16:(r_ + 1) * 16, 0:64],
                    cc_vg[l][r_ * 128:(r_ + 1) * 128, :].rearrange("p (hh k d) -> p hh k d", hh=4, k=16)[:, h, :, :],
                    [CG], [VH])
            DMA("sp", Vh[:, 64:80, 0:64],
                cc_v[l].rearrange("p (hh k d) -> p hh k d", hh=4, k=16)[:, h, :, :], [CCB], [VH])

        for h in range(4):
            load_K(h)
            t, tb = MTP.next()
            RED(t[0:64, 0:40], Kh[0:64, :].rearrange("p (b k) -> p b k", k=256), ALU.add, [KH], [tb])
            CP(kmT[:, h, :], t[0:64, 0:40], [tb], [KMT])
        sa_gen = sample_attention(l)
        DMA("sp", awsf[:, :, :], awsT[l], [], [AWS])
        DMA("sp", absf[:, :, :], absr[l], [], [AWS])
        for h in range(4):
            TT(awsb[:, h, :], awsf[:, h, :], cs(C_TRI, 128), ALU.mult, [AWS, CSTB], [B("awsb")])

        for ti, (c0, n) in enumerate(MT):
            smp = ti == 8
            DMA("sp", xt[:, :, 0:n], xsrc[:, :, c0:c0 + n], [XSRC[ti]], [XT])
            wA, wAb = loadw(win[l][:, :, 0:512], 128, 8 * 512, a=8)
            wAv = w3(wA, 8)
            for h in range(4):
                ps, pb = nextps()
                proj(ps[0:64, 0:n], pb, wAv, wAb, 8, (h * 64, h * 64 + 64), c0, n)
                gelu_from(ps[0:64, 0:n], pb, ya[:, h, 0:n], [YA], n, parts=64)
            for vc in range(2):
                ps, pb = nextps()
                proj(ps[:, 0:n], pb, wAv, wAb, 8, (256 + vc * 128, 256 + vc * 128 + 128), c0, n)
                gelu_from(ps[:, 0:n], pb, gv[:, vc, 0:n], [GV], n)
            layernorm(MTP, [(gv[:, vc, 0:n], [GV]) for vc in range(2)], n, 256.0,
                      [sp(SP_ALNG + vc) for vc in range(2)], [sp(SP_ALNB + vc) for vc in range(2)],
                      [(vn[:, vc, 0:n], [VN]) for vc in range(2)], None)
            if not smp:
                for c in range(2):
                    for vc in range(2):
                        pt_, ptb = nextps()
                        TR(pt_[:, 0:128], vn[:, vc, c * 128:(c + 1) * 128], ident_f, [VN, CSTB], [ptb])
                        CP(vtA[:, vc * 128:(vc + 1) * 128], pt_[:, 0:128], [ptb], [B("vtA")],
                           eng="act" if vc else "dve")
                    for h in range(4):
                        ps, pb = nextps()
                        MM(ps[0:64, 0:128], vtA[:, h * 64:(h + 1) * 64], awsb[:, h, :], True, False,
                           [B("vtA"), B("awsb")], [pb])
                        MM(ps[0:64, 0:128], ones_f[0:1, 0:64], absf[0:1, h, :], False, True,
                           [CSTB, AWS], [pb])
                        TT(ya[:, h, c * 128:(c + 1) * 128], ya[:, h, c * 128:(c + 1) * 128], ps[0:64, 0:128],
                           ALU.mult, [YA, pb], [YA])
            else:
                DMA("sp", o_cv[l], vn[:, :, 0:NS], [VN], [OUTB])
                for h in range(4):
                    ps, pb = nextps()
                    MM(ps[0:64, 0:n], ident_f[:, (h % 2) * 64:(h % 2) * 64 + 64], vn[:, h // 2, 0:n], True, True,
                       [VN, CSTB], [pb])
                    t, tb = MTP.next()
                    TS(t[0:64, 0:n], ps[0:64, 0:n], sp(SP_AW00 + h, 1, 64), sp(SP_ABS0 + h, 1, 64),
                       ALU.mult, ALU.add, [pb, CSTB], [tb])
                    TT(ya[:, h, 0:n], ya[:, h, 0:n], t[0:64, 0:n], ALU.mult, [YA, tb], [YA])
            wB, wBb = loadw(win[l][:, :, 512:1280], 128, 8 * 768, a=8)
            wBv = w3(wB, 8)
            CBS, CDS, SM3 = B("cbs"), B("cds"), B("sm3")
            if smp:
                DMA("sp", cbs[:, :, :, 0:2], scb_in[l], [], [CBS])
            for cc in range(2):
                psc, pbc = nextps()
                proj(psc[:, 0:n], pbc, wBv, wBb, 8, (256 + cc * 128, 256 + cc * 128 + 128), c0, n)
                psx, pbx = nextps()
                proj(psx[:, 0:n], pbx, wBv, wBb, 8, (512 + cc * 128, 512 + cc * 128 + 128), c0, n)
                psb_, pbb = nextps()
                proj(psb_[:, 0:n], pbb, wBv, wBb, 8, (cc * 128, cc * 128 + 128), c0, n)
                t, tb = MTP.next()
                ACT(t[:, 0:n], psc[:, 0:n], AF.Copy, [pbc], [tb])
                a, ab = MTP.next()
                if not smp:
                    CP(cbT[:, cc, 0:2], cbprev[:, cc, :], [CBP], [CBT])
                    TT(cbT[:, cc, 2:2 + n], t[:, 0:n], psx[:, 0:n], ALU.mult, [tb, pbx], [CBT])
                    TS(a[:, 0:n], cbT[:, cc, 0:n], sp(SP_BCW + cc * 3), None, ALU.mult, None, [CBT, CSTB], [ab])
                    for k in (1, 2):
                        STT(a[:, 0:n], cbT[:, cc, k:k + n], sp(SP_BCW + cc * 3 + k), a[:, 0:n], ALU.mult, ALU.add,
                            [CBT, CSTB, ab], [ab])
                    TT(yb[:, cc, 0:n], a[:, 0:n], psb_[:, 0:n], ALU.mult, [ab, pbb], [YB])
                    CP(cbprev[:, cc, :], cbT[:, cc, n:n + 2], [CBT], [CBP])
                else:
                    TT(cbs[:, cc, :, 2], t[:, 0:n], psx[:, 0:n], ALU.mult, [tb, pbx], [CBS])
                    wv_ = spt[l][:, SP_BCW + cc * 3:SP_BCW + cc * 3 + 3]
                    TT(sm3[:, cc, :, 0:3], cbs[:, cc, :, :],
                       wv_.rearrange("p (o k) -> p o k", o=1).to_broadcast([128, NS, 3]),
                       ALU.mult, [CBS, CSTB], [SM3])
                    RED(a[:, 0:n], sm3[:, cc, :, 0:3], ALU.add, [SM3], [ab])
                    TT(yb[:, cc, 0:n], a[:, 0:n], psb_[:, 0:n], ALU.mult, [ab, pbb], [YB])
            if ti == 7:
                DMA("sp", o_cb[l], cbprev[:, :, :], [CBP], [OUTB])
            if smp:
                DMA("sp", o_cbs[l], cbs[:, :, :, 1:3], [CBS], [OUTB])
            wD, wDb = loadw(win[l][:, :, 2048:2560], 128, 8 * 512, a=8)
            wDv = w3(wD, 8)
            if smp:
                DMA("sp", cds[:, :, :, 0:30], scd_in[l], [], [CDS])
            dsrc = []
            for cc in range(2):
                psa, pba = nextps()
                proj(psa[:, 0:n], pba, wDv, wDb, 8, (cc * 128, cc * 128 + 128), c0, n)
                psg, pbg = nextps()
                proj(psg[:, 0:n], pbg, wDv, wDb, 8, (256 + cc * 128, 256 + cc * 128 + 128), c0, n)
                t, tb = MTP.next()
                ACT(t[:, 0:n], psg[:, 0:n], AF.Sigmoid, [pbg], [tb])
                a, ab = dacc[cc], B("dacc", cc)
                if not smp:
                    CP(cdT[:, cc, 0:30], cdprev[:, cc, :], [CDP], [CDT])
                    TT(cdT[:, cc, 30:30 + n], t[:, 0:n], psa[:, 0:n], ALU.mult, [tb, pba], [CDT])
                    TS(a[:, 0:n], cdT[:, cc, 0:n], sp(SP_DCW + cc * 31), sp(SP_DCB + cc), ALU.mult, ALU.add,
                       [CDT, CSTB], [ab])
                    for k in range(1, 31):
                        STT(a[:, 0:n], cdT[:, cc, k:k + n], sp(SP_DCW + cc * 31 + k), a[:, 0:n], ALU.mult, ALU.add,
                            [CDT, CSTB, ab], [ab])
                    CP(cdprev[:, cc, :], cdT[:, cc, n:n + 30], [CDT], [CDP])
                else:
                    TT(cds[:, cc, :, 30], t[:, 0:n], psa[:, 0:n], ALU.mult, [tb, pba], [CDS])
                    wv_ = spt[l][:, SP_DCW + cc * 31:SP_DCW + cc * 31 + 31]
                    TT(sm3[:, cc, :, :], cds[:, cc, :, :],
                       wv_.rearrange("p (o k) -> p o k", o=1).to_broadcast([128, NS, 31]),
                       ALU.mult, [CDS, CSTB], [SM3])
                    RED(a[:, 0:n], sm3[:, cc, :, :], ALU.add, [SM3], [ab])
                    TS(a[:, 0:n], a[:, 0:n], sp(SP_DCB + cc), None, ALU.add, None, [ab, CSTB], [ab])
                dsrc.append((a[:, 0:n], [ab]))
            layernorm(MTP, dsrc, n, 256.0, [sp(SP_DLNG + cc) for cc in range(2)],
                      [sp(SP_DLNB + cc) for cc in range(2)],
                      None, [(yd[:, cc, 0:n], [YD]) for cc in range(2)], silu=True)
            if ti == 7:
                DMA("sp", o_cd[l], cdprev[:, :, :], [CDP], [OUTB])
            if smp:
                DMA("sp", o_cds[l], cds[:, :, :, 1:31], [CDS], [OUTB])
            if not smp:
                for _ in range(9):
                    next(sa_gen, None)
                g = ti
                wq, wqb = loadw(win[l][:, :, 1280:1536], 128, 8 * 256, a=8)
                wqv = w3(wq, 8)
                for h in range(4):
                    ps, pb = nextps()
                    proj(ps[0:64, 0:n], pb, wqv, wqb, 8, (h * 64, h * 64 + 64), c0, n)
                    ACT(QTa[0:64, h, 0:n], ps[0:64, 0:n], AF.Copy, [pb], [QTA], scale=0.125)
                SM, SM2, M8 = B("small"), B("small2"), B("m8")
                PBv = cst[:, C_PB + g * 40:C_PB + g * 40 + 40]
                PVv = cst[:, C_PV + g * 40:C_PV + g * 40 + 40]
                POv = cst[:, C_PO + g * 40:C_PO + g * 40 + 40]
                bc3 = lambda v: v.rearrange("p (o b) -> p o b", o=1).to_broadcast([128, 4, 40])
                for c in range(2):
                    psg, pbg = nextps()
                    for h in range(4):
                        MM(psg[:, h * 40:h * 40 + 40], QTa[0:64, h, c * 128:(c + 1) * 128], kmT[:, h, :], True, True,
                           [QTA, KMT], [pbg])
                    TT(small[:, :, :], psg[:, 0:160].rearrange("p (h b) -> p h b", h=4), bc3(PBv), ALU.add,
                       [pbg, CSTB], [SM])
                    for h in range(4):
                        MAX8(m8[:, :], small[:, h, :], [SM], [M8])
                        TS(small2[:, h, 64:104], small[:, h, :], m8[:, 2:3], None, ALU.is_ge, None, [SM, M8], [SM2])
                    TT(small2[:, :, 64:104], small2[:, :, 64:104], bc3(PVv), ALU.mult, [SM2, CSTB], [SM2])
                    TT(small2[:, :, 64:104], small2[:, :, 64:104], bc3(POv), ALU.add, [SM2, CSTB], [SM2])
                    TS(small2[:, :, 64:104], small2[:, :, 64:104], -1.0, -NEG, ALU.add, ALU.mult, [SM2], [SM2])
                    for h in range(4):
                        pt_, ptb = nextps()
                        TR(pt_[0:104, 0:128], small2[:, h, :], ident_f, [SM2, CSTB], [ptb])
                        CP(QTa[64:104, h, c * 128:(c + 1) * 128], pt_[64:104, 0:128], [ptb], [QTA],
                           eng="act" if h % 2 else "dve")
                for h in range(4):
                    load_K(h)
                    load_V(h)
                    kts = list(range(64)) + [64 + j for j in range(2 * (g + 1))]
                    acc, accb = psum[6 + h % 2], psb[6 + h % 2]
                    for i, kt in enumerate(kts):
                        ps, pb = nextps()
                        own = kt >= 64 + 2 * g
                        MM(ps[:, 0:256], Kh[0:104, kt * 128:(kt + 1) * 128], QTa[0:104, h, 0:256], True, not own,
                           [KH, QTA], [pb])
                        if own:
                            j = kt - (64 + 2 * g)
                            MM(ps[:, 0:256], ident_b, cstb[:, CB_CAUS + j * 256:CB_CAUS + (j + 1) * 256], False, True,
                               [CSTB], [pb])
                        pp, ppb = ptile[i % 2], B("ptile", i % 2)
                        ACT(pp[:, :], ps[:, 0:256], AF.Exp, [pb], [ppb])
                        MM(acc[0:65, 0:256], Vh[:, kt, 0:65], pp[:, :], i == 0, i == len(kts) - 1, [VH, ppb], [accb])
                    ACS = B("accS")
                    CP(accS[:, :], acc[0:65, 0:256], [accb], [ACS])
                    ps, pb = nextps()
                    MM(ps[0:64, 0:256], ones_f[64:65, 0:64], accS[64:65, :], True, True, [CSTB, ACS], [pb])
                    t, tb = MTP.next()
                    RCP(t[0:64, 0:256], ps[0:64, 0:256], [pb], [tb])
                    TT(yc[:, h, 0:256], accS[0:64, :], t[0:64, 0:256], ALU.mult, [ACS, tb], [YC])
            else:
                for _ in sa_gen:
                    pass
                CP(yc[:, :, 0:NS], ycs[:, :, :], [B("ycs")], [YC])
            if DEBUG and l == 0:
                DMA('sp', d_ya[:, :, c0:c0 + n], ya[:, :, 0:n], [YA], [OUTB])
                DMA('sp', d_yb[:, :, c0:c0 + n], yb[:, :, 0:n], [YB], [OUTB])
                DMA('sp', d_yc[:, :, c0:c0 + n], yc[:, :, 0:n], [YC], [OUTB])
                DMA('sp', d_yd[:, :, c0:c0 + n], yd[:, :, 0:n], [YD], [OUTB])
            ysrc = [(ya, YA, 64, 4, wbrA), (yb, YB, 128, 2, wbrB), (yc, YC, 64, 4, wbrC), (yd, YD, 128, 2, wbrD)]
            for j, (yt, ybuf, kp, nk, wsrc) in enumerate(ysrc):
                for half in range(2):
                    wb_, wbb = loadw(wsrc[l][:, :, half * 512:(half + 1) * 512], kp, nk * 512, a=nk)
                    wbv = w3(wb_, nk)
                    wg_, wgb = loadw(wgate[l][:, :, j * 1024 + half * 512:j * 1024 + (half + 1) * 512], 128, 8 * 512, a=8)
                    wgv = w3(wg_, 8)
                    for mm_ in range(4):
                        m = half * 4 + mm_
                        psb_, pbb = nextps()
                        for k in range(nk):
                            MM(psb_[:, 0:n], wbv[:, k, mm_ * 128:(mm_ + 1) * 128], yt[:, k, 0:n], k == 0, k == nk - 1,
                               [wbb, ybuf], [pbb])
                        psg, pbg = nextps()
                        proj(psg[:, 0:n], pbg, wgv, wgb, 8, (mm_ * 128, mm_ * 128 + 128), c0, n)
                        t, tb = MTP.next()
                        ACT(t[:, 0:n], psg[:, 0:n], AF.Sigmoid, [pbg, CSTB], [tb], bias=sp(SP_BGATE + j * 8 + m))
                        if j == 0:
                            TT(mg32[:, m, 0:n], t[:, 0:n], psb_[:, 0:n], ALU.mult, [tb, pbb], [MG32])
                        else:
                            TT(t[:, 0:n], t[:, 0:n], psb_[:, 0:n], ALU.mult, [tb, pbb], [tb])
                            if j < 3:
                                TT(mg32[:, m, 0:n], mg32[:, m, 0:n], t[:, 0:n], ALU.add, [MG32, tb], [MG32])
                            else:
                                TT(mgb[:, m, 0:n], mg32[:, m, 0:n], t[:, 0:n], ALU.add, [MG32, tb], [MGB])
            if DEBUG and l == 0:
                DMA('sp', d_mg[:, :, c0:c0 + n], mgb[:, :, 0:n], [MGB], [OUTB])
            for half in range(2):
                wo_, wob = loadw(wout[l][:, :, half * 512:(half + 1) * 512], 128, 8 * 512, a=8)
                wov = w3(wo_, 8)
                for mm_ in range(4):
                    m = half * 4 + mm_
                    ps, pb = nextps()
                    for kc in range(8):
                        MM(ps[:, 0:n], wov[:, kc, mm_ * 128:(mm_ + 1) * 128], mgb[:, kc, 0:n], kc == 0, kc == 7,
                           [wob, MGB], [pb])
                    STT(xt[:, m, 0:n], xt[:, m, 0:n], ALPHA, ps[:, 0:n], ALU.mult, ALU.add, [XT, pb], [XT])
            layernorm(MTP, [(xt[:, m, 0:n], [XT]) for m in range(8)], n, 1024.0,
                      [sp(SP_LNG + m) for m in range(8)], [sp(SP_LNB + m) for m in range(8)],
                      [(xt[:, m, 0:n], [XT]) for m in range(8)],
                      [(xb[:, m, c0:c0 + n], [XB[ti]]) for m in range(8)])
            DMA("sp", xs1[:, :, c0:c0 + n], xt[:, :, 0:n], [XT], [XS1[ti]])
            if DEBUG and l == 0:
                DMA('sp', d_ln1[:, :, c0:c0 + n], xt[:, :, 0:n], [XT], [OUTB])
        FENCE()
        for ti, (c0, n) in enumerate(MT):
            DMA("sp", xf[:, :, c0:c0 + n], xs1[:, :, c0:c0 + n], [XS1[ti]], [XF[ti]])
        HB = [B("hb", i) for i in range(4)]
        SG = [B("sg", i) for i in range(3)]
        CMB = [B("cmb", i) for i in range(1)]
        CMT = B("cmbT")
        hrr = [0]
        moe = l % 2 == 1
        if moe:
            LG, FM8, FSC, EX8 = B("lg"), B("fm8"), B("fsc"), B("ex8")
            chunks = [(c * 128, 128) for c in range(16)] + [(NPR, NS)]
            for (t0, tn) in chunks:
                xfb = tok_bufs(XF, t0, tn)
                ps, pb = nextps()
                for kc in range(8):
                    MM(ps[0:tn, 0:8], xf[:, kc, t0:t0 + tn], wrt[:, kc, :], kc == 0, False, xfb + [CSTB], [pb])
                MM(ps[0:tn, 0:8], ones_f[0:1, 0:tn], brt[0:1, :], False, True, [CSTB], [pb])
                CP(lg[0:tn, :], ps[0:tn, 0:8], [pb], [LG])
                MAX8(fm8[0:tn, :], lg[0:tn, :], [LG], [FM8])
                TS(fsc[0:tn, 0:1], fm8[0:tn, 0:1], -1.0, None, ALU.mult, None, [FM8], [FSC])
                ACT(ex8[0:tn, :], lg[0:tn, :], AF.Exp, [LG, FSC], [EX8], bias=fsc[0:tn, 0:1])
                TS(lg[0:tn, :], lg[0:tn, :], fm8[0:tn, 1:2], None, ALU.is_ge, None, [LG, FM8], [LG])
                TT(ex8[0:tn, :], ex8[0:tn, :], lg[0:tn, :], ALU.mult, [EX8, LG], [EX8])
                RED(fsc[0:tn, 1:2], ex8[0:tn, :], ALU.add, [EX8], [FSC])
                RCP(fsc[0:tn, 2:3], fsc[0:tn, 1:2], [FSC], [FSC])
                TS(ex8[0:tn, :], ex8[0:tn, :], fsc[0:tn, 2:3], None, ALU.mult, None, [EX8, FSC], [EX8])
                pt_, ptb = nextps()
                TR(pt_[0:8, 0:tn], ex8[0:tn, :], ident_f[0:tn, 0:tn], [EX8, CSTB], [ptb])
                CP(cmbT[:, t0:t0 + tn], pt_[0:8, 0:tn], [ptb], [CMT])
        for ti, (c0, n) in enumerate(MT):
            for m in range(8):
                ACT(xf[:, m, c0:c0 + n], xf[:, m, c0:c0 + n], AF.Copy, [XF[ti]], [XF[ti]], scale=ALPHA)
        ngroups = 8 * NMOE_G if moe else NFFN_G
        wsrc = moew if moe else ffnw
        for gi in range(ngroups):
            e_ = gi // NMOE_G
            if moe and gi % NMOE_G == 0:
                for (c0, n) in FT:
                    ps, pb = nextps()
                    MM(ps[:, 0:n], cst[0:8, C_SEL8 + e_ * 128:C_SEL8 + (e_ + 1) * 128], cmbT[0:8, c0:c0 + n], True, True,
                       [CSTB, CMT], [pb])
                    CP(cmbbc[0][:, c0:c0 + n], ps[:, 0:n], [pb], [CMB[0]], eng="act")
            wf_, wfb = loadw(wsrc[gi], 128, 6144)
            wgv = w3(wf_[:, 0:2048], 8)
            wuv = w3(wf_[:, 2048:4096], 8)
            wdv = w3(wf_[:, 4096:6144], 2)
            for (c0, n) in FT:
                xfb = tok_bufs(XF, c0, n)
                hs = []
                for hc in range(2):
                    psg, pbg = nextps()
                    proj(psg[:, 0:n], pbg, wgv, wfb, 8, (hc * 128, hc * 128 + 128), c0, n)
                    psu, pbu = nextps()
                    proj(psu[:, 0:n], pbu, wuv, wfb, 8, (hc * 128, hc * 128 + 128), c0, n)
                    si = hrr[0] % 3
                    hi = hrr[0] % 4
                    hrr[0] += 1
                    ACT(sgt[si][:, 0:n], psg[:, 0:n], AF.Sigmoid, [pbg], [SG[si]])
                    TT(sgt[si][:, 0:n], sgt[si][:, 0:n], psg[:, 0:n], ALU.mult, [SG[si], pbg], [SG[si]])
                    if moe:
                        TT(sgt[si][:, 0:n], sgt[si][:, 0:n], cmbbc[0][:, c0:c0 + n], ALU.mult,
                           [SG[si], CMB[0]], [SG[si]])
                    TT(hb[hi][:, 0:n], sgt[si][:, 0:n], psu[:, 0:n], ALU.mult, [SG[si], pbu], [HB[hi]])
                    hs.append((hb[hi], HB[hi]))
                for m in range(8):
                    ps, pb = nextps()
                    for hc in range(2):
                        MM(ps[:, 0:n], wdv[:, hc, m * 128:(m + 1) * 128], hs[hc][0][:, 0:n], hc == 0, hc == 1,
                           [wfb, hs[hc][1]], [pb])
                    TT(xf[:, m, c0:c0 + n], xf[:, m, c0:c0 + n], ps[:, 0:n], ALU.add, xfb + [pb], xfb)
        for (c0, n) in FT:
            layernorm_x(l, 1, c0, n)
            if DEBUG and l == 0:
                DMA('sp', d_ln2[:, :, c0:c0 + n], xf[:, :, c0:c0 + n], tok_bufs(XF, c0, n), [OUTB])
        PTB = B("pTb")
        for (c0, n) in FT:
            DMA("pool", pTb[:, :, c0:c0 + n], p_in[l][:, :, c0:c0 + n], [], [PTB])
        wppb = B("wppt")
        DMA("pool", wppt[:, :], plep[l].rearrange("p a b -> p (a b)"), [], [wppb])
        wppv = w3(wppt[:, :], 2)
        for (c0, n) in FT:
            xfb = tok_bufs(XF, c0, n)
            for half in range(2):
                wpg_, wpgb = loadw(pleg[l][:, :, half * 512:(half + 1) * 512], 128, 8 * 512, a=8)
                wpgv = w3(wpg_, 8)
                for mm_ in range(4):
                    m = half * 4 + mm_
                    ps1, pb1 = nextps()
                    proj(ps1[:, 0:n], pb1, wpgv, wpgb, 8, (mm_ * 128, mm_ * 128 + 128), c0, n)
                    ps2, pb2 = nextps()
                    for c in range(2):
                        MM(ps2[:, 0:n], wppv[:, c, m * 128:(m + 1) * 128], pTb[:, c, c0:c0 + n], c == 0, c == 1,
                           [wppb, PTB], [pb2])
                    si = hrr[0] % 3
                    hrr[0] += 1
                    ACT(sgt[si][:, 0:n], ps1[:, 0:n], AF.Sigmoid, [pb1], [SG[si]])
                    TT(sgt[si][:, 0:n], sgt[si][:, 0:n], ps2[:, 0:n], ALU.mult, [SG[si], pb2], [SG[si]])
                    STT(xf[:, m, c0:c0 + n], xf[:, m, c0:c0 + n], ALPHA, sgt[si][:, 0:n], ALU.mult, ALU.add,
                        xfb + [SG[si]], xfb)
            layernorm_x(l, 2, c0, n)
        if DEBUG and l == 0:
            for ti, (c0, n) in enumerate(MT):
                DMA('sp', d_ln3[:, :, c0:c0 + n], xf[:, :, c0:c0 + n], [XF[ti]], [OUTB])
        dst = o_y if l == DEPTH - 1 else xs2
        for ti, (c0, n) in enumerate(MT):
            DMA("sp", dst[:, :, c0:c0 + n], xf[:, :, c0:c0 + n], [XF[ti]], [OUTB if l == DEPTH - 1 else XS2[ti]])
        FENCE()

    emit(nc, P)
    return nc, len(P.ops)


def _fm(a, nch):
    return np.ascontiguousarray(a.reshape(nch, 128, -1).transpose(1, 0, 2))


_CACHE = {}


def _constants():
    c = np.zeros((128, NCST), np.float32)
    c[:, C_ID:C_ID + 128] = np.eye(128, dtype=np.float32)
    c[:, C_ONE:C_ONE + 128] = 1.0
    s = np.arange(128)
    c[:, C_TRI:C_TRI + 128] = (s[:, None] <= s[None, :]).astype(np.float32)
    pair = (s[:, None] // 2 == np.arange(64)[None, :]).astype(np.float32)
    c[:, C_PAIR:C_PAIR + 64] = pair
    c[0:64, C_PAIRT:C_PAIRT + 128] = pair.T
    for e in range(8):
        c[e, C_SEL8 + e * 128:C_SEL8 + (e + 1) * 128] = 1.0
    cb = np.zeros((128, NCSTB), np.float32)
    cb[:, CB_ID:CB_ID + 128] = np.eye(128, dtype=np.float32)
    q = np.arange(256)
    for j in range(2):
        key = j * 128 + s
        cb[:, CB_CAUS + j * 256:CB_CAUS + (j + 1) * 256] = np.where(key[:, None] <= q[None, :], 0.0, NEG)
    koh = np.zeros((40, 10240), np.float32)
    for b in range(40):
        koh[b, b * 256:(b + 1) * 256] = 1.0
    return c, cb, koh


def kernel(x_prompt, x_sample, p_prompt, p_sample, cache_k, cache_v, state_conv_b, state_conv_d, page_table,
           w_in, w_gate, b_gate, a_ln_g, a_ln_b, a_w_s, a_b_s, b_conv_w, d_conv_w, d_conv_b, d_ln_g, d_ln_b,
           w_branch, w_out, ln_g, ln_b, ffn_w_gate, ffn_w_up, ffn_w_down, moe_w_router, moe_b_router,
           moe_w_gate, moe_w_up, moe_w_down, ple_w_gate, ple_w_proj):
    f = lambda a: np.asarray(a, dtype=np.float32)
    x_prompt, x_sample, p_prompt, p_sample = f(x_prompt), f(x_sample), f(p_prompt), f(p_sample)
    cache_k, cache_v = f(cache_k), f(cache_v)
    if "nc" not in _CACHE:
        _CACHE["nc"] = build_program()
    nc, _ = _CACHE["nc"]

    sh = {}
    sh["win"] = np.stack([_fm(f(w_in[l]), 8) for l in range(2)])
    sh["wgate"] = np.stack([_fm(f(w_gate[l]), 8) for l in range(2)])
    wbr = f(w_branch)
    sh["wbrA"] = np.ascontiguousarray(wbr[:, 0].reshape(2, 4, 64, 1024).transpose(0, 2, 1, 3))
    sh["wbrB"] = np.stack([_fm(wbr[l, 1], 2) for l in range(2)])
    sh["wbrC"] = np.ascontiguousarray(wbr[:, 2].reshape(2, 4, 64, 1024).transpose(0, 2, 1, 3))
    sh["wbrD"] = np.stack([_fm(wbr[l, 3], 2) for l in range(2)])
    sh["wout"] = np.stack([_fm(f(w_out[l]), 8) for l in range(2)])
    aws = f(a_w_s)
    sh["awsT"] = np.ascontiguousarray(aws.transpose(0, 3, 1, 2))
    sh["absr"] = np.ascontiguousarray(f(a_b_s).reshape(2, 1, 4, 128))
    spa = np.zeros((2, 128, NSP), np.float32)
    for l in range(2):
        spa[l, :, SP_BGATE:SP_BGATE + 32] = f(b_gate[l]).reshape(32, 128).T
        spa[l, :, SP_LNG:SP_LNG + 24] = f(ln_g[l]).reshape(24, 128).T
        spa[l, :, SP_LNB:SP_LNB + 24] = f(ln_b[l]).reshape(24, 128).T
        spa[l, :, SP_ALNG:SP_ALNG + 2] = f(a_ln_g[l]).reshape(2, 128).T
        spa[l, :, SP_ALNB:SP_ALNB + 2] = f(a_ln_b[l]).reshape(2, 128).T
        spa[l, :, SP_DLNG:SP_DLNG + 2] = f(d_ln_g[l]).reshape(2, 128).T
        spa[l, :, SP_DLNB:SP_DLNB + 2] = f(d_ln_b[l]).reshape(2, 128).T
        spa[l, :, SP_DCB:SP_DCB + 2] = f(d_conv_b[l]).reshape(2, 128).T
        spa[l, :, SP_BCW:SP_BCW + 6] = f(b_conv_w[l]).reshape(3, 2, 128).transpose(2, 1, 0).reshape(128, 6)
        spa[l, :, SP_DCW:SP_DCW + 62] = f(d_conv_w[l]).reshape(31, 2, 128).transpose(2, 1, 0).reshape(128, 62)
        spa[l, :, SP_AW00:SP_AW00 + 4] = aws[l, :, 0, 0][None, :]
        spa[l, :, SP_ABS0:SP_ABS0 + 4] = f(a_b_s[l])[:, 0][None, :]
    sh["sp_in"] = spa

    def pack_ffn(wg, wu, wd, ng):
        H = wg.shape[1]
        g = _fm(wg, 8).reshape(128, 8, ng, 256).transpose(2, 0, 1, 3).reshape(ng, 128, 2048)
        u = _fm(wu, 8).reshape(128, 8, ng, 256).transpose(2, 0, 1, 3).reshape(ng, 128, 2048)
        d = wd.reshape(ng, 2, 128, 1024).transpose(0, 2, 1, 3).reshape(ng, 128, 2048)
        return np.ascontiguousarray(np.concatenate([g, u, d], axis=2))

    sh["ffnw"] = pack_ffn(f(ffn_w_gate[0]), f(ffn_w_up[0]), f(ffn_w_down[0]), NFFN_G)
    sh["moew"] = np.concatenate([pack_ffn(f(moe_w_gate[0, e]), f(moe_w_up[0, e]), f(moe_w_down[0, e]), NMOE_G)
                                 for e in range(8)], axis=0)
    sh["wr_in"] = _fm(f(moe_w_router[0]), 8)
    sh["br_in"] = f(moe_b_router).reshape(1, 8)
    sh["pleg"] = np.stack([_fm(f(ple_w_gate[l]), 8) for l in range(2)])
    sh["plep"] = np.stack([_fm(f(ple_w_proj[l]), 2) for l in range(2)])
    cst, cstb, koh = _constants()
    sh["cstb_in"] = cstb
    sh["koh_in"] = koh
    sh["ck_in"] = cache_k.reshape(2 * NPOOL * NCHK, PCH * 256)
    sh["cv_in"] = cache_v.reshape(2 * NPOOL * NCHK, PCH * 256)
    scb = f(state_conv_b)
    scd = f(state_conv_d)
    pt = np.asarray(page_table).astype(np.int32)

    in_maps = []
    for c in range(8):
        b, r = c // 4, c % 4
        m = dict(sh)
        xs = np.concatenate([x_prompt[b, r * NPR:(r + 1) * NPR], x_sample[4 * c:4 * c + 4, 0]], axis=0)
        m["x_in"] = _fm(np.ascontiguousarray(xs.T), 8)
        ps_ = [np.concatenate([p_prompt[l, b, r * NPR:(r + 1) * NPR], p_sample[l, 4 * c:4 * c + 4, 0]], axis=0)
               for l in range(2)]
        m["p_in"] = np.stack([_fm(np.ascontiguousarray(p.T), 2) for p in ps_])
        m["scb_in"] = np.ascontiguousarray(scb[:, 4 * c:4 * c + 4].reshape(2, NS, 2, 2, 128).transpose(0, 4, 3, 1, 2))
        m["scd_in"] = np.ascontiguousarray(scd[:, 4 * c:4 * c + 4].reshape(2, NS, 30, 2, 128).transpose(0, 4, 3, 1, 2))
        m["pt_in"] = np.ascontiguousarray(pt[4 * c:4 * c + 4].T)
        cc_ = cst.copy()
        for g in range(8):
            pb = np.full(40, -1e6, np.float32)
            pv = np.zeros(40, np.float32)
            po = np.zeros(40, np.float32)
            pb[0:8 * r] = 0.0
            pv[0:8 * r] = 1.0
            pb[32:32 + g] = 0.0
            pv[32:32 + g] = 1.0
            po[32 + g] = 1.0
            cc_[:, C_PB + g * 40:C_PB + (g + 1) * 40] = pb[None, :]
            cc_[:, C_PV + g * 40:C_PV + (g + 1) * 40] = pv[None, :]
            cc_[:, C_PO + g * 40:C_PO + (g + 1) * 40] = po[None, :]
        if r > 0:
            cc_[:, C_SELR + r - 1] = 1.0
        m["cst_in"] = cc_
        in_maps.append(m)

    res = run_bass_kernel_spmd(nc, in_maps, core_ids=list(range(8)))
    R = res.results
    if DEBUG:
        _CACHE['dbg'] = {k: np.asarray(v).astype(np.float32) for k, v in R[0].items() if k.startswith('d_')}

    y_prompt = np.zeros((2, 8192, 1024), np.float32)
    y_sample = np.zeros((32, 1, 1024), np.float32)
    k_prompt = np.zeros((2, 2, 8192, 4, 64), np.float32)
    v_prompt = np.zeros((2, 2, 8192, 4, 64), np.float32)
    k_sample = np.zeros((2, 32, 1, 4, 64), np.float32)
    v_sample = np.zeros((2, 32, 1, 4, 64), np.float32)
    conv_b_prompt = np.zeros((2, 2, 2, 256), np.float32)
    conv_b_sample = np.zeros((2, 32, 2, 256), np.float32)
    conv_d_prompt = np.zeros((2, 2, 30, 256), np.float32)
    conv_d_sample = np.zeros((2, 32, 30, 256), np.float32)
    chunk_v_sample = np.zeros((2, 32, 1, 256), np.float32)
    for c in range(8):
        b, r = c // 4, c % 4
        o = R[c]
        y = np.asarray(o["o_y"]).transpose(2, 1, 0).reshape(T, 1024)
        y_prompt[b, r * NPR:(r + 1) * NPR] = y[:NPR]
        y_sample[4 * c:4 * c + 4, 0] = y[NPR:]
        ok = np.asarray(o["o_k"]).transpose(0, 3, 2, 1)
        k_prompt[:, b, r * NPR:(r + 1) * NPR] = ok[:, :NPR]
        k_sample[:, 4 * c:4 * c + 4, 0] = ok[:, NPR:]
        ov = np.asarray(o["o_v"]).transpose(0, 3, 2, 1).reshape(2, T, 4, 64)
        v_prompt[:, b, r * NPR:(r + 1) * NPR] = ov[:, :NPR]
        v_sample[:, 4 * c:4 * c + 4, 0] = ov[:, NPR:]
        if r == 3:
            conv_b_prompt[:, b] = np.asarray(o["o_cb"]).transpose(0, 3, 2, 1).reshape(2, 2, 256)
            conv_d_prompt[:, b] = np.asarray(o["o_cd"]).transpose(0, 3, 2, 1).reshape(2, 30, 256)
        conv_b_sample[:, 4 * c:4 * c + 4] = np.asarray(o["o_cbs"]).transpose(0, 3, 4, 2, 1).reshape(2, NS, 2, 256)
        conv_d_sample[:, 4 * c:4 * c + 4] = np.asarray(o["o_cds"]).transpose(0, 3, 4, 2, 1).reshape(2, NS, 30, 256)
        chunk_v_sample[:, 4 * c:4 * c + 4, 0] = np.asarray(o["o_cv"]).transpose(0, 3, 2, 1).reshape(2, NS, 256)
    return (y_prompt, y_sample, k_prompt, v_prompt, k_sample, v_sample, conv_b_prompt, conv_b_sample,
            conv_d_prompt, conv_d_sample, chunk_v_sample)
```

```python
import numpy as np
import concourse.bass as bass
import concourse.mybir as mybir
from concourse.bass_utils import run_bass_kernel_spmd

F32 = mybir.dt.float32
BF16 = mybir.dt.bfloat16
I32 = mybir.dt.int32
AF = mybir.ActivationFunctionType
ALU = mybir.AluOpType
AX = mybir.AxisListType

NPR = 2048
NS = 4
T = NPR + NS
MT = [(i * 256, 256) for i in range(8)] + [(NPR, NS)]
FT = [(0, 512), (512, 512), (1024, 512), (1536, 512), (2048, 4)]
DEPTH = 2
ALPHA = (2 * DEPTH) ** 0.25
EPS = 1e-5
NEG = -30000.0
NPOOL = 5120
GROUPS = [[0, 1, 2, 3], [4, 5, 6, 7]]
NFFN_G = 11
NMOE_G = 14
PCH = 16
import os
DEBUG = bool(os.environ.get('KDBG'))
NCHK = 128 // PCH

SP_BGATE = 0
SP_LNG = 32
SP_LNB = 56
SP_ALNG = 80
SP_ALNB = 82
SP_DLNG = 84
SP_DLNB = 86
SP_DCB = 88
SP_BCW = 90
SP_DCW = 96
SP_AW00 = 158
SP_ABS0 = 162
NSP = 166
C_ID = 0
C_ONE = 128
C_TRI = 256
C_PAIR = 384
C_PAIRT = 448
C_SEL8 = 576
C_PB = 1600
C_PV = 1920
C_PO = 2240
C_SELR = 2560
NCST = 2564
CB_ID = 0
CB_CAUS = 128
NCSTB = 640


class Buf:
    __slots__ = ("w", "r", "name")

    def __init__(self, name=""):
        self.w = None
        self.r = {}
        self.name = name


class Op:
    __slots__ = ("eng", "fn", "deps", "kind", "sem", "val", "signal", "idx")


class Prog:
    def __init__(self):
        self.ops = []
        self.bufs = {}
        self.always = []

    def B(self, *key):
        b = self.bufs.get(key)
        if b is None:
            b = self.bufs[key] = Buf(str(key))
        return b

    def add(self, eng, fn, reads=(), writes=(), kind="c"):
        op = Op()
        op.eng, op.fn, op.kind = eng, fn, kind
        op.idx = len(self.ops)
        op.signal = False
        op.sem = None
        op.val = 0
        deps = {}
        reads = list(reads) + [b for b in self.always if b not in writes]
        for b in reads:
            if b.w is not None:
                deps[b.w.idx] = b.w
        for b in writes:
            for o in b.r.values():
                deps[o.idx] = o
            if b.w is not None:
                deps[b.w.idx] = b.w
        rkey = eng if kind == "c" else ("d", op.idx)
        for b in reads:
            b.r[rkey] = op
        for b in writes:
            b.w = op
            b.r = {}
        deps.pop(op.idx, None)
        dl = []
        for o in deps.values():
            if o.kind == "c" and kind == "c" and o.eng == "pe" and eng == "pe":
                continue
            dl.append(o)
        op.deps = dl
        self.ops.append(op)
        return op


def emit(nc, P):
    ops = P.ops
    for op in ops:
        for d in op.deps:
            d.signal = True
    NRING = 12
    csem = {e: nc.alloc_semaphore(name=f"c_{e}") for e in ("pe", "act", "dve", "pool")}
    rings = {q: [nc.alloc_semaphore(name=f"d_{q}{i}") for i in range(NRING)] for q in ("sp", "pool")}
    ccsem = nc.alloc_semaphore(name="ccs")
    ccount = {e: 0 for e in csem}
    rcount = {}
    rlast = {}
    rpos = {"sp": 0, "pool": 0}
    cccount = 0
    finals = {}
    for op in ops:
        if op.kind == "c":
            if op.signal:
                ccount[op.eng] += 1
                op.sem = csem[op.eng]
                op.val = ccount[op.eng]
        elif op.kind == "d":
            s = rings[op.eng][rpos[op.eng] % NRING]
            rpos[op.eng] += 1
            key = id(s)
            prev = rlast.get(key)
            if prev is not None:
                op.deps.append(prev)
            rcount[key] = rcount.get(key, 0) + 1
            rlast[key] = op
            op.sem = s
            op.val = 16 * rcount[key]
            finals[key] = (s, op.val)
        else:
            cccount += 1
            op.sem = ccsem
            op.val = cccount
            finals[id(ccsem)] = (ccsem, cccount)
    assert max(ccount.values()) < 60000, ccount
    queues = {"pe": [], "act": [], "dve": [], "pool": [], "sp": []}
    for op in ops:
        queues[op.eng].append(op)

    def run(eng_name, e):
        waited = {}
        for op in queues[eng_name]:
            need = {}
            for d in op.deps:
                k = id(d.sem)
                if d.val > need.get(k, (None, 0))[1]:
                    need[k] = (d.sem, d.val)
            for k, (s, v) in need.items():
                if waited.get(k, 0) >= v:
                    continue
                e.wait_ge(s, v)
                waited[k] = v
            ins = op.fn(e)
            if op.kind == "c":
                if op.signal:
                    ins.then_inc(op.sem, 1)
            elif op.kind == "d":
                ins.then_inc(op.sem, 16)
            else:
                ins.then_inc(op.sem, 1)
        if eng_name == "sp":
            for k, (s, v) in finals.items():
                e.wait_ge(s, v)

    with nc.Block() as block:
        @block.tensor
        def _(e):
            run("pe", e)

        @block.scalar
        def _(e):
            run("act", e)

        @block.vector
        def _(e):
            run("dve", e)

        @block.gpsimd
        def _(e):
            run("pool", e)

        @block.sync
        def _(e):
            run("sp", e)


def build_program():
    nc = bass.Bass("TRN2", target_bir_lowering=False)
    P = Prog()
    B = P.B

    def din(name, shape, dt=F32):
        return nc.dram_tensor(name, list(shape), dt, kind="ExternalInput").ap()

    def dout(name, shape, dt=F32):
        return nc.dram_tensor(name, list(shape), dt, kind="ExternalOutput").ap()

    def dint(name, shape, dt):
        return nc.dram_tensor(name, list(shape), dt, kind="Internal").ap()

    x_in = din("x_in", [128, 8, T])
    p_in = din("p_in", [2, 128, 2, T])
    scb_in = din("scb_in", [2, 128, 2, NS, 2])
    scd_in = din("scd_in", [2, 128, 2, NS, 30])
    pt_in = din("pt_in", [128, NS], I32)
    ck_in = din("ck_in", [2 * NPOOL * NCHK, PCH * 256])
    cv_in = din("cv_in", [2 * NPOOL * NCHK, PCH * 256])
    win = din("win", [2, 128, 8, 2560])
    wgate = din("wgate", [2, 128, 8, 4096])
    wbrA = din("wbrA", [2, 64, 4, 1024])
    wbrB = din("wbrB", [2, 128, 2, 1024])
    wbrC = din("wbrC", [2, 64, 4, 1024])
    wbrD = din("wbrD", [2, 128, 2, 1024])
    wout = din("wout", [2, 128, 8, 1024])
    awsT = din("awsT", [2, 128, 4, 128])
    absr = din("absr", [2, 1, 4, 128])
    sp_in = din("sp_in", [2, 128, NSP])
    ffnw = din("ffnw", [NFFN_G, 128, 6144])
    moew = din("moew", [8 * NMOE_G, 128, 6144])
    wr_in = din("wr_in", [128, 8, 8])
    br_in = din("br_in", [1, 8])
    pleg = din("pleg", [2, 128, 8, 1024])
    plep = din("plep", [2, 128, 2, 1024])
    cst_in = din("cst_in", [128, NCST])
    cstb_in = din("cstb_in", [128, NCSTB])
    koh_in = din("koh_in", [40, 10240])

    o_y = dout("o_y", [128, 8, T])
    o_k = dout("o_k", [2, 64, 4, T])
    o_v = dout("o_v", [2, 128, 2, T])
    o_cb = dout("o_cb", [2, 128, 2, 2])
    o_cd = dout("o_cd", [2, 128, 2, 30])
    o_cbs = dout("o_cbs", [2, 128, 2, NS, 2])
    o_cds = dout("o_cds", [2, 128, 2, NS, 30])
    o_cv = dout("o_cv", [2, 128, 2, NS])
    if DEBUG:
        d_ya = dout('d_ya', [64, 4, T], BF16)
        d_yb = dout('d_yb', [128, 2, T], BF16)
        d_yc = dout('d_yc', [64, 4, T], BF16)
        d_yd = dout('d_yd', [128, 2, T], BF16)
        d_mg = dout('d_mg', [128, 8, T], BF16)
        d_ln1 = dout('d_ln1', [128, 8, T])
        d_ln2 = dout('d_ln2', [128, 8, T])
        d_ln3 = dout('d_ln3', [128, 8, T])

    cc_kt = [dint(f"cc_kt{l}", [256, NPR], BF16) for l in range(2)]
    cc_ktg = [dint(f"cc_ktg{l}", [4 * 256, NPR], BF16) for l in range(2)]
    cc_v = [dint(f"cc_v{l}", [128, 4096], BF16) for l in range(2)]
    cc_vg = [dint(f"cc_vg{l}", [4 * 128, 4096], BF16) for l in range(2)]
    cc_t = [dint(f"cc_t{l}", [128, 64], F32) for l in range(2)]
    cc_tg = [dint(f"cc_tg{l}", [4 * 128, 64], F32) for l in range(2)]
    win_b = [dint(f"win_b{l}", [128, 8, 2560], BF16) for l in range(2)]
    wgate_b = [dint(f"wgate_b{l}", [128, 8, 4096], BF16) for l in range(2)]
    wbrA_b = [dint(f"wbrA_b{l}", [64, 4, 1024], BF16) for l in range(2)]
    wbrB_b = [dint(f"wbrB_b{l}", [128, 2, 1024], BF16) for l in range(2)]
    wbrC_b = [dint(f"wbrC_b{l}", [64, 4, 1024], BF16) for l in range(2)]
    wbrD_b = [dint(f"wbrD_b{l}", [128, 2, 1024], BF16) for l in range(2)]
    wout_b = [dint(f"wout_b{l}", [128, 8, 1024], BF16) for l in range(2)]
    xs1 = dint("xs1", [128, 8, T], F32)
    xs2 = dint("xs2", [128, 8, T], F32)

    ARENA_B = 206000
    arena = nc.alloc_sbuf_tensor("arena", [128, ARENA_B // 2], BF16)
    cur = [0]

    def carve(nbytes):
        off = (cur[0] + 63) // 64 * 64
        cur[0] = off + nbytes
        assert cur[0] <= ARENA_B, f"SBUF arena overflow {cur[0]}"
        return off

    def tile(shape, dt):
        esz = 4 if dt in (F32, I32) else 2
        n = 1
        for s in shape[1:]:
            n *= s
        off = carve(n * esz)
        v = arena[0:shape[0], off // 2: off // 2 + n * esz // 2]
        if dt != BF16:
            v = v.bitcast(dt)
        if len(shape) == 3:
            v = v.rearrange("p (a b) -> p a b", a=shape[1])
        elif len(shape) == 4:
            v = v.rearrange("p (a b c) -> p a b c", a=shape[1], b=shape[2])
        return v

    xb = tile([128, 8, T], BF16)
    WSLOT = 8192
    NW = 2
    wslots = [tile([128, WSLOT], BF16) for _ in range(NW)]
    cst = tile([128, NCST], F32)
    cstb = tile([128, NCSTB], BF16)
    spt = [tile([128, NSP], F32) for _ in range(2)]
    awsb = tile([128, 4, 128], BF16)
    awsf = tile([128, 4, 128], F32)
    absf = tile([1, 4, 128], F32)
    wrt = tile([128, 8, 8], F32)
    brt = tile([1, 8], F32)
    cbprev = tile([128, 2, 2], F32)
    cdprev = tile([128, 2, 30], F32)
    tails = tile([128, 64], F32)
    tailg = tile([128, 4, 64], F32)
    pti = tile([128, NS], I32)
    idx = tile([128, 2], I32)
    QTs = tile([64, 4, NS], F32)
    KTs = tile([64, 4, NS], F32)
    VTs = tile([64, 4, NS], F32)
    kmT = tile([64, 4, 40], BF16)
    ycs = tile([64, 4, NS], BF16)
    m8s = tile([4, 8], F32)
    fsc = tile([128, 4], F32)
    phase_base = cur[0]

    xt = tile([128, 8, 256], F32)
    ya = tile([64, 4, 256], BF16)
    yb = tile([128, 2, 256], BF16)
    yc = tile([64, 4, 256], BF16)
    yd = tile([128, 2, 256], BF16)
    mg32 = tile([128, 8, 256], F32)
    mgb = tile([128, 8, 256], BF16)
    QTa = tile([104, 4, 256], BF16)
    Kh = tile([104, 10240], BF16)
    Vh = tile([128, 80, 65], BF16)
    vtm = tile([128, 4, 16, 64], BF16)
    cbT = tile([128, 2, 2 + 256], F32)
    cdT = tile([128, 2, 30 + 256], F32)
    NTMP = 4
    mtmp = [tile([128, 256], F32) for _ in range(NTMP)]
    mlnm = tile([128, 256], F32)
    mlnr = tile([128, 256], F32)
    dacc = [tile([128, 256], F32) for _ in range(2)]
    gv = tile([128, 2, 256], F32)
    vn = tile([128, 2, 256], F32)
    vtA = tile([128, 256], BF16)
    ptile = [tile([128, 256], BF16) for _ in range(2)]
    accS = tile([65, 256], F32)
    small = tile([128, 4, 40], F32)
    small2 = tile([128, 4, 104], F32)
    m8 = tile([128, 8], F32)
    kTb = tile([64, 4, 256], BF16)
    cbs = tile([128, 2, NS, 3], F32)
    cds = tile([128, 2, NS, 31], F32)
    sm3 = tile([128, 2, NS, 31], F32)
    Kc = tile([128, PCH * 256], F32)
    Ssc = tile([128, 128, 4], F32)
    Psc = tile([128, 128, 4], F32)
    qbc = tile([128, 256], F32)
    pvp = tile([128, 256], F32)
    pvc = tile([128, 256], F32)
    sdg = tile([64, 64], F32)
    ssm = tile([128, 16], F32)
    mix_end = cur[0]

    cur[0] = phase_base
    xf = tile([128, 8, T], F32)
    cmbT = tile([8, T], F32)
    cmbbc = [tile([128, T], F32) for _ in range(1)]
    hb = [tile([128, 512], BF16) for _ in range(4)]
    sgt = [tile([128, 512], F32) for _ in range(3)]
    ftmp = [tile([128, 512], F32) for _ in range(NTMP)]
    flnm = tile([128, 512], F32)
    flnr = tile([128, 512], F32)
    pTb = tile([128, 2, T], BF16)
    wppt = tile([128, 2048], BF16)
    lg = tile([128, 8], F32)
    ex8 = tile([128, 8], F32)
    fm8 = tile([128, 8], F32)
    ffn_end = cur[0]
    cur[0] = max(mix_end, ffn_end)
    REG = B("region")
    P.always = [REG]

    psum = [nc.alloc_psum_tensor(f"ps{i}", [128, 512], F32) for i in range(8)]
    psb = [B("ps", i) for i in range(8)]
    psrr = [0]

    def nextps():
        i = psrr[0] % 6
        psrr[0] += 1
        return psum[i], psb[i]

    def MM(out, lhsT, rhs, start, stop, R, W):
        P.add("pe", lambda e, o=out, a=lhsT, b=rhs, s=start, t=stop: e.matmul(o, a, b, start=s, stop=t), R, W)

    def TR(out, in_, ident, R, W):
        P.add("pe", lambda e, o=out, a=in_, i=ident: e.transpose(o, a, i), R, W)

    def ACT(out, in_, func, R, W, bias=0.0, scale=1.0):
        P.add("act", lambda e, o=out, a=in_, f=func, b=bias, s=scale: e.activation(o, a, f, bias=b, scale=s), R, W)

    def TT(out, a, b, op, R, W, eng="dve"):
        P.add(eng, lambda e, o=out, x=a, y=b, p=op: e.tensor_tensor(o, x, y, p), R, W)

    def TS(out, a, s1, s2, op0, op1, R, W, eng="dve"):
        if s2 is None:
            P.add(eng, lambda e, o=out, x=a, q=s1, p=op0: e.tensor_scalar(o, x, q, None, p), R, W)
        else:
            P.add(eng, lambda e, o=out, x=a, q=s1, r=s2, p=op0, p1=op1: e.tensor_scalar(o, x, q, r, p, p1), R, W)

    def STT(out, a, sc, b, op0, op1, R, W, eng="dve"):
        P.add(eng, lambda e, o=out, x=a, s=sc, y=b, p=op0, q=op1: e.scalar_tensor_tensor(o, x, s, y, p, q), R, W)

    def CP(out, in_, R, W, eng="dve"):
        if eng == "act":
            P.add("act", lambda e, o=out, a=in_: e.activation(o, a, AF.Copy), R, W)
        else:
            P.add(eng, lambda e, o=out, a=in_: e.tensor_copy(o, a), R, W)

    def RED(out, in_, op, R, W, eng="dve"):
        P.add(eng, lambda e, o=out, a=in_, p=op: e.tensor_reduce(o, a, AX.X, p), R, W)

    def MAX8(out, in_, R, W):
        P.add("dve", lambda e, o=out, a=in_: e.max(o, a), R, W)

    def RCP(out, in_, R, W):
        P.add("dve", lambda e, o=out, a=in_: e.reciprocal(o, a), R, W)

    def DMA(q, out, in_, R, W):
        P.add(q, lambda e, o=out, a=in_: e.dma_start(out=o, in_=a), R, W, kind="d")

    def FENCE():
        P.add("dve", lambda e: e.tensor_copy(fsc[0:1, 3:4], fsc[0:1, 3:4]), [], [REG])

    def w3(ap2d, a):
        return ap2d.rearrange("p (a b) -> p a b", a=a)

    wrr = [0]
    wsb = [B("wslot", i) for i in range(NW)]

    def loadw(src, parts, n, a=None, R=()):
        i = wrr[0] % NW
        wrr[0] += 1
        dst = wslots[i][0:parts, 0:n]
        DMA("pool", w3(dst, a) if a else dst, src, list(R), [wsb[i]])
        return dst, wsb[i]

    cs = lambda off, n, p=128: cst[0:p, off:off + n]
    ident_f = cs(C_ID, 128)
    ones_f = cs(C_ONE, 128)
    CSTB = B("cst")
    ident_b = cstb[:, CB_ID:CB_ID + 128]

    class TmpPool:
        def __init__(self, tiles, lnm, lnr, name):
            self.tiles = tiles
            self.bufs = [B(name, i) for i in range(len(tiles))]
            self.lnm, self.lnr = lnm, lnr
            self.lnmb, self.lnrb = B(name, "lnm"), B(name, "lnr")
            self.i = 0

        def next(self):
            i = self.i % len(self.tiles)
            self.i += 1
            return self.tiles[i], self.bufs[i]

    MTP = TmpPool(mtmp, mlnm, mlnr, "mtmp")
    FTP = TmpPool(ftmp, flnm, flnr, "ftmp")

    XB = [B("xb", i) for i in range(9)]
    XF = [B("xf", i) for i in range(9)]

    def tok_bufs(lst, c0, n):
        if c0 >= NPR:
            return [lst[8]]
        return [lst[i] for i in range(c0 // 256, (c0 + n - 1) // 256 + 1)]

    DMA("sp", cst[:, :], cst_in, [], [CSTB])
    DMA("pool", cstb[:, :], cstb_in, [], [CSTB])
    for l in range(2):
        DMA("sp", spt[l][:, :], sp_in[l], [], [CSTB])
    DMA("sp", wrt[:, :, :], wr_in, [], [CSTB])
    DMA("sp", brt[:, :], br_in, [], [CSTB])
    DMA("sp", pti[:, :], pt_in, [], [CSTB])
    for ti, (c0, n) in enumerate(MT):
        DMA("pool", xb[:, :, c0:c0 + n], x_in[:, :, c0:c0 + n], [], [XB[ti]])
    WSC = [B("wsc", l) for l in range(2)]
    for l in range(2):
        for src_, dst_ in ((win, win_b), (wgate, wgate_b), (wbrA, wbrA_b), (wbrB, wbrB_b), (wbrC, wbrC_b),
                           (wbrD, wbrD_b), (wout, wout_b)):
            DMA("pool", dst_[l], src_[l], [], [WSC[l]])
    KH = [B("Kh", 0), B("Kh", 1)]
    VH = [B("Vh", 0), B("Vh", 1)]

    def layernorm(tp, srcs, n, nfeat, gcols, bcols, outs32, outsb, silu=False):
        nch = len(srcs)
        ps1, pb1 = nextps()
        ps2, pb2 = nextps()
        for i, (s, sb) in enumerate(srcs):
            MM(ps1[:, 0:n], ones_f, s, i == 0, i == nch - 1, sb + [CSTB], [pb1])
        for i, (s, sb) in enumerate(srcs):
            t, tb = tp.next()
            ACT(t[:, 0:n], s, AF.Square, sb, [tb])
            MM(ps2[:, 0:n], ones_f, t[:, 0:n], i == 0, i == nch - 1, [tb, CSTB], [pb2])
        mean, mb, rstd, rb = tp.lnm, tp.lnmb, tp.lnr, tp.lnrb
        ACT(mean[:, 0:n], ps1[:, 0:n], AF.Copy, [pb1], [mb], scale=1.0 / nfeat)
        t, tb = tp.next()
        TT(t[:, 0:n], mean[:, 0:n], mean[:, 0:n], ALU.mult, [mb], [tb])
        STT(rstd[:, 0:n], ps2[:, 0:n], 1.0 / nfeat, t[:, 0:n], ALU.mult, ALU.subtract, [pb2, tb], [rb])
        TS(rstd[:, 0:n], rstd[:, 0:n], EPS, None, ALU.add, None, [rb], [rb])
        RCP(rstd[:, 0:n], rstd[:, 0:n], [rb], [rb])
        ACT(rstd[:, 0:n], rstd[:, 0:n], AF.Sqrt, [rb], [rb])
        for i, (s, sb) in enumerate(srcs):
            t, tb = tp.next()
            TT(t[:, 0:n], s, mean[:, 0:n], ALU.subtract, sb + [mb], [tb])
            TT(t[:, 0:n], t[:, 0:n], rstd[:, 0:n], ALU.mult, [tb, rb], [tb])
            if silu:
                ACT(t[:, 0:n], t[:, 0:n], AF.Identity, [tb, CSTB], [tb], bias=bcols[i], scale=gcols[i])
                t2, tb2 = tp.next()
                ACT(t2[:, 0:n], t[:, 0:n], AF.Sigmoid, [tb], [tb2])
                ob, obb = outsb[i]
                TT(ob, t[:, 0:n], t2[:, 0:n], ALU.mult, [tb, tb2], obb)
            else:
                o32, ob32 = outs32[i]
                ACT(o32, t[:, 0:n], AF.Identity, [tb, CSTB], ob32, bias=bcols[i], scale=gcols[i])
                if outsb is not None:
                    ob, obb = outsb[i]
                    CP(ob, o32, ob32, obb)

    def gelu_from(ps_ap, pbuf, out_ap, obufs, n, parts=128):
        x, xbf = MTP.next()
        t, tb = MTP.next()
        ACT(x[0:parts, 0:n], ps_ap, AF.Copy, [pbuf], [xbf])
        TT(t[0:parts, 0:n], x[0:parts, 0:n], x[0:parts, 0:n], ALU.mult, [xbf], [tb])
        TS(t[0:parts, 0:n], t[0:parts, 0:n], 0.044715, 1.0, ALU.mult, ALU.add, [tb], [tb])
        TT(t[0:parts, 0:n], t[0:parts, 0:n], x[0:parts, 0:n], ALU.mult, [tb, xbf], [tb])
        ACT(t[0:parts, 0:n], t[0:parts, 0:n], AF.Sigmoid, [tb], [tb], scale=1.5957691216057308)
        TT(out_ap, x[0:parts, 0:n], t[0:parts, 0:n], ALU.mult, [xbf, tb], obufs)

    def proj(ps_ap, pbuf, wv, wbuf, kcs, cols, c0, n):
        xbufs = tok_bufs(XB, c0, n)
        for kc in range(kcs):
            MM(ps_ap, wv[:, kc, cols[0]:cols[1]], xb[:, kc, c0:c0 + n], kc == 0, kc == kcs - 1,
               [wbuf] + xbufs, [pbuf])

    YA, YB, YC, YD = B("ya"), B("yb"), B("yc"), B("yd")
    MG32, MGB = B("mg32"), B("mgb")
    QTA = B("QTa")
    VTM = B("vtm")
    CBT, CDT = B("cbT"), B("cdT")
    CBP, CDP = B("cbprev"), B("cdprev")
    GV, VN = B("gv"), B("vn")
    SQ = B("sqkv")
    KMT = B("kmT")
    OUTB = B("outs")
    XT = B("xt")
    AWS = B("aws")

    def sample_attention(l):
        KC, SS, PS_, QBC, PVP, PVC, SDG, SSM, IDX = (B("Kc"), B("Ssc"), B("Psc"), B("qbc"), B("pvp"), B("pvc"),
                                                     B("sdg"), B("ssm"), B("idx"))
        for s_ in range(NS):
            psq, pbq = nextps()
            for h in range(4):
                TS(sdg[:, :], ident_f[0:64, 0:64], QTs[:, h, s_:s_ + 1], None, ALU.mult, None, [CSTB, SQ], [SDG])
                MM(psq[:, h * 64:(h + 1) * 64], ones_f[0:64, :], sdg[:, :], True, True, [CSTB, SDG], [pbq])
            CP(qbc[:, :], psq[:, 0:256], [pbq], [QBC])
            for ck in range(NCHK):
                TS(idx[:, 0:1], pti[:, s_:s_ + 1], NCHK, l * NPOOL * NCHK + ck, ALU.mult, ALU.add, [CSTB], [IDX])
                P.add("pool", lambda e: e.indirect_dma_start(
                    out=Kc[:, :], out_offset=None, in_=ck_in,
                    in_offset=bass.IndirectOffsetOnAxis(ap=idx[:, 0:1], axis=0)), [IDX], [KC], kind="d")
                kv = Kc[:, :].rearrange("p (a b) -> p a b", b=256)
                TT(kv, kv, qbc[:, :].rearrange("p (o b) -> p o b", o=1).to_broadcast([128, PCH, 256]), ALU.mult,
                   [KC, QBC], [KC])
                RED(Ssc[:, ck * PCH:(ck + 1) * PCH, :], Kc[:, :].rearrange("p (a h d) -> p a h d", h=4, d=64), ALU.add,
                    [KC], [SS])
                yield
            RED(ssm[:, 0:4], Ssc[:, :, :].rearrange("p a h -> p h a"), ALU.add, [SS], [SSM])
            psg, pbg = nextps()
            MM(psg[0:4, 0:64], ssm[:, 0:4], cs(C_PAIR, 64), True, True, [SSM, CSTB], [pbg])
            t, tb = MTP.next()
            CP(t[0:4, 0:64], psg[0:4, 0:64], [pbg], [tb])
            MAX8(m8s[0:4, :], t[0:4, 0:64], [tb], [B("m8s")])
            TS(t[0:4, 0:64], t[0:4, 0:64], m8s[0:4, 2:3], None, ALU.is_ge, None, [tb, B("m8s")], [tb])
            pst, pbt = nextps()
            TR(pst[0:64, 0:4], t[0:4, 0:64], ident_f[0:4, 0:4], [tb, CSTB], [pbt])
            t2, tb2 = MTP.next()
            CP(t2[0:64, 0:4], pst[0:64, 0:4], [pbt], [tb2])
            psm, pbm = nextps()
            MM(psm[:, 0:4], cs(C_PAIRT, 128, 64), t2[0:64, 0:4], True, True, [CSTB, tb2], [pbm])
            TS(ssm[:, 4:8], psm[:, 0:4], -1.0, -NEG, ALU.add, ALU.mult, [pbm], [SSM])
            for h in range(4):
                ACT(Psc[:, :, h], Ssc[:, :, h], AF.Exp, [SS, SSM], [PS_], bias=ssm[:, 4 + h:5 + h])
            RED(ssm[:, 8:12], Psc[:, :, :].rearrange("p a h -> p h a"), ALU.add, [PS_], [SSM])
            for ck in range(NCHK):
                TS(idx[:, 1:2], pti[:, s_:s_ + 1], NCHK, l * NPOOL * NCHK + ck, ALU.mult, ALU.add, [CSTB], [IDX])
                P.add("pool", lambda e: e.indirect_dma_start(
                    out=Kc[:, :], out_offset=None, in_=cv_in,
                    in_offset=bass.IndirectOffsetOnAxis(ap=idx[:, 1:2], axis=0)), [IDX], [KC], kind="d")
                kv4 = Kc[:, :].rearrange("p (a h d) -> p a h d", h=4, d=64)
                for h in range(4):
                    TT(kv4[:, :, h, :], kv4[:, :, h, :],
                       Psc[:, ck * PCH:(ck + 1) * PCH, h:h + 1].to_broadcast([128, PCH, 64]), ALU.mult, [KC, PS_], [KC])
                dst, dstb = (pvp, PVP) if ck == 0 else (pvc, PVC)
                RED(dst[:, :], Kc[:, :].rearrange("p (a c) -> p c a", c=256), ALU.add, [KC], [dstb])
                if ck > 0:
                    TT(pvp[:, :], pvp[:, :], pvc[:, :], ALU.add, [PVP, PVC], [PVP])
                yield
            psn, pbn = nextps()
            for h in range(4):
                MM(psn[0:64, h:h + 1], pvp[:, h * 64:(h + 1) * 64], ones_f[:, 0:1], True, True, [PVP, CSTB], [pbn])
            psd, pbd = nextps()
            MM(psd[0:64, 0:4], ones_f[:, 0:64], ssm[:, 8:12], True, True, [CSTB, SSM], [pbd])
            t, tb = MTP.next()
            TT(t[0:64, 0:4], QTs[:, :, s_], KTs[:, :, s_], ALU.mult, [SQ], [tb])
            pss, pbs = nextps()
            MM(pss[0:64, 0:4], ones_f[0:64, 0:64], t[0:64, 0:4], True, True, [CSTB, tb], [pbs])
            t2, tb2 = MTP.next()
            ACT(t2[0:64, 0:4], pss[0:64, 0:4], AF.Exp, [pbs], [tb2])
            t3, tb3 = MTP.next()
            TT(t3[0:64, 0:4], t2[0:64, 0:4], VTs[:, :, s_], ALU.mult, [tb2, SQ], [tb3])
            TT(t3[0:64, 0:4], t3[0:64, 0:4], psn[0:64, 0:4], ALU.add, [tb3, pbn], [tb3])
            TT(t2[0:64, 0:4], t2[0:64, 0:4], psd[0:64, 0:4], ALU.add, [tb2, pbd], [tb2])
            RCP(t2[0:64, 0:4], t2[0:64, 0:4], [tb2], [tb2])
            TT(ycs[:, :, s_], t3[0:64, 0:4], t2[0:64, 0:4], ALU.mult, [tb3, tb2], [B("ycs")])
            yield

    def layernorm_x(l, i, c0, n):
        bufs = tok_bufs(XF, c0, n)
        bufsb = tok_bufs(XB, c0, n)
        layernorm(FTP, [(xf[:, m, c0:c0 + n], bufs) for m in range(8)], n, 1024.0,
                  [spt[l][:, SP_LNG + i * 8 + m:SP_LNG + i * 8 + m + 1] for m in range(8)],
                  [spt[l][:, SP_LNB + i * 8 + m:SP_LNB + i * 8 + m + 1] for m in range(8)],
                  [(xf[:, m, c0:c0 + n], bufs) for m in range(8)],
                  [(xb[:, m, c0:c0 + n], bufsb) for m in range(8)])

    for l in range(DEPTH):
        sp = lambda off, n=1, p=128, l=l: spt[l][0:p, off:off + n]
        xsrc = x_in if l == 0 else xs2
        XSRC = [B("xsrc", l, i) for i in range(9)] if l == 0 else [B("xs2", i) for i in range(9)]
        XS1 = [B("xs1", i) for i in range(9)]
        XS2 = [B("xs2", i) for i in range(9)]
        CCB = B("cc", l)
        CG = B("ccg", l)
        DMA("pool", Kh[64:104, :], koh_in, [], KH)
        P.add("dve", lambda e: e.memset(Vh[:, :, 64:65], 1.0), [], VH)
        wC, wCb = loadw(win_b[l][:, :, 1280:2048], 128, 8 * 768, a=8, R=[WSC[l]])
        wCv = w3(wC, 8)
        for ti, (c0, n) in enumerate(MT):
            smp = ti == 8
            for h in range(4):
                ps, pb = nextps()
                proj(ps[0:64, 0:n], pb, wCv, wCb, 8, (256 + h * 64, 256 + h * 64 + 64), c0, n)
                t, tb = MTP.next()
                ACT(t[0:64, 0:n], ps[0:64, 0:n], AF.Copy, [pb], [tb])
                DMA("sp", o_k[l][:, h, c0:c0 + n], t[0:64, 0:n], [tb], [OUTB])
                if smp:
                    CP(KTs[:, h, :], t[0:64, 0:n], [tb], [SQ])
                else:
                    CP(kTb[:, h, 0:n], t[0:64, 0:n], [tb], [B("kTb")])
            if not smp:
                for h in range(4):
                    DMA("sp", cc_kt[l][h * 64:(h + 1) * 64, c0:c0 + n], kTb[:, h, 0:n], [B("kTb")], [CCB])
            for vc in range(2):
                ps, pb = nextps()
                proj(ps[:, 0:n], pb, wCv, wCb, 8, (512 + vc * 128, 512 + vc * 128 + 128), c0, n)
                t, tb = MTP.next()
                ACT(t[:, 0:n], ps[:, 0:n], AF.Copy, [pb], [tb])
                DMA("sp", o_v[l][:, vc, c0:c0 + n], t[:, 0:n], [tb], [OUTB])
                if not smp:
                    for c in range(2):
                        pt_, ptb = nextps()
                        TR(pt_[:, 0:128], t[:, c * 128:(c + 1) * 128], ident_f, [tb, CSTB], [ptb])
                        kt = ti * 2 + c
                        CP(vtm[:, 2 * vc:2 * vc + 2, kt, :],
                           pt_[:, 0:128].rearrange("p (a b) -> p a b", a=2), [ptb], [VTM],
                           eng="act" if c % 2 else "dve")
            if smp:
                for h in range(4):
                    ps, pb = nextps()
                    proj(ps[0:64, 0:n], pb, wCv, wCb, 8, (h * 64, h * 64 + 64), c0, n)
                    ACT(QTs[:, h, :], ps[0:64, 0:n], AF.Copy, [pb], [SQ], scale=0.125)
                    ps, pb = nextps()
                    proj(ps[0:64, 0:n], pb, wCv, wCb, 8, (512 + h * 64, 512 + h * 64 + 64), c0, n)
                    ACT(VTs[:, h, :], ps[0:64, 0:n], AF.Copy, [pb], [SQ])
        DMA("sp", cc_v[l], vtm.rearrange("p a b c -> p (a b c)"), [VTM], [CCB])
        wBt, wBtb = loadw(win_b[l][:, :, 768:1280], 128, 8 * 512, a=8, R=[WSC[l]])
        wBtv = w3(wBt, 8)
        wDt, wDtb = loadw(win_b[l][:, :, 2048:2560], 128, 8 * 512, a=8, R=[WSC[l]])
        wDtv = w3(wDt, 8)
        c0t, nt = NPR - 32, 32
        TL = B("tails")
        for cc in range(2):
            psc, pbc = nextps()
            proj(psc[:, 0:nt], pbc, wBtv, wBtb, 8, (cc * 128, cc * 128 + 128), c0t, nt)
            psx, pbx = nextps()
            proj(psx[:, 0:nt], pbx, wBtv, wBtb, 8, (256 + cc * 128, 256 + cc * 128 + 128), c0t, nt)
            t, tb = MTP.next()
            ACT(t[:, 0:nt], psc[:, 0:nt], AF.Copy, [pbc], [tb])
            TT(tails[:, cc * 32:cc * 32 + 2], t[:, 30:32], psx[:, 30:32], ALU.mult, [tb, pbx], [TL])
            psa, pba = nextps()
            proj(psa[:, 0:nt], pba, wDtv, wDtb, 8, (cc * 128, cc * 128 + 128), c0t, nt)
            psg, pbg = nextps()
            proj(psg[:, 0:nt], pbg, wDtv, wDtb, 8, (256 + cc * 128, 256 + cc * 128 + 128), c0t, nt)
            t, tb = MTP.next()
            ACT(t[:, 0:nt], psg[:, 0:nt], AF.Sigmoid, [pbg], [tb])
            TT(tails[:, cc * 32 + 2:cc * 32 + 32], t[:, 2:32], psa[:, 2:32], ALU.mult, [tb, pba], [TL])
        DMA("sp", cc_t[l], tails[:, :], [TL], [CCB])
        for src, dst in ((cc_kt[l], cc_ktg[l]), (cc_v[l], cc_vg[l]), (cc_t[l], cc_tg[l])):
            P.add("pool", lambda e, s=src, d=dst: e.collective_compute(
                "AllGather", ALU.bypass, replica_groups=GROUPS, ins=[s], outs=[d]), [CCB], [CG], kind="cc")
        TG = B("tailg")
        DMA("sp", tailg[:, :, :], cc_tg[l].rearrange("(r p) c -> p r c", p=128), [CG], [TG])
        for rk in range(4):
            selc = cst[:, C_SELR + rk:C_SELR + rk + 1]
            for cc in range(2):
                if rk == 0:
                    TS(cbprev[:, cc, :], tailg[:, rk, cc * 32:cc * 32 + 2], selc, None, ALU.mult, None,
                       [TG, CSTB], [CBP])
                    TS(cdprev[:, cc, :], tailg[:, rk, cc * 32 + 2:cc * 32 + 32], selc, None, ALU.mult, None,
                       [TG, CSTB], [CDP])
                else:
                    STT(cbprev[:, cc, :], tailg[:, rk, cc * 32:cc * 32 + 2], selc, cbprev[:, cc, :],
                        ALU.mult, ALU.add, [TG, CSTB, CBP], [CBP])
                    STT(cdprev[:, cc, :], tailg[:, rk, cc * 32 + 2:cc * 32 + 32], selc, cdprev[:, cc, :],
                        ALU.mult, ALU.add, [TG, CSTB, CDP], [CDP])

        def load_KV(h, half, l=l, CG=CG, CCB=CCB):
            ktg = cc_ktg[l].rearrange("(r hh d) t -> d r hh t", r=4, hh=4)
            r0 = 2 * half
            DMA("sp", Kh[0:64, r0 * 2048:(r0 + 2) * 2048].rearrange("p (r t) -> p r t", r=2),
                ktg[:, r0:r0 + 2, h, :], [CG], [KH[half]])
            for r_ in (r0, r0 + 1):
                DMA("sp", Vh[:, r_ * 16:(r_ + 1) * 16, 0:64],
                    cc_vg[l][r_ * 128:(r_ + 1) * 128, :].rearrange("p (hh k d) -> p hh k d", hh=4, k=16)[:, h, :, :],
                    [CG], [VH[half]])
            if half == 1:
                DMA("sp", Kh[0:64, 8192:10240], cc_kt[l][h * 64:(h + 1) * 64, :], [CCB], [KH[1]])
                DMA("sp", Vh[:, 64:80, 0:64],
                    cc_v[l].rearrange("p (hh k d) -> p hh k d", hh=4, k=16)[:, h, :, :], [CCB], [VH[1]])

        for h in range(4):
            load_KV(h, 0)
            load_KV(h, 1)
            t, tb = MTP.next()
            RED(t[0:64, 0:40], Kh[0:64, :].rearrange("p (b k) -> p b k", k=256), ALU.add, KH, [tb])
            CP(kmT[:, h, :], t[0:64, 0:40], [tb], [KMT])
        sa_gen = sample_attention(l)
        DMA("sp", awsf[:, :, :], awsT[l], [], [AWS])
        DMA("sp", absf[:, :, :], absr[l], [], [AWS])
        for h in range(4):
            TT(awsb[:, h, :], awsf[:, h, :], cs(C_TRI, 128), ALU.mult, [AWS, CSTB], [B("awsb")])

        for ti, (c0, n) in enumerate(MT):
            smp = ti == 8
            DMA("sp", xt[:, :, 0:n], xsrc[:, :, c0:c0 + n], [XSRC[ti]], [XT])
            wA, wAb = loadw(win_b[l][:, :, 0:512], 128, 8 * 512, a=8, R=[WSC[l]])
            wAv = w3(wA, 8)
            for h in range(4):
                ps, pb = nextps()
                proj(ps[0:64, 0:n], pb, wAv, wAb, 8, (h * 64, h * 64 + 64), c0, n)
                gelu_from(ps[0:64, 0:n], pb, ya[:, h, 0:n], [YA], n, parts=64)
            for vc in range(2):
                ps, pb = nextps()
                proj(ps[:, 0:n], pb, wAv, wAb, 8, (256 + vc * 128, 256 + vc * 128 + 128), c0, n)
                gelu_from(ps[:, 0:n], pb, gv[:, vc, 0:n], [GV], n)
            layernorm(MTP, [(gv[:, vc, 0:n], [GV]) for vc in range(2)], n, 256.0,
                      [sp(SP_ALNG + vc) for vc in range(2)], [sp(SP_ALNB + vc) for vc in range(2)],
                      [(vn[:, vc, 0:n], [VN]) for vc in range(2)], None)
            if not smp:
                for c in range(2):
                    for vc in range(2):
                        pt_, ptb = nextps()
                        TR(pt_[:, 0:128], vn[:, vc, c * 128:(c + 1) * 128], ident_f, [VN, CSTB], [ptb])
                        CP(vtA[:, vc * 128:(vc + 1) * 128], pt_[:, 0:128], [ptb], [B("vtA")],
                           eng="act" if vc else "dve")
                    for h in range(4):
                        ps, pb = nextps()
                        MM(ps[0:64, 0:128], vtA[:, h * 64:(h + 1) * 64], awsb[:, h, :], True, False,
                           [B("vtA"), B("awsb")], [pb])
                        MM(ps[0:64, 0:128], ones_f[0:1, 0:64], absf[0:1, h, :], False, True,
                           [CSTB, AWS], [pb])
                        TT(ya[:, h, c * 128:(c + 1) * 128], ya[:, h, c * 128:(c + 1) * 128], ps[0:64, 0:128],
                           ALU.mult, [YA, pb], [YA])
            else:
                DMA("sp", o_cv[l], vn[:, :, 0:NS], [VN], [OUTB])
                for h in range(4):
                    ps, pb = nextps()
                    MM(ps[0:64, 0:n], ident_f[:, (h % 2) * 64:(h % 2) * 64 + 64], vn[:, h // 2, 0:n], True, True,
                       [VN, CSTB], [pb])
                    t, tb = MTP.next()
                    TS(t[0:64, 0:n], ps[0:64, 0:n], sp(SP_AW00 + h, 1, 64), sp(SP_ABS0 + h, 1, 64),
                       ALU.mult, ALU.add, [pb, CSTB], [tb])
                    TT(ya[:, h, 0:n], ya[:, h, 0:n], t[0:64, 0:n], ALU.mult, [YA, tb], [YA])
            wB, wBb = loadw(win_b[l][:, :, 512:1280], 128, 8 * 768, a=8, R=[WSC[l]])
            wBv = w3(wB, 8)
            CBS, CDS, SM3 = B("cbs"), B("cds"), B("sm3")
            if smp:
                DMA("sp", cbs[:, :, :, 0:2], scb_in[l], [], [CBS])
            for cc in range(2):
                psc, pbc = nextps()
                proj(psc[:, 0:n], pbc, wBv, wBb, 8, (256 + cc * 128, 256 + cc * 128 + 128), c0, n)
                psx, pbx = nextps()
                proj(psx[:, 0:n], pbx, wBv, wBb, 8, (512 + cc * 128, 512 + cc * 128 + 128), c0, n)
                psb_, pbb = nextps()
                proj(psb_[:, 0:n], pbb, wBv, wBb, 8, (cc * 128, cc * 128 + 128), c0, n)
                t, tb = MTP.next()
                ACT(t[:, 0:n], psc[:, 0:n], AF.Copy, [pbc], [tb])
                a, ab = MTP.next()
                if not smp:
                    CP(cbT[:, cc, 0:2], cbprev[:, cc, :], [CBP], [CBT])
                    TT(cbT[:, cc, 2:2 + n], t[:, 0:n], psx[:, 0:n], ALU.mult, [tb, pbx], [CBT])
                    TS(a[:, 0:n], cbT[:, cc, 0:n], sp(SP_BCW + cc * 3), None, ALU.mult, None, [CBT, CSTB], [ab])
                    for k in (1, 2):
                        STT(a[:, 0:n], cbT[:, cc, k:k + n], sp(SP_BCW + cc * 3 + k), a[:, 0:n], ALU.mult, ALU.add,
                            [CBT, CSTB, ab], [ab])
                    TT(yb[:, cc, 0:n], a[:, 0:n], psb_[:, 0:n], ALU.mult, [ab, pbb], [YB])
                    CP(cbprev[:, cc, :], cbT[:, cc, n:n + 2], [CBT], [CBP])
                else:
                    TT(cbs[:, cc, :, 2], t[:, 0:n], psx[:, 0:n], ALU.mult, [tb, pbx], [CBS])
                    wv_ = spt[l][:, SP_BCW + cc * 3:SP_BCW + cc * 3 + 3]
                    TT(sm3[:, cc, :, 0:3], cbs[:, cc, :, :],
                       wv_.rearrange("p (o k) -> p o k", o=1).to_broadcast([128, NS, 3]),
                       ALU.mult, [CBS, CSTB], [SM3])
                    RED(a[:, 0:n], sm3[:, cc, :, 0:3], ALU.add, [SM3], [ab])
                    TT(yb[:, cc, 0:n], a[:, 0:n], psb_[:, 0:n], ALU.mult, [ab, pbb], [YB])
            if ti == 7:
                DMA("sp", o_cb[l], cbprev[:, :, :], [CBP], [OUTB])
            if smp:
                DMA("sp", o_cbs[l], cbs[:, :, :, 1:3], [CBS], [OUTB])
            wD, wDb = loadw(win_b[l][:, :, 2048:2560], 128, 8 * 512, a=8, R=[WSC[l]])
            wDv = w3(wD, 8)
            if smp:
                DMA("sp", cds[:, :, :, 0:30], scd_in[l], [], [CDS])
            dsrc = []
            for cc in range(2):
                psa, pba = nextps()
                proj(psa[:, 0:n], pba, wDv, wDb, 8, (cc * 128, cc * 128 + 128), c0, n)
                psg, pbg = nextps()
                proj(psg[:, 0:n], pbg, wDv, wDb, 8, (256 + cc * 128, 256 + cc * 128 + 128), c0, n)
                t, tb = MTP.next()
                ACT(t[:, 0:n], psg[:, 0:n], AF.Sigmoid, [pbg], [tb])
                a, ab = dacc[cc], B("dacc", cc)
                if not smp:
                    CP(cdT[:, cc, 0:30], cdprev[:, cc, :], [CDP], [CDT])
                    TT(cdT[:, cc, 30:30 + n], t[:, 0:n], psa[:, 0:n], ALU.mult, [tb, pba], [CDT])
                    TS(a[:, 0:n], cdT[:, cc, 0:n], sp(SP_DCW + cc * 31), sp(SP_DCB + cc), ALU.mult, ALU.add,
                       [CDT, CSTB], [ab])
                    for k in range(1, 31):
                        STT(a[:, 0:n], cdT[:, cc, k:k + n], sp(SP_DCW + cc * 31 + k), a[:, 0:n], ALU.mult, ALU.add,
                            [CDT, CSTB, ab], [ab])
                    CP(cdprev[:, cc, :], cdT[:, cc, n:n + 30], [CDT], [CDP])
                else:
                    TT(cds[:, cc, :, 30], t[:, 0:n], psa[:, 0:n], ALU.mult, [tb, pba], [CDS])
                    wv_ = spt[l][:, SP_DCW + cc * 31:SP_DCW + cc * 31 + 31]
                    TT(sm3[:, cc, :, :], cds[:, cc, :, :],
                       wv_.rearrange("p (o k) -> p o k", o=1).to_broadcast([128, NS, 31]),
                       ALU.mult, [CDS, CSTB], [SM3])
                    RED(a[:, 0:n], sm3[:, cc, :, :], ALU.add, [SM3], [ab])
                    TS(a[:, 0:n], a[:, 0:n], sp(SP_DCB + cc), None, ALU.add, None, [ab, CSTB], [ab])
                dsrc.append((a[:, 0:n], [ab]))
            layernorm(MTP, dsrc, n, 256.0, [sp(SP_DLNG + cc) for cc in range(2)],
                      [sp(SP_DLNB + cc) for cc in range(2)],
                      None, [(yd[:, cc, 0:n], [YD]) for cc in range(2)], silu=True)
            if ti == 7:
                DMA("sp", o_cd[l], cdprev[:, :, :], [CDP], [OUTB])
            if smp:
                DMA("sp", o_cds[l], cds[:, :, :, 1:31], [CDS], [OUTB])
            if not smp:
                for _ in range(9):
                    next(sa_gen, None)
                g = ti
                wq, wqb = loadw(win_b[l][:, :, 1280:1536], 128, 8 * 256, a=8, R=[WSC[l]])
                wqv = w3(wq, 8)
                for h in range(4):
                    ps, pb = nextps()
                    proj(ps[0:64, 0:n], pb, wqv, wqb, 8, (h * 64, h * 64 + 64), c0, n)
                    ACT(QTa[0:64, h, 0:n], ps[0:64, 0:n], AF.Copy, [pb], [QTA], scale=0.125)
                SM, SM2, M8 = B("small"), B("small2"), B("m8")
                PBv = cst[:, C_PB + g * 40:C_PB + g * 40 + 40]
                PVv = cst[:, C_PV + g * 40:C_PV + g * 40 + 40]
                POv = cst[:, C_PO + g * 40:C_PO + g * 40 + 40]
                bc3 = lambda v: v.rearrange("p (o b) -> p o b", o=1).to_broadcast([128, 4, 40])
                for c in range(2):
                    psg, pbg = nextps()
                    for h in range(4):
                        MM(psg[:, h * 40:h * 40 + 40], QTa[0:64, h, c * 128:(c + 1) * 128], kmT[:, h, :], True, True,
                           [QTA, KMT], [pbg])
                    TT(small[:, :, :], psg[:, 0:160].rearrange("p (h b) -> p h b", h=4), bc3(PBv), ALU.add,
                       [pbg, CSTB], [SM])
                    for h in range(4):
                        MAX8(m8[:, :], small[:, h, :], [SM], [M8])
                        TS(small2[:, h, 64:104], small[:, h, :], m8[:, 2:3], None, ALU.is_ge, None, [SM, M8], [SM2])
                    TT(small2[:, :, 64:104], small2[:, :, 64:104], bc3(PVv), ALU.mult, [SM2, CSTB], [SM2])
                    TT(small2[:, :, 64:104], small2[:, :, 64:104], bc3(POv), ALU.add, [SM2, CSTB], [SM2])
                    TS(small2[:, :, 64:104], small2[:, :, 64:104], -1.0, -NEG, ALU.add, ALU.mult, [SM2], [SM2])
                    for h in range(4):
                        pt_, ptb = nextps()
                        TR(pt_[0:104, 0:128], small2[:, h, :], ident_f, [SM2, CSTB], [ptb])
                        CP(QTa[64:104, h, c * 128:(c + 1) * 128], pt_[64:104, 0:128], [ptb], [QTA],
                           eng="act" if h % 2 else "dve")
                for h in range(4):
                    load_KV(h, 0)
                    load_KV(h, 1)
                    kts = list(range(64)) + [64 + j for j in range(2 * (g + 1))]
                    acc, accb = psum[6 + h % 2], psb[6 + h % 2]
                    for i, kt in enumerate(kts):
                        ps, pb = nextps()
                        own = kt >= 64 + 2 * g
                        hf = 0 if kt < 32 else 1
                        MM(ps[:, 0:256], Kh[0:104, kt * 128:(kt + 1) * 128], QTa[0:104, h, 0:256], True, not own,
                           [KH[hf], QTA], [pb])
                        if own:
                            j = kt - (64 + 2 * g)
                            MM(ps[:, 0:256], ident_b, cstb[:, CB_CAUS + j * 256:CB_CAUS + (j + 1) * 256], False, True,
                               [CSTB], [pb])
                        pp, ppb = ptile[i % 2], B("ptile", i % 2)
                        ACT(pp[:, :], ps[:, 0:256], AF.Exp, [pb], [ppb])
                        MM(acc[0:65, 0:256], Vh[:, kt, 0:65], pp[:, :], i == 0, i == len(kts) - 1, [VH[hf], ppb], [accb])
                    ACS = B("accS")
                    CP(accS[:, :], acc[0:65, 0:256], [accb], [ACS])
                    ps, pb = nextps()
                    MM(ps[0:64, 0:256], ones_f[64:65, 0:64], accS[64:65, :], True, True, [CSTB, ACS], [pb])
                    t, tb = MTP.next()
                    RCP(t[0:64, 0:256], ps[0:64, 0:256], [pb], [tb])
                    TT(yc[:, h, 0:256], accS[0:64, :], t[0:64, 0:256], ALU.mult, [ACS, tb], [YC])
            else:
                for _ in sa_gen:
                    pass
                CP(yc[:, :, 0:NS], ycs[:, :, :], [B("ycs")], [YC])
            if DEBUG and l == 0:
                DMA('sp', d_ya[:, :, c0:c0 + n], ya[:, :, 0:n], [YA], [OUTB])
                DMA('sp', d_yb[:, :, c0:c0 + n], yb[:, :, 0:n], [YB], [OUTB])
                DMA('sp', d_yc[:, :, c0:c0 + n], yc[:, :, 0:n], [YC], [OUTB])
                DMA('sp', d_yd[:, :, c0:c0 + n], yd[:, :, 0:n], [YD], [OUTB])
            ysrc = [(ya, YA, 64, 4, wbrA_b), (yb, YB, 128, 2, wbrB_b), (yc, YC, 64, 4, wbrC_b), (yd, YD, 128, 2, wbrD_b)]
            for j, (yt, ybuf, kp, nk, wsrc) in enumerate(ysrc):
                for half in range(2):
                    wb_, wbb = loadw(wsrc[l][:, :, half * 512:(half + 1) * 512], kp, nk * 512, a=nk, R=[WSC[l]])
                    wbv = w3(wb_, nk)
                    wg_, wgb = loadw(wgate_b[l][:, :, j * 1024 + half * 512:j * 1024 + (half + 1) * 512], 128, 8 * 512, a=8, R=[WSC[l]])
                    wgv = w3(wg_, 8)
                    for mm_ in range(4):
                        m = half * 4 + mm_
                        psb_, pbb = nextps()
                        for k in range(nk):
                            MM(psb_[:, 0:n], wbv[:, k, mm_ * 128:(mm_ + 1) * 128], yt[:, k, 0:n], k == 0, k == nk - 1,
                               [wbb, ybuf], [pbb])
                        psg, pbg = nextps()
                        proj(psg[:, 0:n], pbg, wgv, wgb, 8, (mm_ * 128, mm_ * 128 + 128), c0, n)
                        t, tb = MTP.next()
                        ACT(t[:, 0:n], psg[:, 0:n], AF.Sigmoid, [pbg, CSTB], [tb], bias=sp(SP_BGATE + j * 8 + m))
                        if j == 0:
                            TT(mg32[:, m, 0:n], t[:, 0:n], psb_[:, 0:n], ALU.mult, [tb, pbb], [MG32])
                        else:
                            TT(t[:, 0:n], t[:, 0:n], psb_[:, 0:n], ALU.mult, [tb, pbb], [tb])
                            if j < 3:
                                TT(mg32[:, m, 0:n], mg32[:, m, 0:n], t[:, 0:n], ALU.add, [MG32, tb], [MG32])
                            else:
                                TT(mgb[:, m, 0:n], mg32[:, m, 0:n], t[:, 0:n], ALU.add, [MG32, tb], [MGB])
            if DEBUG and l == 0:
                DMA('sp', d_mg[:, :, c0:c0 + n], mgb[:, :, 0:n], [MGB], [OUTB])
            for half in range(2):
                wo_, wob = loadw(wout_b[l][:, :, half * 512:(half + 1) * 512], 128, 8 * 512, a=8, R=[WSC[l]])
                wov = w3(wo_, 8)
                for mm_ in range(4):
                    m = half * 4 + mm_
                    ps, pb = nextps()
                    for kc in range(8):
                        MM(ps[:, 0:n], wov[:, kc, mm_ * 128:(mm_ + 1) * 128], mgb[:, kc, 0:n], kc == 0, kc == 7,
                           [wob, MGB], [pb])
                    STT(xt[:, m, 0:n], xt[:, m, 0:n], ALPHA, ps[:, 0:n], ALU.mult, ALU.add, [XT, pb], [XT])
            layernorm(MTP, [(xt[:, m, 0:n], [XT]) for m in range(8)], n, 1024.0,
                      [sp(SP_LNG + m) for m in range(8)], [sp(SP_LNB + m) for m in range(8)],
                      [(xt[:, m, 0:n], [XT]) for m in range(8)],
                      [(xb[:, m, c0:c0 + n], [XB[ti]]) for m in range(8)])
            DMA("sp", xs1[:, :, c0:c0 + n], xt[:, :, 0:n], [XT], [XS1[ti]])
            if DEBUG and l == 0:
                DMA('sp', d_ln1[:, :, c0:c0 + n], xt[:, :, 0:n], [XT], [OUTB])
        FENCE()
        for ti, (c0, n) in enumerate(MT):
            DMA("sp", xf[:, :, c0:c0 + n], xs1[:, :, c0:c0 + n], [XS1[ti]], [XF[ti]])
        HB = [B("hb", i) for i in range(4)]
        SG = [B("sg", i) for i in range(3)]
        CMB = [B("cmb", i) for i in range(1)]
        CMT = B("cmbT")
        hrr = [0]
        moe = l % 2 == 1
        if moe:
            LG, FM8, FSC, EX8 = B("lg"), B("fm8"), B("fsc"), B("ex8")
            chunks = [(c * 128, 128) for c in range(16)] + [(NPR, NS)]
            for (t0, tn) in chunks:
                xfb = tok_bufs(XF, t0, tn)
                ps, pb = nextps()
                for kc in range(8):
                    MM(ps[0:tn, 0:8], xf[:, kc, t0:t0 + tn], wrt[:, kc, :], kc == 0, False, xfb + [CSTB], [pb])
                MM(ps[0:tn, 0:8], ones_f[0:1, 0:tn], brt[0:1, :], False, True, [CSTB], [pb])
                CP(lg[0:tn, :], ps[0:tn, 0:8], [pb], [LG])
                MAX8(fm8[0:tn, :], lg[0:tn, :], [LG], [FM8])
                TS(fsc[0:tn, 0:1], fm8[0:tn, 0:1], -1.0, None, ALU.mult, None, [FM8], [FSC])
                ACT(ex8[0:tn, :], lg[0:tn, :], AF.Exp, [LG, FSC], [EX8], bias=fsc[0:tn, 0:1])
                TS(lg[0:tn, :], lg[0:tn, :], fm8[0:tn, 1:2], None, ALU.is_ge, None, [LG, FM8], [LG])
                TT(ex8[0:tn, :], ex8[0:tn, :], lg[0:tn, :], ALU.mult, [EX8, LG], [EX8])
                RED(fsc[0:tn, 1:2], ex8[0:tn, :], ALU.add, [EX8], [FSC])
                RCP(fsc[0:tn, 2:3], fsc[0:tn, 1:2], [FSC], [FSC])
                TS(ex8[0:tn, :], ex8[0:tn, :], fsc[0:tn, 2:3], None, ALU.mult, None, [EX8, FSC], [EX8])
                pt_, ptb = nextps()
                TR(pt_[0:8, 0:tn], ex8[0:tn, :], ident_f[0:tn, 0:tn], [EX8, CSTB], [ptb])
                CP(cmbT[:, t0:t0 + tn], pt_[0:8, 0:tn], [ptb], [CMT])
        for ti, (c0, n) in enumerate(MT):
            for m in range(8):
                ACT(xf[:, m, c0:c0 + n], xf[:, m, c0:c0 + n], AF.Copy, [XF[ti]], [XF[ti]], scale=ALPHA)
        ngroups = 8 * NMOE_G if moe else NFFN_G
        wsrc = moew if moe else ffnw
        for gi in range(ngroups):
            e_ = gi // NMOE_G
            if moe and gi % NMOE_G == 0:
                for (c0, n) in FT:
                    ps, pb = nextps()
                    MM(ps[:, 0:n], cst[0:8, C_SEL8 + e_ * 128:C_SEL8 + (e_ + 1) * 128], cmbT[0:8, c0:c0 + n], True, True,
                       [CSTB, CMT], [pb])
                    CP(cmbbc[0][:, c0:c0 + n], ps[:, 0:n], [pb], [CMB[0]], eng="act")
            wf_, wfb = loadw(wsrc[gi], 128, 6144)
            wgv = w3(wf_[:, 0:2048], 8)
            wuv = w3(wf_[:, 2048:4096], 8)
            wdv = w3(wf_[:, 4096:6144], 2)
            for (c0, n) in FT:
                xfb = tok_bufs(XF, c0, n)
                hs = []
                for hc in range(2):
                    psg, pbg = nextps()
                    proj(psg[:, 0:n], pbg, wgv, wfb, 8, (hc * 128, hc * 128 + 128), c0, n)
                    psu, pbu = nextps()
                    proj(psu[:, 0:n], pbu, wuv, wfb, 8, (hc * 128, hc * 128 + 128), c0, n)
                    si = hrr[0] % 3
                    hi = hrr[0] % 4
                    hrr[0] += 1
                    ACT(sgt[si][:, 0:n], psg[:, 0:n], AF.Sigmoid, [pbg], [SG[si]])
                    TT(sgt[si][:, 0:n], sgt[si][:, 0:n], psg[:, 0:n], ALU.mult, [SG[si], pbg], [SG[si]])
                    if moe:
                        TT(sgt[si][:, 0:n], sgt[si][:, 0:n], cmbbc[0][:, c0:c0 + n], ALU.mult,
                           [SG[si], CMB[0]], [SG[si]])
                    TT(hb[hi][:, 0:n], sgt[si][:, 0:n], psu[:, 0:n], ALU.mult, [SG[si], pbu], [HB[hi]])
                    hs.append((hb[hi], HB[hi]))
                for m in range(8):
                    ps, pb = nextps()
                    for hc in range(2):
                        MM(ps[:, 0:n], wdv[:, hc, m * 128:(m + 1) * 128], hs[hc][0][:, 0:n], hc == 0, hc == 1,
                           [wfb, hs[hc][1]], [pb])
                    TT(xf[:, m, c0:c0 + n], xf[:, m, c0:c0 + n], ps[:, 0:n], ALU.add, xfb + [pb], xfb)
        for (c0, n) in FT:
            layernorm_x(l, 1, c0, n)
            if DEBUG and l == 0:
                DMA('sp', d_ln2[:, :, c0:c0 + n], xf[:, :, c0:c0 + n], tok_bufs(XF, c0, n), [OUTB])
        PTB = B("pTb")
        for (c0, n) in FT:
            DMA("pool", pTb[:, :, c0:c0 + n], p_in[l][:, :, c0:c0 + n], [], [PTB])
        wppb = B("wppt")
        DMA("pool", wppt[:, :], plep[l].rearrange("p a b -> p (a b)"), [], [wppb])
        wppv = w3(wppt[:, :], 2)
        for (c0, n) in FT:
            xfb = tok_bufs(XF, c0, n)
            for half in range(2):
                wpg_, wpgb = loadw(pleg[l][:, :, half * 512:(half + 1) * 512], 128, 8 * 512, a=8)
                wpgv = w3(wpg_, 8)
                for mm_ in range(4):
                    m = half * 4 + mm_
                    ps1, pb1 = nextps()
                    proj(ps1[:, 0:n], pb1, wpgv, wpgb, 8, (mm_ * 128, mm_ * 128 + 128), c0, n)
                    ps2, pb2 = nextps()
                    for c in range(2):
                        MM(ps2[:, 0:n], wppv[:, c, m * 128:(m + 1) * 128], pTb[:, c, c0:c0 + n], c == 0, c == 1,
                           [wppb, PTB], [pb2])
                    si = hrr[0] % 3
                    hrr[0] += 1
                    ACT(sgt[si][:, 0:n], ps1[:, 0:n], AF.Sigmoid, [pb1], [SG[si]])
                    TT(sgt[si][:, 0:n], sgt[si][:, 0:n], ps2[:, 0:n], ALU.mult, [SG[si], pb2], [SG[si]])
                    STT(xf[:, m, c0:c0 + n], xf[:, m, c0:c0 + n], ALPHA, sgt[si][:, 0:n], ALU.mult, ALU.add,
                        xfb + [SG[si]], xfb)
            layernorm_x(l, 2, c0, n)
        if DEBUG and l == 0:
            for ti, (c0, n) in enumerate(MT):
                DMA('sp', d_ln3[:, :, c0:c0 + n], xf[:, :, c0:c0 + n], [XF[ti]], [OUTB])
        dst = o_y if l == DEPTH - 1 else xs2
        for ti, (c0, n) in enumerate(MT):
            DMA("sp", dst[:, :, c0:c0 + n], xf[:, :, c0:c0 + n], [XF[ti]], [OUTB if l == DEPTH - 1 else XS2[ti]])
        FENCE()

    emit(nc, P)
    return nc, len(P.ops)


def _fm(a, nch):
    return np.ascontiguousarray(a.reshape(nch, 128, -1).transpose(1, 0, 2))


_CACHE = {}


def _constants():
    c = np.zeros((128, NCST), np.float32)
    c[:, C_ID:C_ID + 128] = np.eye(128, dtype=np.float32)
    c[:, C_ONE:C_ONE + 128] = 1.0
    s = np.arange(128)
    c[:, C_TRI:C_TRI + 128] = (s[:, None] <= s[None, :]).astype(np.float32)
    pair = (s[:, None] // 2 == np.arange(64)[None, :]).astype(np.float32)
    c[:, C_PAIR:C_PAIR + 64] = pair
    c[0:64, C_PAIRT:C_PAIRT + 128] = pair.T
    for e in range(8):
        c[e, C_SEL8 + e * 128:C_SEL8 + (e + 1) * 128] = 1.0
    cb = np.zeros((128, NCSTB), np.float32)
    cb[:, CB_ID:CB_ID + 128] = np.eye(128, dtype=np.float32)
    q = np.arange(256)
    for j in range(2):
        key = j * 128 + s
        cb[:, CB_CAUS + j * 256:CB_CAUS + (j + 1) * 256] = np.where(key[:, None] <= q[None, :], 0.0, NEG)
    koh = np.zeros((40, 10240), np.float32)
    for b in range(40):
        koh[b, b * 256:(b + 1) * 256] = 1.0
    return c, cb, koh


def kernel(x_prompt, x_sample, p_prompt, p_sample, cache_k, cache_v, state_conv_b, state_conv_d, page_table,
           w_in, w_gate, b_gate, a_ln_g, a_ln_b, a_w_s, a_b_s, b_conv_w, d_conv_w, d_conv_b, d_ln_g, d_ln_b,
           w_branch, w_out, ln_g, ln_b, ffn_w_gate, ffn_w_up, ffn_w_down, moe_w_router, moe_b_router,
           moe_w_gate, moe_w_up, moe_w_down, ple_w_gate, ple_w_proj):
    f = lambda a: np.asarray(a, dtype=np.float32)
    x_prompt, x_sample, p_prompt, p_sample = f(x_prompt), f(x_sample), f(p_prompt), f(p_sample)
    cache_k, cache_v = f(cache_k), f(cache_v)
    if "nc" not in _CACHE:
        _CACHE["nc"] = build_program()
    nc, _ = _CACHE["nc"]

    sh = {}
    sh["win"] = np.stack([_fm(f(w_in[l]), 8) for l in range(2)])
    sh["wgate"] = np.stack([_fm(f(w_gate[l]), 8) for l in range(2)])
    wbr = f(w_branch)
    sh["wbrA"] = np.ascontiguousarray(wbr[:, 0].reshape(2, 4, 64, 1024).transpose(0, 2, 1, 3))
    sh["wbrB"] = np.stack([_fm(wbr[l, 1], 2) for l in range(2)])
    sh["wbrC"] = np.ascontiguousarray(wbr[:, 2].reshape(2, 4, 64, 1024).transpose(0, 2, 1, 3))
    sh["wbrD"] = np.stack([_fm(wbr[l, 3], 2) for l in range(2)])
    sh["wout"] = np.stack([_fm(f(w_out[l]), 8) for l in range(2)])
    aws = f(a_w_s)
    sh["awsT"] = np.ascontiguousarray(aws.transpose(0, 3, 1, 2))
    sh["absr"] = np.ascontiguousarray(f(a_b_s).reshape(2, 1, 4, 128))
    spa = np.zeros((2, 128, NSP), np.float32)
    for l in range(2):
        spa[l, :, SP_BGATE:SP_BGATE + 32] = f(b_gate[l]).reshape(32, 128).T
        spa[l, :, SP_LNG:SP_LNG + 24] = f(ln_g[l]).reshape(24, 128).T
        spa[l, :, SP_LNB:SP_LNB + 24] = f(ln_b[l]).reshape(24, 128).T
        spa[l, :, SP_ALNG:SP_ALNG + 2] = f(a_ln_g[l]).reshape(2, 128).T
        spa[l, :, SP_ALNB:SP_ALNB + 2] = f(a_ln_b[l]).reshape(2, 128).T
        spa[l, :, SP_DLNG:SP_DLNG + 2] = f(d_ln_g[l]).reshape(2, 128).T
        spa[l, :, SP_DLNB:SP_DLNB + 2] = f(d_ln_b[l]).reshape(2, 128).T
        spa[l, :, SP_DCB:SP_DCB + 2] = f(d_conv_b[l]).reshape(2, 128).T
        spa[l, :, SP_BCW:SP_BCW + 6] = f(b_conv_w[l]).reshape(3, 2, 128).transpose(2, 1, 0).reshape(128, 6)
        spa[l, :, SP_DCW:SP_DCW + 62] = f(d_conv_w[l]).reshape(31, 2, 128).transpose(2, 1, 0).reshape(128, 62)
        spa[l, :, SP_AW00:SP_AW00 + 4] = aws[l, :, 0, 0][None, :]
        spa[l, :, SP_ABS0:SP_ABS0 + 4] = f(a_b_s[l])[:, 0][None, :]
    sh["sp_in"] = spa

    def pack_ffn(wg, wu, wd, ng):
        H = wg.shape[1]
        g = _fm(wg, 8).reshape(128, 8, ng, 256).transpose(2, 0, 1, 3).reshape(ng, 128, 2048)
        u = _fm(wu, 8).reshape(128, 8, ng, 256).transpose(2, 0, 1, 3).reshape(ng, 128, 2048)
        d = wd.reshape(ng, 2, 128, 1024).transpose(0, 2, 1, 3).reshape(ng, 128, 2048)
        return np.ascontiguousarray(np.concatenate([g, u, d], axis=2))

    sh["ffnw"] = pack_ffn(f(ffn_w_gate[0]), f(ffn_w_up[0]), f(ffn_w_down[0]), NFFN_G)
    sh["moew"] = np.concatenate([pack_ffn(f(moe_w_gate[0, e]), f(moe_w_up[0, e]), f(moe_w_down[0, e]), NMOE_G)
                                 for e in range(8)], axis=0)
    sh["wr_in"] = _fm(f(moe_w_router[0]), 8)
    sh["br_in"] = f(moe_b_router).reshape(1, 8)
    sh["pleg"] = np.stack([_fm(f(ple_w_gate[l]), 8) for l in range(2)])
    sh["plep"] = np.stack([_fm(f(ple_w_proj[l]), 2) for l in range(2)])
    cst, cstb, koh = _constants()
    sh["cstb_in"] = cstb
    sh["koh_in"] = koh
    sh["ck_in"] = cache_k.reshape(2 * NPOOL * NCHK, PCH * 256)
    sh["cv_in"] = cache_v.reshape(2 * NPOOL * NCHK, PCH * 256)
    scb = f(state_conv_b)
    scd = f(state_conv_d)
    pt = np.asarray(page_table).astype(np.int32)

    in_maps = []
    for c in range(8):
        b, r = c // 4, c % 4
        m = dict(sh)
        xs = np.concatenate([x_prompt[b, r * NPR:(r + 1) * NPR], x_sample[4 * c:4 * c + 4, 0]], axis=0)
        m["x_in"] = _fm(np.ascontiguousarray(xs.T), 8)
        ps_ = [np.concatenate([p_prompt[l, b, r * NPR:(r + 1) * NPR], p_sample[l, 4 * c:4 * c + 4, 0]], axis=0)
               for l in range(2)]
        m["p_in"] = np.stack([_fm(np.ascontiguousarray(p.T), 2) for p in ps_])
        m["scb_in"] = np.ascontiguousarray(scb[:, 4 * c:4 * c + 4].reshape(2, NS, 2, 2, 128).transpose(0, 4, 3, 1, 2))
        m["scd_in"] = np.ascontiguousarray(scd[:, 4 * c:4 * c + 4].reshape(2, NS, 30, 2, 128).transpose(0, 4, 3, 1, 2))
        m["pt_in"] = np.ascontiguousarray(pt[4 * c:4 * c + 4].T)
        cc_ = cst.copy()
        for g in range(8):
            pb = np.full(40, -1e6, np.float32)
            pv = np.zeros(40, np.float32)
            po = np.zeros(40, np.float32)
            pb[0:8 * r] = 0.0
            pv[0:8 * r] = 1.0
            pb[32:32 + g] = 0.0
            pv[32:32 + g] = 1.0
            po[32 + g] = 1.0
            cc_[:, C_PB + g * 40:C_PB + (g + 1) * 40] = pb[None, :]
            cc_[:, C_PV + g * 40:C_PV + (g + 1) * 40] = pv[None, :]
            cc_[:, C_PO + g * 40:C_PO + (g + 1) * 40] = po[None, :]
        if r > 0:
            cc_[:, C_SELR + r - 1] = 1.0
        m["cst_in"] = cc_
        in_maps.append(m)

    res = run_bass_kernel_spmd(nc, in_maps, core_ids=list(range(8)))
    R = res.results
    if DEBUG:
        _CACHE['dbg'] = {k: np.asarray(v).astype(np.float32) for k, v in R[0].items() if k.startswith('d_')}

    y_prompt = np.zeros((2, 8192, 1024), np.float32)
    y_sample = np.zeros((32, 1, 1024), np.float32)
    k_prompt = np.zeros((2, 2, 8192, 4, 64), np.float32)
    v_prompt = np.zeros((2, 2, 8192, 4, 64), np.float32)
    k_sample = np.zeros((2, 32, 1, 4, 64), np.float32)
    v_sample = np.zeros((2, 32, 1, 4, 64), np.float32)
    conv_b_prompt = np.zeros((2, 2, 2, 256), np.float32)
    conv_b_sample = np.zeros((2, 32, 2, 256), np.float32)
    conv_d_prompt = np.zeros((2, 2, 30, 256), np.float32)
    conv_d_sample = np.zeros((2, 32, 30, 256), np.float32)
    chunk_v_sample = np.zeros((2, 32, 1, 256), np.float32)
    for c in range(8):
        b, r = c // 4, c % 4
        o = R[c]
        y = np.asarray(o["o_y"]).transpose(2, 1, 0).reshape(T, 1024)
        y_prompt[b, r * NPR:(r + 1) * NPR] = y[:NPR]
        y_sample[4 * c:4 * c + 4, 0] = y[NPR:]
        ok = np.asarray(o["o_k"]).transpose(0, 3, 2, 1)
        k_prompt[:, b, r * NPR:(r + 1) * NPR] = ok[:, :NPR]
        k_sample[:, 4 * c:4 * c + 4, 0] = ok[:, NPR:]
        ov = np.asarray(o["o_v"]).transpose(0, 3, 2, 1).reshape(2, T, 4, 64)
        v_prompt[:, b, r * NPR:(r + 1) * NPR] = ov[:, :NPR]
        v_sample[:, 4 * c:4 * c + 4, 0] = ov[:, NPR:]
        if r == 3:
            conv_b_prompt[:, b] = np.asarray(o["o_cb"]).transpose(0, 3, 2, 1).reshape(2, 2, 256)
            conv_d_prompt[:, b] = np.asarray(o["o_cd"]).transpose(0, 3, 2, 1).reshape(2, 30, 256)
        conv_b_sample[:, 4 * c:4 * c + 4] = np.asarray(o["o_cbs"]).transpose(0, 3, 4, 2, 1).reshape(2, NS, 2, 256)
        conv_d_sample[:, 4 * c:4 * c + 4] = np.asarray(o["o_cds"]).transpose(0, 3, 4, 2, 1).reshape(2, NS, 30, 256)
        chunk_v_sample[:, 4 * c:4 * c + 4, 0] = np.asarray(o["o_cv"]).transpose(0, 3, 2, 1).reshape(2, NS, 256)
    return (y_prompt, y_sample, k_prompt, v_prompt, k_sample, v_sample, conv_b_prompt, conv_b_sample,
            conv_d_prompt, conv_d_sample, chunk_v_sample)
```

```python
import numpy as np
import concourse.bass as bass
import concourse.mybir as mybir
from concourse.bass_utils import run_bass_kernel_spmd

F32 = mybir.dt.float32
BF16 = mybir.dt.bfloat16
I32 = mybir.dt.int32
AF = mybir.ActivationFunctionType
ALU = mybir.AluOpType
AX = mybir.AxisListType

NPR = 2048
NS = 4
T = NPR + NS
MT = [(i * 256, 256) for i in range(8)] + [(NPR, NS)]
FT = [(0, 512), (512, 512), (1024, 512), (1536, 512), (2048, 4)]
DEPTH = 2
ALPHA = (2 * DEPTH) ** 0.25
EPS = 1e-5
NEG = -30000.0
NPOOL = 5120
GROUPS = [[0, 1, 2, 3], [4, 5, 6, 7]]
NFFN_G = 11
NMOE_G = 14
PCH = 16
import os
DEBUG = bool(os.environ.get('KDBG'))
NCHK = 128 // PCH

SP_BGATE = 0
SP_LNG = 32
SP_LNB = 56
SP_ALNG = 80
SP_ALNB = 82
SP_DLNG = 84
SP_DLNB = 86
SP_DCB = 88
SP_BCW = 90
SP_DCW = 96
SP_AW00 = 158
SP_ABS0 = 162
NSP = 166
C_ID = 0
C_ONE = 128
C_TRI = 256
C_PAIR = 384
C_PAIRT = 448
C_SEL8 = 576
C_PB = 1600
C_PV = 1920
C_PO = 2240
C_SELR = 2560
NCST = 2564
CB_ID = 0
CB_CAUS = 128
NCSTB = 640


class Buf:
    __slots__ = ("w", "r", "name")

    def __init__(self, name=""):
        self.w = None
        self.r = {}
        self.name = name


class Op:
    __slots__ = ("eng", "fn", "deps", "kind", "sem", "val", "signal", "idx")


class Prog:
    def __init__(self):
        self.ops = []
        self.bufs = {}
        self.always = []

    def B(self, *key):
        b = self.bufs.get(key)
        if b is None:
            b = self.bufs[key] = Buf(str(key))
        return b

    def add(self, eng, fn, reads=(), writes=(), kind="c"):
        op = Op()
        op.eng, op.fn, op.kind = eng, fn, kind
        op.idx = len(self.ops)
        op.signal = False
        op.sem = None
        op.val = 0
        deps = {}
        reads = list(reads) + [b for b in self.always if b not in writes]
        for b in reads:
            if b.w is not None:
                deps[b.w.idx] = b.w
        for b in writes:
            for o in b.r.values():
                deps[o.idx] = o
            if b.w is not None:
                deps[b.w.idx] = b.w
        rkey = eng if kind == "c" else ("d", op.idx)
        for b in reads:
            b.r[rkey] = op
        for b in writes:
            b.w = op
            b.r = {}
        deps.pop(op.idx, None)
        dl = []
        for o in deps.values():
            if o.kind == "c" and kind == "c" and o.eng == "pe" and eng == "pe":
                continue
            dl.append(o)
        op.deps = dl
        self.ops.append(op)
        return op


def emit(nc, P):
    ops = P.ops
    for op in ops:
        for d in op.deps:
            d.signal = True
    NRING = 12
    csem = {e: nc.alloc_semaphore(name=f"c_{e}") for e in ("pe", "act", "dve", "pool")}
    rings = {q: [nc.alloc_semaphore(name=f"d_{q}{i}") for i in range(NRING)] for q in ("sp", "pool")}
    ccsem = nc.alloc_semaphore(name="ccs")
    ccount = {e: 0 for e in csem}
    rcount = {}
    rlast = {}
    rpos = {"sp": 0, "pool": 0}
    cccount = 0
    finals = {}
    for op in ops:
        if op.kind == "c":
            if op.signal:
                ccount[op.eng] += 1
                op.sem = csem[op.eng]
                op.val = ccount[op.eng]
        elif op.kind == "d":
            s = rings[op.eng][rpos[op.eng] % NRING]
            rpos[op.eng] += 1
            key = id(s)
            prev = rlast.get(key)
            if prev is not None:
                op.deps.append(prev)
            rcount[key] = rcount.get(key, 0) + 1
            rlast[key] = op
            op.sem = s
            op.val = 16 * rcount[key]
            finals[key] = (s, op.val)
        else:
            cccount += 1
            op.sem = ccsem
            op.val = cccount
            finals[id(ccsem)] = (ccsem, cccount)
    assert max(ccount.values()) < 60000, ccount
    queues = {"pe": [], "act": [], "dve": [], "pool": [], "sp": []}
    for op in ops:
        queues[op.eng].append(op)

    def run(eng_name, e):
        waited = {}
        for op in queues[eng_name]:
            need = {}
            for d in op.deps:
                k = id(d.sem)
                if d.val > need.get(k, (None, 0))[1]:
                    need[k] = (d.sem, d.val)
            for k, (s, v) in need.items():
                if waited.get(k, 0) >= v:
                    continue
                e.wait_ge(s, v)
                waited[k] = v
            ins = op.fn(e)
            if op.kind == "c":
                if op.signal:
                    ins.then_inc(op.sem, 1)
            elif op.kind == "d":
                ins.then_inc(op.sem, 16)
            else:
                ins.then_inc(op.sem, 1)
        if eng_name == "sp":
            for k, (s, v) in finals.items():
                e.wait_ge(s, v)

    with nc.Block() as block:
        @block.tensor
        def _(e):
            run("pe", e)

        @block.scalar
        def _(e):
            run("act", e)

        @block.vector
        def _(e):
            run("dve", e)

        @block.gpsimd
        def _(e):
            run("pool", e)

        @block.sync
        def _(e):
            run("sp", e)


def build_program():
    nc = bass.Bass("TRN2", target_bir_lowering=False)
    P = Prog()
    B = P.B

    def din(name, shape, dt=F32):
        return nc.dram_tensor(name, list(shape), dt, kind="ExternalInput").ap()

    def dout(name, shape, dt=F32):
        return nc.dram_tensor(name, list(shape), dt, kind="ExternalOutput").ap()

    def dint(name, shape, dt):
        return nc.dram_tensor(name, list(shape), dt, kind="Internal").ap()

    x_in = din("x_in", [128, 8, T])
    p_in = din("p_in", [2, 128, 2, T])
    scb_in = din("scb_in", [2, 128, 2, NS, 2])
    scd_in = din("scd_in", [2, 128, 2, NS, 30])
    pt_in = din("pt_in", [128, NS], I32)
    ck_in = din("ck_in", [2 * NPOOL * NCHK, PCH * 256])
    cv_in = din("cv_in", [2 * NPOOL * NCHK, PCH * 256])
    win = din("win", [2, 128, 8, 2560])
    wgate = din("wgate", [2, 128, 8, 4096])
    wbrA = din("wbrA", [2, 64, 4, 1024])
    wbrB = din("wbrB", [2, 128, 2, 1024])
    wbrC = din("wbrC", [2, 64, 4, 1024])
    wbrD = din("wbrD", [2, 128, 2, 1024])
    wout = din("wout", [2, 128, 8, 1024])
    awsT = din("awsT", [2, 128, 4, 128])
    absr = din("absr", [2, 1, 4, 128])
    sp_in = din("sp_in", [2, 128, NSP])
    ffnw = din("ffnw", [NFFN_G, 128, 6144])
    moew = din("moew", [8 * NMOE_G, 128, 6144])
    wr_in = din("wr_in", [128, 8, 8])
    br_in = din("br_in", [1, 8])
    pleg = din("pleg", [2, 128, 8, 1024])
    plep = din("plep", [2, 128, 2, 1024])
    cst_in = din("cst_in", [128, NCST])
    cstb_in = din("cstb_in", [128, NCSTB])
    koh_in = din("koh_in", [40, 10240])

    o_y = dout("o_y", [128, 8, T])
    o_k = dout("o_k", [2, 64, 4, T])
    o_v = dout("o_v", [2, 128, 2, T])
    o_cb = dout("o_cb", [2, 128, 2, 2])
    o_cd = dout("o_cd", [2, 128, 2, 30])
    o_cbs = dout("o_cbs", [2, 128, 2, NS, 2])
    o_cds = dout("o_cds", [2, 128, 2, NS, 30])
    o_cv = dout("o_cv", [2, 128, 2, NS])
    if DEBUG:
        d_ya = dout('d_ya', [64, 4, T], BF16)
        d_yb = dout('d_yb', [128, 2, T], BF16)
        d_yc = dout('d_yc', [64, 4, T], BF16)
        d_yd = dout('d_yd', [128, 2, T], BF16)
        d_mg = dout('d_mg', [128, 8, T], BF16)
        d_ln1 = dout('d_ln1', [128, 8, T])
        d_ln2 = dout('d_ln2', [128, 8, T])
        d_ln3 = dout('d_ln3', [128, 8, T])

    cc_kt = [dint(f"cc_kt{l}", [256, NPR], BF16) for l in range(2)]
    cc_ktg = [dint(f"cc_ktg{l}", [4 * 256, NPR], BF16) for l in range(2)]
    cc_v = [dint(f"cc_v{l}", [128, 4096], BF16) for l in range(2)]
    cc_vg = [dint(f"cc_vg{l}", [4 * 128, 4096], BF16) for l in range(2)]
    cc_t = [dint(f"cc_t{l}", [128, 64], F32) for l in range(2)]
    cc_tg = [dint(f"cc_tg{l}", [4 * 128, 64], F32) for l in range(2)]
    win_b = [dint(f"win_b{l}", [128, 8, 2560], BF16) for l in range(2)]
    wgate_b = [dint(f"wgate_b{l}", [128, 8, 4096], BF16) for l in range(2)]
    wbrA_b = [dint(f"wbrA_b{l}", [64, 4, 1024], BF16) for l in range(2)]
    wbrB_b = [dint(f"wbrB_b{l}", [128, 2, 1024], BF16) for l in range(2)]
    wbrC_b = [dint(f"wbrC_b{l}", [64, 4, 1024], BF16) for l in range(2)]
    wbrD_b = [dint(f"wbrD_b{l}", [128, 2, 1024], BF16) for l in range(2)]
    wout_b = [dint(f"wout_b{l}", [128, 8, 1024], BF16) for l in range(2)]
    xs1 = dint("xs1", [128, 8, T], F32)
    xs2 = dint("xs2", [128, 8, T], F32)

    ARENA_B = 206000
    arena = nc.alloc_sbuf_tensor("arena", [128, ARENA_B // 2], BF16)
    cur = [0]

    def carve(nbytes):
        off = (cur[0] + 63) // 64 * 64
        cur[0] = off + nbytes
        assert cur[0] <= ARENA_B, f"SBUF arena overflow {cur[0]}"
        return off

    def tile(shape, dt):
        esz = 4 if dt in (F32, I32) else 2
        n = 1
        for s in shape[1:]:
            n *= s
        off = carve(n * esz)
        v = arena[0:shape[0], off // 2: off // 2 + n * esz // 2]
        if dt != BF16:
            v = v.bitcast(dt)
        if len(shape) == 3:
            v = v.rearrange("p (a b) -> p a b", a=shape[1])
        elif len(shape) == 4:
            v = v.rearrange("p (a b c) -> p a b c", a=shape[1], b=shape[2])
        return v

    xb = tile([128, 8, T], BF16)
    WSLOT = 8192
    NW = 2
    wslots = [tile([128, WSLOT], BF16) for _ in range(NW)]
    cst = tile([128, NCST], F32)
    cstb = tile([128, NCSTB], BF16)
    spt = [tile([128, NSP], F32) for _ in range(2)]
    awsb = tile([128, 4, 128], BF16)
    awsf = tile([128, 4, 128], F32)
    absf = tile([1, 4, 128], F32)
    wrt = tile([128, 8, 8], F32)
    brt = tile([1, 8], F32)
    cbprev = tile([128, 2, 2], F32)
    cdprev = tile([128, 2, 30], F32)
    tails = tile([128, 64], F32)
    tailg = tile([128, 4, 64], F32)
    pti = tile([128, NS], I32)
    idx = tile([128, 2], I32)
    QTs = tile([64, 4, NS], F32)
    KTs = tile([64, 4, NS], F32)
    VTs = tile([64, 4, NS], F32)
    kmT = tile([64, 4, 40], BF16)
    ycs = tile([64, 4, NS], BF16)
    m8s = tile([4, 8], F32)
    fsc = tile([128, 4], F32)
    phase_base = cur[0]

    xt = tile([128, 8, 256], F32)
    ya = tile([64, 4, 256], BF16)
    yb = tile([128, 2, 256], BF16)
    yc = tile([64, 4, 256], BF16)
    yd = tile([128, 2, 256], BF16)
    mg32 = tile([128, 8, 256], F32)
    mgb = tile([128, 8, 256], BF16)
    QTa = tile([104, 4, 256], BF16)
    Kh = tile([104, 10240], BF16)
    Vh = tile([128, 80, 65], BF16)
    vtm = tile([128, 4, 16, 64], BF16)
    cbT = tile([128, 2, 2 + 256], F32)
    cdT = tile([128, 2, 30 + 256], F32)
    NTMP = 4
    mtmp = [tile([128, 256], F32) for _ in range(NTMP)]
    mlnm = tile([128, 256], F32)
    mlnr = tile([128, 256], F32)
    dacc = [tile([128, 256], F32) for _ in range(2)]
    gv = tile([128, 2, 256], F32)
    vn = tile([128, 2, 256], F32)
    vtA = tile([128, 256], BF16)
    ptile = [tile([128, 256], BF16) for _ in range(3)]
    accS = tile([65, 256], F32)
    small = tile([128, 4, 40], F32)
    small2 = tile([128, 4, 104], F32)
    m8 = tile([128, 8], F32)
    kTb = tile([64, 4, 256], BF16)
    cbs = tile([128, 2, NS, 3], F32)
    cds = tile([128, 2, NS, 31], F32)
    sm3 = tile([128, 2, NS, 31], F32)
    Kc = tile([128, PCH * 256], F32)
    Ssc = tile([128, 128, 4], F32)
    Psc = tile([128, 128, 4], F32)
    qbc = tile([128, 256], F32)
    pvp = tile([128, 256], F32)
    pvc = tile([128, 256], F32)
    sdg = tile([64, 64], F32)
    ssm = tile([128, 16], F32)
    mix_end = cur[0]

    cur[0] = phase_base
    xf = tile([128, 8, T], F32)
    cmbT = tile([8, T], F32)
    cmbbc = [tile([128, T], F32) for _ in range(1)]
    hb = [tile([128, 512], BF16) for _ in range(4)]
    sgt = [tile([128, 512], F32) for _ in range(3)]
    ftmp = [tile([128, 512], F32) for _ in range(NTMP)]
    flnm = tile([128, 512], F32)
    flnr = tile([128, 512], F32)
    pTb = tile([128, 2, T], BF16)
    wppt = tile([128, 2048], BF16)
    lg = tile([128, 8], F32)
    ex8 = tile([128, 8], F32)
    fm8 = tile([128, 8], F32)
    ffn_end = cur[0]
    cur[0] = max(mix_end, ffn_end)
    REG = B("region")
    P.always = [REG]

    psum = [nc.alloc_psum_tensor(f"ps{i}", [128, 512], F32) for i in range(8)]
    psb = [B("ps", i) for i in range(8)]
    psrr = [0]

    psmod = [6]

    def nextps():
        i = psrr[0] % psmod[0]
        psrr[0] += 1
        return psum[i], psb[i]

    def MM(out, lhsT, rhs, start, stop, R, W):
        P.add("pe", lambda e, o=out, a=lhsT, b=rhs, s=start, t=stop: e.matmul(o, a, b, start=s, stop=t), R, W)

    def TR(out, in_, ident, R, W):
        P.add("pe", lambda e, o=out, a=in_, i=ident: e.transpose(o, a, i), R, W)

    def ACT(out, in_, func, R, W, bias=0.0, scale=1.0):
        P.add("act", lambda e, o=out, a=in_, f=func, b=bias, s=scale: e.activation(o, a, f, bias=b, scale=s), R, W)

    def TT(out, a, b, op, R, W, eng="dve"):
        P.add(eng, lambda e, o=out, x=a, y=b, p=op: e.tensor_tensor(o, x, y, p), R, W)

    def TS(out, a, s1, s2, op0, op1, R, W, eng="dve"):
        if s2 is None:
            P.add(eng, lambda e, o=out, x=a, q=s1, p=op0: e.tensor_scalar(o, x, q, None, p), R, W)
        else:
            P.add(eng, lambda e, o=out, x=a, q=s1, r=s2, p=op0, p1=op1: e.tensor_scalar(o, x, q, r, p, p1), R, W)

    def STT(out, a, sc, b, op0, op1, R, W, eng="dve"):
        P.add(eng, lambda e, o=out, x=a, s=sc, y=b, p=op0, q=op1: e.scalar_tensor_tensor(o, x, s, y, p, q), R, W)

    def CP(out, in_, R, W, eng="dve"):
        if eng == "act":
            P.add("act", lambda e, o=out, a=in_: e.activation(o, a, AF.Copy), R, W)
        else:
            P.add(eng, lambda e, o=out, a=in_: e.tensor_copy(o, a), R, W)

    def RED(out, in_, op, R, W, eng="dve"):
        P.add(eng, lambda e, o=out, a=in_, p=op: e.tensor_reduce(o, a, AX.X, p), R, W)

    def MAX8(out, in_, R, W):
        P.add("dve", lambda e, o=out, a=in_: e.max(o, a), R, W)

    def RCP(out, in_, R, W):
        P.add("dve", lambda e, o=out, a=in_: e.reciprocal(o, a), R, W)

    def DMA(q, out, in_, R, W):
        P.add(q, lambda e, o=out, a=in_: e.dma_start(out=o, in_=a), R, W, kind="d")

    def FENCE():
        P.add("dve", lambda e: e.tensor_copy(fsc[0:1, 3:4], fsc[0:1, 3:4]), [], [REG])

    def w3(ap2d, a):
        return ap2d.rearrange("p (a b) -> p a b", a=a)

    wrr = [0]
    wsb = [B("wslot", i) for i in range(NW)]

    def loadw(src, parts, n, a=None, R=()):
        i = wrr[0] % NW
        wrr[0] += 1
        dst = wslots[i][0:parts, 0:n]
        DMA("pool", w3(dst, a) if a else dst, src, list(R), [wsb[i]])
        return dst, wsb[i]

    cs = lambda off, n, p=128: cst[0:p, off:off + n]
    ident_f = cs(C_ID, 128)
    ones_f = cs(C_ONE, 128)
    CSTB = B("cst")
    ident_b = cstb[:, CB_ID:CB_ID + 128]

    class TmpPool:
        def __init__(self, tiles, lnm, lnr, name):
            self.tiles = tiles
            self.bufs = [B(name, i) for i in range(len(tiles))]
            self.lnm, self.lnr = lnm, lnr
            self.lnmb, self.lnrb = B(name, "lnm"), B(name, "lnr")
            self.i = 0

        def next(self):
            i = self.i % len(self.tiles)
            self.i += 1
            return self.tiles[i], self.bufs[i]

    MTP = TmpPool(mtmp, mlnm, mlnr, "mtmp")
    FTP = TmpPool(ftmp, flnm, flnr, "ftmp")

    XB = [B("xb", i) for i in range(9)]
    XF = [B("xf", i) for i in range(9)]

    def tok_bufs(lst, c0, n):
        if c0 >= NPR:
            return [lst[8]]
        return [lst[i] for i in range(c0 // 256, (c0 + n - 1) // 256 + 1)]

    DMA("sp", cst[:, :], cst_in, [], [CSTB])
    DMA("pool", cstb[:, :], cstb_in, [], [CSTB])
    for l in range(2):
        DMA("sp", spt[l][:, :], sp_in[l], [], [CSTB])
    DMA("sp", wrt[:, :, :], wr_in, [], [CSTB])
    DMA("sp", brt[:, :], br_in, [], [CSTB])
    DMA("sp", pti[:, :], pt_in, [], [CSTB])
    for ti, (c0, n) in enumerate(MT):
        DMA("pool", xb[:, :, c0:c0 + n], x_in[:, :, c0:c0 + n], [], [XB[ti]])
    WSC = [B("wsc", l) for l in range(2)]
    for l in range(2):
        for src_, dst_ in ((win, win_b), (wgate, wgate_b), (wbrA, wbrA_b), (wbrB, wbrB_b), (wbrC, wbrC_b),
                           (wbrD, wbrD_b), (wout, wout_b)):
            DMA("pool", dst_[l], src_[l], [], [WSC[l]])
    KH = [B("Kh", 0), B("Kh", 1)]
    VH = [B("Vh", 0), B("Vh", 1)]

    def layernorm(tp, srcs, n, nfeat, gcols, bcols, outs32, outsb, silu=False):
        nch = len(srcs)
        ps1, pb1 = nextps()
        ps2, pb2 = nextps()
        for i, (s, sb) in enumerate(srcs):
            MM(ps1[:, 0:n], ones_f, s, i == 0, i == nch - 1, sb + [CSTB], [pb1])
        for i, (s, sb) in enumerate(srcs):
            t, tb = tp.next()
            ACT(t[:, 0:n], s, AF.Square, sb, [tb])
            MM(ps2[:, 0:n], ones_f, t[:, 0:n], i == 0, i == nch - 1, [tb, CSTB], [pb2])
        mean, mb, rstd, rb = tp.lnm, tp.lnmb, tp.lnr, tp.lnrb
        ACT(mean[:, 0:n], ps1[:, 0:n], AF.Copy, [pb1], [mb], scale=1.0 / nfeat)
        t, tb = tp.next()
        TT(t[:, 0:n], mean[:, 0:n], mean[:, 0:n], ALU.mult, [mb], [tb])
        STT(rstd[:, 0:n], ps2[:, 0:n], 1.0 / nfeat, t[:, 0:n], ALU.mult, ALU.subtract, [pb2, tb], [rb])
        TS(rstd[:, 0:n], rstd[:, 0:n], EPS, None, ALU.add, None, [rb], [rb])
        RCP(rstd[:, 0:n], rstd[:, 0:n], [rb], [rb])
        ACT(rstd[:, 0:n], rstd[:, 0:n], AF.Sqrt, [rb], [rb])
        for i, (s, sb) in enumerate(srcs):
            t, tb = tp.next()
            TT(t[:, 0:n], s, mean[:, 0:n], ALU.subtract, sb + [mb], [tb])
            TT(t[:, 0:n], t[:, 0:n], rstd[:, 0:n], ALU.mult, [tb, rb], [tb])
            if silu:
                ACT(t[:, 0:n], t[:, 0:n], AF.Identity, [tb, CSTB], [tb], bias=bcols[i], scale=gcols[i])
                t2, tb2 = tp.next()
                ACT(t2[:, 0:n], t[:, 0:n], AF.Sigmoid, [tb], [tb2])
                ob, obb = outsb[i]
                TT(ob, t[:, 0:n], t2[:, 0:n], ALU.mult, [tb, tb2], obb)
            else:
                o32, ob32 = outs32[i]
                ACT(o32, t[:, 0:n], AF.Identity, [tb, CSTB], ob32, bias=bcols[i], scale=gcols[i])
                if outsb is not None:
                    ob, obb = outsb[i]
                    CP(ob, o32, ob32, obb)

    def gelu_from(ps_ap, pbuf, out_ap, obufs, n, parts=128):
        x, xbf = MTP.next()
        t, tb = MTP.next()
        ACT(x[0:parts, 0:n], ps_ap, AF.Copy, [pbuf], [xbf])
        TT(t[0:parts, 0:n], x[0:parts, 0:n], x[0:parts, 0:n], ALU.mult, [xbf], [tb])
        TS(t[0:parts, 0:n], t[0:parts, 0:n], 0.044715, 1.0, ALU.mult, ALU.add, [tb], [tb])
        TT(t[0:parts, 0:n], t[0:parts, 0:n], x[0:parts, 0:n], ALU.mult, [tb, xbf], [tb])
        ACT(t[0:parts, 0:n], t[0:parts, 0:n], AF.Sigmoid, [tb], [tb], scale=1.5957691216057308)
        TT(out_ap, x[0:parts, 0:n], t[0:parts, 0:n], ALU.mult, [xbf, tb], obufs)

    def proj(ps_ap, pbuf, wv, wbuf, kcs, cols, c0, n):
        xbufs = tok_bufs(XB, c0, n)
        for kc in range(kcs):
            MM(ps_ap, wv[:, kc, cols[0]:cols[1]], xb[:, kc, c0:c0 + n], kc == 0, kc == kcs - 1,
               [wbuf] + xbufs, [pbuf])

    YA, YB, YC, YD = B("ya"), B("yb"), B("yc"), B("yd")
    MG32, MGB = B("mg32"), B("mgb")
    QTA = B("QTa")
    VTM = B("vtm")
    CBT, CDT = B("cbT"), B("cdT")
    CBP, CDP = B("cbprev"), B("cdprev")
    GV, VN = B("gv"), B("vn")
    SQ = B("sqkv")
    KMT = B("kmT")
    OUTB = B("outs")
    XT = B("xt")
    AWS = B("aws")

    def sample_attention(l):
        KC, SS, PS_, QBC, PVP, PVC, SDG, SSM, IDX = (B("Kc"), B("Ssc"), B("Psc"), B("qbc"), B("pvp"), B("pvc"),
                                                     B("sdg"), B("ssm"), B("idx"))
        for s_ in range(NS):
            psq, pbq = nextps()
            for h in range(4):
                TS(sdg[:, :], ident_f[0:64, 0:64], QTs[:, h, s_:s_ + 1], None, ALU.mult, None, [CSTB, SQ], [SDG])
                MM(psq[:, h * 64:(h + 1) * 64], ones_f[0:64, :], sdg[:, :], True, True, [CSTB, SDG], [pbq])
            CP(qbc[:, :], psq[:, 0:256], [pbq], [QBC])
            for ck in range(NCHK):
                TS(idx[:, 0:1], pti[:, s_:s_ + 1], NCHK, l * NPOOL * NCHK + ck, ALU.mult, ALU.add, [CSTB], [IDX])
                P.add("pool", lambda e: e.indirect_dma_start(
                    out=Kc[:, :], out_offset=None, in_=ck_in,
                    in_offset=bass.IndirectOffsetOnAxis(ap=idx[:, 0:1], axis=0)), [IDX], [KC], kind="d")
                kv = Kc[:, :].rearrange("p (a b) -> p a b", b=256)
                TT(kv, kv, qbc[:, :].rearrange("p (o b) -> p o b", o=1).to_broadcast([128, PCH, 256]), ALU.mult,
                   [KC, QBC], [KC])
                RED(Ssc[:, ck * PCH:(ck + 1) * PCH, :], Kc[:, :].rearrange("p (a h d) -> p a h d", h=4, d=64), ALU.add,
                    [KC], [SS])
                yield
            RED(ssm[:, 0:4], Ssc[:, :, :].rearrange("p a h -> p h a"), ALU.add, [SS], [SSM])
            psg, pbg = nextps()
            MM(psg[0:4, 0:64], ssm[:, 0:4], cs(C_PAIR, 64), True, True, [SSM, CSTB], [pbg])
            t, tb = MTP.next()
            CP(t[0:4, 0:64], psg[0:4, 0:64], [pbg], [tb])
            MAX8(m8s[0:4, :], t[0:4, 0:64], [tb], [B("m8s")])
            TS(t[0:4, 0:64], t[0:4, 0:64], m8s[0:4, 2:3], None, ALU.is_ge, None, [tb, B("m8s")], [tb])
            pst, pbt = nextps()
            TR(pst[0:64, 0:4], t[0:4, 0:64], ident_f[0:4, 0:4], [tb, CSTB], [pbt])
            t2, tb2 = MTP.next()
            CP(t2[0:64, 0:4], pst[0:64, 0:4], [pbt], [tb2])
            psm, pbm = nextps()
            MM(psm[:, 0:4], cs(C_PAIRT, 128, 64), t2[0:64, 0:4], True, True, [CSTB, tb2], [pbm])
            TS(ssm[:, 4:8], psm[:, 0:4], -1.0, -NEG, ALU.add, ALU.mult, [pbm], [SSM])
            for h in range(4):
                ACT(Psc[:, :, h], Ssc[:, :, h], AF.Exp, [SS, SSM], [PS_], bias=ssm[:, 4 + h:5 + h])
            RED(ssm[:, 8:12], Psc[:, :, :].rearrange("p a h -> p h a"), ALU.add, [PS_], [SSM])
            for ck in range(NCHK):
                TS(idx[:, 1:2], pti[:, s_:s_ + 1], NCHK, l * NPOOL * NCHK + ck, ALU.mult, ALU.add, [CSTB], [IDX])
                P.add("pool", lambda e: e.indirect_dma_start(
                    out=Kc[:, :], out_offset=None, in_=cv_in,
                    in_offset=bass.IndirectOffsetOnAxis(ap=idx[:, 1:2], axis=0)), [IDX], [KC], kind="d")
                kv4 = Kc[:, :].rearrange("p (a h d) -> p a h d", h=4, d=64)
                for h in range(4):
                    TT(kv4[:, :, h, :], kv4[:, :, h, :],
                       Psc[:, ck * PCH:(ck + 1) * PCH, h:h + 1].to_broadcast([128, PCH, 64]), ALU.mult, [KC, PS_], [KC])
                dst, dstb = (pvp, PVP) if ck == 0 else (pvc, PVC)
                RED(dst[:, :], Kc[:, :].rearrange("p (a c) -> p c a", c=256), ALU.add, [KC], [dstb])
                if ck > 0:
                    TT(pvp[:, :], pvp[:, :], pvc[:, :], ALU.add, [PVP, PVC], [PVP])
                yield
            psn, pbn = nextps()
            for h in range(4):
                MM(psn[0:64, h:h + 1], pvp[:, h * 64:(h + 1) * 64], ones_f[:, 0:1], True, True, [PVP, CSTB], [pbn])
            psd, pbd = nextps()
            MM(psd[0:64, 0:4], ones_f[:, 0:64], ssm[:, 8:12], True, True, [CSTB, SSM], [pbd])
            t, tb = MTP.next()
            TT(t[0:64, 0:4], QTs[:, :, s_], KTs[:, :, s_], ALU.mult, [SQ], [tb])
            pss, pbs = nextps()
            MM(pss[0:64, 0:4], ones_f[0:64, 0:64], t[0:64, 0:4], True, True, [CSTB, tb], [pbs])
            t2, tb2 = MTP.next()
            ACT(t2[0:64, 0:4], pss[0:64, 0:4], AF.Exp, [pbs], [tb2])
            t3, tb3 = MTP.next()
            TT(t3[0:64, 0:4], t2[0:64, 0:4], VTs[:, :, s_], ALU.mult, [tb2, SQ], [tb3])
            TT(t3[0:64, 0:4], t3[0:64, 0:4], psn[0:64, 0:4], ALU.add, [tb3, pbn], [tb3])
            TT(t2[0:64, 0:4], t2[0:64, 0:4], psd[0:64, 0:4], ALU.add, [tb2, pbd], [tb2])
            RCP(t2[0:64, 0:4], t2[0:64, 0:4], [tb2], [tb2])
            TT(ycs[:, :, s_], t3[0:64, 0:4], t2[0:64, 0:4], ALU.mult, [tb3, tb2], [B("ycs")])
            yield

    def layernorm_x(l, i, c0, n):
        bufs = tok_bufs(XF, c0, n)
        bufsb = tok_bufs(XB, c0, n)
        layernorm(FTP, [(xf[:, m, c0:c0 + n], bufs) for m in range(8)], n, 1024.0,
                  [spt[l][:, SP_LNG + i * 8 + m:SP_LNG + i * 8 + m + 1] for m in range(8)],
                  [spt[l][:, SP_LNB + i * 8 + m:SP_LNB + i * 8 + m + 1] for m in range(8)],
                  [(xf[:, m, c0:c0 + n], bufs) for m in range(8)],
                  [(xb[:, m, c0:c0 + n], bufsb) for m in range(8)])

    for l in range(DEPTH):
        sp = lambda off, n=1, p=128, l=l: spt[l][0:p, off:off + n]
        xsrc = x_in if l == 0 else xs2
        XSRC = [B("xsrc", l, i) for i in range(9)] if l == 0 else [B("xs2", i) for i in range(9)]
        XS1 = [B("xs1", i) for i in range(9)]
        XS2 = [B("xs2", i) for i in range(9)]
        CCB = B("cc", l)
        CG = B("ccg", l)
        DMA("pool", Kh[64:104, :], koh_in, [], KH)
        P.add("dve", lambda e: e.memset(Vh[:, :, 64:65], 1.0), [], VH)
        wC, wCb = loadw(win_b[l][:, :, 1280:2048], 128, 8 * 768, a=8, R=[WSC[l]])
        wCv = w3(wC, 8)
        for ti, (c0, n) in enumerate(MT):
            smp = ti == 8
            for h in range(4):
                ps, pb = nextps()
                proj(ps[0:64, 0:n], pb, wCv, wCb, 8, (256 + h * 64, 256 + h * 64 + 64), c0, n)
                t, tb = MTP.next()
                ACT(t[0:64, 0:n], ps[0:64, 0:n], AF.Copy, [pb], [tb])
                DMA("sp", o_k[l][:, h, c0:c0 + n], t[0:64, 0:n], [tb], [OUTB])
                if smp:
                    CP(KTs[:, h, :], t[0:64, 0:n], [tb], [SQ])
                else:
                    CP(kTb[:, h, 0:n], t[0:64, 0:n], [tb], [B("kTb")])
            if not smp:
                for h in range(4):
                    DMA("sp", cc_kt[l][h * 64:(h + 1) * 64, c0:c0 + n], kTb[:, h, 0:n], [B("kTb")], [CCB])
            for vc in range(2):
                ps, pb = nextps()
                proj(ps[:, 0:n], pb, wCv, wCb, 8, (512 + vc * 128, 512 + vc * 128 + 128), c0, n)
                t, tb = MTP.next()
                ACT(t[:, 0:n], ps[:, 0:n], AF.Copy, [pb], [tb])
                DMA("sp", o_v[l][:, vc, c0:c0 + n], t[:, 0:n], [tb], [OUTB])
                if not smp:
                    for c in range(2):
                        pt_, ptb = nextps()
                        TR(pt_[:, 0:128], t[:, c * 128:(c + 1) * 128], ident_f, [tb, CSTB], [ptb])
                        kt = ti * 2 + c
                        CP(vtm[:, 2 * vc:2 * vc + 2, kt, :],
                           pt_[:, 0:128].rearrange("p (a b) -> p a b", a=2), [ptb], [VTM],
                           eng="act" if c % 2 else "dve")
            if smp:
                for h in range(4):
                    ps, pb = nextps()
                    proj(ps[0:64, 0:n], pb, wCv, wCb, 8, (h * 64, h * 64 + 64), c0, n)
                    ACT(QTs[:, h, :], ps[0:64, 0:n], AF.Copy, [pb], [SQ], scale=0.125)
                    ps, pb = nextps()
                    proj(ps[0:64, 0:n], pb, wCv, wCb, 8, (512 + h * 64, 512 + h * 64 + 64), c0, n)
                    ACT(VTs[:, h, :], ps[0:64, 0:n], AF.Copy, [pb], [SQ])
        DMA("sp", cc_v[l], vtm.rearrange("p a b c -> p (a b c)"), [VTM], [CCB])
        wBt, wBtb = loadw(win_b[l][:, :, 768:1280], 128, 8 * 512, a=8, R=[WSC[l]])
        wBtv = w3(wBt, 8)
        wDt, wDtb = loadw(win_b[l][:, :, 2048:2560], 128, 8 * 512, a=8, R=[WSC[l]])
        wDtv = w3(wDt, 8)
        c0t, nt = NPR - 32, 32
        TL = B("tails")
        for cc in range(2):
            psc, pbc = nextps()
            proj(psc[:, 0:nt], pbc, wBtv, wBtb, 8, (cc * 128, cc * 128 + 128), c0t, nt)
            psx, pbx = nextps()
            proj(psx[:, 0:nt], pbx, wBtv, wBtb, 8, (256 + cc * 128, 256 + cc * 128 + 128), c0t, nt)
            t, tb = MTP.next()
            ACT(t[:, 0:nt], psc[:, 0:nt], AF.Copy, [pbc], [tb])
            TT(tails[:, cc * 32:cc * 32 + 2], t[:, 30:32], psx[:, 30:32], ALU.mult, [tb, pbx], [TL])
            psa, pba = nextps()
            proj(psa[:, 0:nt], pba, wDtv, wDtb, 8, (cc * 128, cc * 128 + 128), c0t, nt)
            psg, pbg = nextps()
            proj(psg[:, 0:nt], pbg, wDtv, wDtb, 8, (256 + cc * 128, 256 + cc * 128 + 128), c0t, nt)
            t, tb = MTP.next()
            ACT(t[:, 0:nt], psg[:, 0:nt], AF.Sigmoid, [pbg], [tb])
            TT(tails[:, cc * 32 + 2:cc * 32 + 32], t[:, 2:32], psa[:, 2:32], ALU.mult, [tb, pba], [TL])
        DMA("sp", cc_t[l], tails[:, :], [TL], [CCB])
        for src, dst in ((cc_kt[l], cc_ktg[l]), (cc_v[l], cc_vg[l]), (cc_t[l], cc_tg[l])):
            P.add("pool", lambda e, s=src, d=dst: e.collective_compute(
                "AllGather", ALU.bypass, replica_groups=GROUPS, ins=[s], outs=[d]), [CCB], [CG], kind="cc")
        TG = B("tailg")
        DMA("sp", tailg[:, :, :], cc_tg[l].rearrange("(r p) c -> p r c", p=128), [CG], [TG])
        for rk in range(4):
            selc = cst[:, C_SELR + rk:C_SELR + rk + 1]
            for cc in range(2):
                if rk == 0:
                    TS(cbprev[:, cc, :], tailg[:, rk, cc * 32:cc * 32 + 2], selc, None, ALU.mult, None,
                       [TG, CSTB], [CBP])
                    TS(cdprev[:, cc, :], tailg[:, rk, cc * 32 + 2:cc * 32 + 32], selc, None, ALU.mult, None,
                       [TG, CSTB], [CDP])
                else:
                    STT(cbprev[:, cc, :], tailg[:, rk, cc * 32:cc * 32 + 2], selc, cbprev[:, cc, :],
                        ALU.mult, ALU.add, [TG, CSTB, CBP], [CBP])
                    STT(cdprev[:, cc, :], tailg[:, rk, cc * 32 + 2:cc * 32 + 32], selc, cdprev[:, cc, :],
                        ALU.mult, ALU.add, [TG, CSTB, CDP], [CDP])

        def load_KV(h, half, l=l, CG=CG, CCB=CCB):
            ktg = cc_ktg[l].rearrange("(r hh d) t -> d r hh t", r=4, hh=4)
            r0 = 2 * half
            DMA("sp", Kh[0:64, r0 * 2048:(r0 + 2) * 2048].rearrange("p (r t) -> p r t", r=2),
                ktg[:, r0:r0 + 2, h, :], [CG], [KH[half]])
            for r_ in (r0, r0 + 1):
                DMA("sp", Vh[:, r_ * 16:(r_ + 1) * 16, 0:64],
                    cc_vg[l][r_ * 128:(r_ + 1) * 128, :].rearrange("p (hh k d) -> p hh k d", hh=4, k=16)[:, h, :, :],
                    [CG], [VH[half]])
            if half == 1:
                DMA("sp", Kh[0:64, 8192:10240], cc_kt[l][h * 64:(h + 1) * 64, :], [CCB], [KH[1]])
                DMA("sp", Vh[:, 64:80, 0:64],
                    cc_v[l].rearrange("p (hh k d) -> p hh k d", hh=4, k=16)[:, h, :, :], [CCB], [VH[1]])

        for h in range(4):
            load_KV(h, 0)
            load_KV(h, 1)
            t, tb = MTP.next()
            RED(t[0:64, 0:40], Kh[0:64, :].rearrange("p (b k) -> p b k", k=256), ALU.add, KH, [tb])
            CP(kmT[:, h, :], t[0:64, 0:40], [tb], [KMT])
        sa_gen = sample_attention(l)
        DMA("sp", awsf[:, :, :], awsT[l], [], [AWS])
        DMA("sp", absf[:, :, :], absr[l], [], [AWS])
        for h in range(4):
            TT(awsb[:, h, :], awsf[:, h, :], cs(C_TRI, 128), ALU.mult, [AWS, CSTB], [B("awsb")])

        for ti, (c0, n) in enumerate(MT):
            smp = ti == 8
            DMA("sp", xt[:, :, 0:n], xsrc[:, :, c0:c0 + n], [XSRC[ti]], [XT])
            wA, wAb = loadw(win_b[l][:, :, 0:512], 128, 8 * 512, a=8, R=[WSC[l]])
            wAv = w3(wA, 8)
            for h in range(4):
                ps, pb = nextps()
                proj(ps[0:64, 0:n], pb, wAv, wAb, 8, (h * 64, h * 64 + 64), c0, n)
                gelu_from(ps[0:64, 0:n], pb, ya[:, h, 0:n], [YA], n, parts=64)
            for vc in range(2):
                ps, pb = nextps()
                proj(ps[:, 0:n], pb, wAv, wAb, 8, (256 + vc * 128, 256 + vc * 128 + 128), c0, n)
                gelu_from(ps[:, 0:n], pb, gv[:, vc, 0:n], [GV], n)
            layernorm(MTP, [(gv[:, vc, 0:n], [GV]) for vc in range(2)], n, 256.0,
                      [sp(SP_ALNG + vc) for vc in range(2)], [sp(SP_ALNB + vc) for vc in range(2)],
                      [(vn[:, vc, 0:n], [VN]) for vc in range(2)], None)
            if not smp:
                for c in range(2):
                    for vc in range(2):
                        pt_, ptb = nextps()
                        TR(pt_[:, 0:128], vn[:, vc, c * 128:(c + 1) * 128], ident_f, [VN, CSTB], [ptb])
                        CP(vtA[:, vc * 128:(vc + 1) * 128], pt_[:, 0:128], [ptb], [B("vtA")],
                           eng="act" if vc else "dve")
                    for h in range(4):
                        ps, pb = nextps()
                        MM(ps[0:64, 0:128], vtA[:, h * 64:(h + 1) * 64], awsb[:, h, :], True, False,
                           [B("vtA"), B("awsb")], [pb])
                        MM(ps[0:64, 0:128], ones_f[0:1, 0:64], absf[0:1, h, :], False, True,
                           [CSTB, AWS], [pb])
                        TT(ya[:, h, c * 128:(c + 1) * 128], ya[:, h, c * 128:(c + 1) * 128], ps[0:64, 0:128],
                           ALU.mult, [YA, pb], [YA])
            else:
                DMA("sp", o_cv[l], vn[:, :, 0:NS], [VN], [OUTB])
                for h in range(4):
                    ps, pb = nextps()
                    MM(ps[0:64, 0:n], ident_f[:, (h % 2) * 64:(h % 2) * 64 + 64], vn[:, h // 2, 0:n], True, True,
                       [VN, CSTB], [pb])
                    t, tb = MTP.next()
                    TS(t[0:64, 0:n], ps[0:64, 0:n], sp(SP_AW00 + h, 1, 64), sp(SP_ABS0 + h, 1, 64),
                       ALU.mult, ALU.add, [pb, CSTB], [tb])
                    TT(ya[:, h, 0:n], ya[:, h, 0:n], t[0:64, 0:n], ALU.mult, [YA, tb], [YA])
            wB, wBb = loadw(win_b[l][:, :, 512:1280], 128, 8 * 768, a=8, R=[WSC[l]])
            wBv = w3(wB, 8)
            CBS, CDS, SM3 = B("cbs"), B("cds"), B("sm3")
            if smp:
                DMA("sp", cbs[:, :, :, 0:2], scb_in[l], [], [CBS])
            for cc in range(2):
                psc, pbc = nextps()
                proj(psc[:, 0:n], pbc, wBv, wBb, 8, (256 + cc * 128, 256 + cc * 128 + 128), c0, n)
                psx, pbx = nextps()
                proj(psx[:, 0:n], pbx, wBv, wBb, 8, (512 + cc * 128, 512 + cc * 128 + 128), c0, n)
                psb_, pbb = nextps()
                proj(psb_[:, 0:n], pbb, wBv, wBb, 8, (cc * 128, cc * 128 + 128), c0, n)
                t, tb = MTP.next()
                ACT(t[:, 0:n], psc[:, 0:n], AF.Copy, [pbc], [tb])
                a, ab = MTP.next()
                if not smp:
                    CP(cbT[:, cc, 0:2], cbprev[:, cc, :], [CBP], [CBT])
                    TT(cbT[:, cc, 2:2 + n], t[:, 0:n], psx[:, 0:n], ALU.mult, [tb, pbx], [CBT])
                    TS(a[:, 0:n], cbT[:, cc, 0:n], sp(SP_BCW + cc * 3), None, ALU.mult, None, [CBT, CSTB], [ab])
                    for k in (1, 2):
                        STT(a[:, 0:n], cbT[:, cc, k:k + n], sp(SP_BCW + cc * 3 + k), a[:, 0:n], ALU.mult, ALU.add,
                            [CBT, CSTB, ab], [ab])
                    TT(yb[:, cc, 0:n], a[:, 0:n], psb_[:, 0:n], ALU.mult, [ab, pbb], [YB])
                    CP(cbprev[:, cc, :], cbT[:, cc, n:n + 2], [CBT], [CBP])
                else:
                    TT(cbs[:, cc, :, 2], t[:, 0:n], psx[:, 0:n], ALU.mult, [tb, pbx], [CBS])
                    wv_ = spt[l][:, SP_BCW + cc * 3:SP_BCW + cc * 3 + 3]
                    TT(sm3[:, cc, :, 0:3], cbs[:, cc, :, :],
                       wv_.rearrange("p (o k) -> p o k", o=1).to_broadcast([128, NS, 3]),
                       ALU.mult, [CBS, CSTB], [SM3])
                    RED(a[:, 0:n], sm3[:, cc, :, 0:3], ALU.add, [SM3], [ab])
                    TT(yb[:, cc, 0:n], a[:, 0:n], psb_[:, 0:n], ALU.mult, [ab, pbb], [YB])
            if ti == 7:
                DMA("sp", o_cb[l], cbprev[:, :, :], [CBP], [OUTB])
            if smp:
                DMA("sp", o_cbs[l], cbs[:, :, :, 1:3], [CBS], [OUTB])
            wD, wDb = loadw(win_b[l][:, :, 2048:2560], 128, 8 * 512, a=8, R=[WSC[l]])
            wDv = w3(wD, 8)
            if smp:
                DMA("sp", cds[:, :, :, 0:30], scd_in[l], [], [CDS])
            dsrc = []
            for cc in range(2):
                psa, pba = nextps()
                proj(psa[:, 0:n], pba, wDv, wDb, 8, (cc * 128, cc * 128 + 128), c0, n)
                psg, pbg = nextps()
                proj(psg[:, 0:n], pbg, wDv, wDb, 8, (256 + cc * 128, 256 + cc * 128 + 128), c0, n)
                t, tb = MTP.next()
                ACT(t[:, 0:n], psg[:, 0:n], AF.Sigmoid, [pbg], [tb])
                a, ab = dacc[cc], B("dacc", cc)
                if not smp:
                    CP(cdT[:, cc, 0:30], cdprev[:, cc, :], [CDP], [CDT])
                    TT(cdT[:, cc, 30:30 + n], t[:, 0:n], psa[:, 0:n], ALU.mult, [tb, pba], [CDT])
                    TS(a[:, 0:n], cdT[:, cc, 0:n], sp(SP_DCW + cc * 31), sp(SP_DCB + cc), ALU.mult, ALU.add,
                       [CDT, CSTB], [ab])
                    for k in range(1, 31):
                        STT(a[:, 0:n], cdT[:, cc, k:k + n], sp(SP_DCW + cc * 31 + k), a[:, 0:n], ALU.mult, ALU.add,
                            [CDT, CSTB, ab], [ab])
                    CP(cdprev[:, cc, :], cdT[:, cc, n:n + 30], [CDT], [CDP])
                else:
                    TT(cds[:, cc, :, 30], t[:, 0:n], psa[:, 0:n], ALU.mult, [tb, pba], [CDS])
                    wv_ = spt[l][:, SP_DCW + cc * 31:SP_DCW + cc * 31 + 31]
                    TT(sm3[:, cc, :, :], cds[:, cc, :, :],
                       wv_.rearrange("p (o k) -> p o k", o=1).to_broadcast([128, NS, 31]),
                       ALU.mult, [CDS, CSTB], [SM3])
                    RED(a[:, 0:n], sm3[:, cc, :, :], ALU.add, [SM3], [ab])
                    TS(a[:, 0:n], a[:, 0:n], sp(SP_DCB + cc), None, ALU.add, None, [ab, CSTB], [ab])
                dsrc.append((a[:, 0:n], [ab]))
            layernorm(MTP, dsrc, n, 256.0, [sp(SP_DLNG + cc) for cc in range(2)],
                      [sp(SP_DLNB + cc) for cc in range(2)],
                      None, [(yd[:, cc, 0:n], [YD]) for cc in range(2)], silu=True)
            if ti == 7:
                DMA("sp", o_cd[l], cdprev[:, :, :], [CDP], [OUTB])
            if smp:
                DMA("sp", o_cds[l], cds[:, :, :, 1:31], [CDS], [OUTB])
            if not smp:
                for _ in range(9):
                    next(sa_gen, None)
                g = ti
                wq, wqb = loadw(win_b[l][:, :, 1280:1536], 128, 8 * 256, a=8, R=[WSC[l]])
                wqv = w3(wq, 8)
                for h in range(4):
                    ps, pb = nextps()
                    proj(ps[0:64, 0:n], pb, wqv, wqb, 8, (h * 64, h * 64 + 64), c0, n)
                    ACT(QTa[0:64, h, 0:n], ps[0:64, 0:n], AF.Copy, [pb], [QTA], scale=0.125)
                SM, SM2, M8 = B("small"), B("small2"), B("m8")
                PBv = cst[:, C_PB + g * 40:C_PB + g * 40 + 40]
                PVv = cst[:, C_PV + g * 40:C_PV + g * 40 + 40]
                POv = cst[:, C_PO + g * 40:C_PO + g * 40 + 40]
                bc3 = lambda v: v.rearrange("p (o b) -> p o b", o=1).to_broadcast([128, 4, 40])
                for c in range(2):
                    psg, pbg = nextps()
                    for h in range(4):
                        MM(psg[:, h * 40:h * 40 + 40], QTa[0:64, h, c * 128:(c + 1) * 128], kmT[:, h, :], True, True,
                           [QTA, KMT], [pbg])
                    TT(small[:, :, :], psg[:, 0:160].rearrange("p (h b) -> p h b", h=4), bc3(PBv), ALU.add,
                       [pbg, CSTB], [SM])
                    for h in range(4):
                        MAX8(m8[:, :], small[:, h, :], [SM], [M8])
                        TS(small2[:, h, 64:104], small[:, h, :], m8[:, 2:3], None, ALU.is_ge, None, [SM, M8], [SM2])
                    TT(small2[:, :, 64:104], small2[:, :, 64:104], bc3(PVv), ALU.mult, [SM2, CSTB], [SM2])
                    TT(small2[:, :, 64:104], small2[:, :, 64:104], bc3(POv), ALU.add, [SM2, CSTB], [SM2])
                    TS(small2[:, :, 64:104], small2[:, :, 64:104], -1.0, -NEG, ALU.add, ALU.mult, [SM2], [SM2])
                    for h in range(4):
                        pt_, ptb = nextps()
                        TR(pt_[0:104, 0:128], small2[:, h, :], ident_f, [SM2, CSTB], [ptb])
                        CP(QTa[64:104, h, c * 128:(c + 1) * 128], pt_[64:104, 0:128], [ptb], [QTA],
                           eng="act" if h % 2 else "dve")
                for h in range(4):
                    load_KV(h, 0)
                    load_KV(h, 1)
                    kts = list(range(64)) + [64 + j for j in range(2 * (g + 1))]
                    acc, accb = psum[6 + h % 2], psb[6 + h % 2]
                    pend = None

                    def emit_pv(i, kt, hf, pp, ppb, acc=acc, accb=accb, nk=len(kts)):
                        MM(acc[0:65, 0:256], Vh[:, kt, 0:65], pp[:, :], i == 0, i == nk - 1, [VH[hf], ppb], [accb])

                    for i, kt in enumerate(kts):
                        ps, pb = nextps()
                        own = kt >= 64 + 2 * g
                        hf = 0 if kt < 32 else 1
                        MM(ps[:, 0:256], Kh[0:104, kt * 128:(kt + 1) * 128], QTa[0:104, h, 0:256], True, not own,
                           [KH[hf], QTA], [pb])
                        if own:
                            j = kt - (64 + 2 * g)
                            MM(ps[:, 0:256], ident_b, cstb[:, CB_CAUS + j * 256:CB_CAUS + (j + 1) * 256], False, True,
                               [CSTB], [pb])
                        pp, ppb = ptile[i % 3], B("ptile", i % 3)
                        ACT(pp[:, :], ps[:, 0:256], AF.Exp, [pb], [ppb])
                        if pend is not None:
                            emit_pv(*pend)
                        pend = (i, kt, hf, pp, ppb)
                    emit_pv(*pend)
                    ACS = B("accS")
                    CP(accS[:, :], acc[0:65, 0:256], [accb], [ACS])
                    ps, pb = nextps()
                    MM(ps[0:64, 0:256], ones_f[64:65, 0:64], accS[64:65, :], True, True, [CSTB, ACS], [pb])
                    t, tb = MTP.next()
                    RCP(t[0:64, 0:256], ps[0:64, 0:256], [pb], [tb])
                    TT(yc[:, h, 0:256], accS[0:64, :], t[0:64, 0:256], ALU.mult, [ACS, tb], [YC])
            else:
                for _ in sa_gen:
                    pass
                CP(yc[:, :, 0:NS], ycs[:, :, :], [B("ycs")], [YC])
            if DEBUG and l == 0:
                DMA('sp', d_ya[:, :, c0:c0 + n], ya[:, :, 0:n], [YA], [OUTB])
                DMA('sp', d_yb[:, :, c0:c0 + n], yb[:, :, 0:n], [YB], [OUTB])
                DMA('sp', d_yc[:, :, c0:c0 + n], yc[:, :, 0:n], [YC], [OUTB])
                DMA('sp', d_yd[:, :, c0:c0 + n], yd[:, :, 0:n], [YD], [OUTB])
            ysrc = [(ya, YA, 64, 4, wbrA_b), (yb, YB, 128, 2, wbrB_b), (yc, YC, 64, 4, wbrC_b), (yd, YD, 128, 2, wbrD_b)]
            for j, (yt, ybuf, kp, nk, wsrc) in enumerate(ysrc):
                for half in range(2):
                    wb_, wbb = loadw(wsrc[l][:, :, half * 512:(half + 1) * 512], kp, nk * 512, a=nk, R=[WSC[l]])
                    wbv = w3(wb_, nk)
                    wg_, wgb = loadw(wgate_b[l][:, :, j * 1024 + half * 512:j * 1024 + (half + 1) * 512], 128, 8 * 512, a=8, R=[WSC[l]])
                    wgv = w3(wg_, 8)
                    for mm_ in range(4):
                        m = half * 4 + mm_
                        psb_, pbb = nextps()
                        for k in range(nk):
                            MM(psb_[:, 0:n], wbv[:, k, mm_ * 128:(mm_ + 1) * 128], yt[:, k, 0:n], k == 0, k == nk - 1,
                               [wbb, ybuf], [pbb])
                        psg, pbg = nextps()
                        proj(psg[:, 0:n], pbg, wgv, wgb, 8, (mm_ * 128, mm_ * 128 + 128), c0, n)
                        t, tb = MTP.next()
                        ACT(t[:, 0:n], psg[:, 0:n], AF.Sigmoid, [pbg, CSTB], [tb], bias=sp(SP_BGATE + j * 8 + m))
                        if j == 0:
                            TT(mg32[:, m, 0:n], t[:, 0:n], psb_[:, 0:n], ALU.mult, [tb, pbb], [MG32])
                        else:
                            TT(t[:, 0:n], t[:, 0:n], psb_[:, 0:n], ALU.mult, [tb, pbb], [tb])
                            if j < 3:
                                TT(mg32[:, m, 0:n], mg32[:, m, 0:n], t[:, 0:n], ALU.add, [MG32, tb], [MG32])
                            else:
                                TT(mgb[:, m, 0:n], mg32[:, m, 0:n], t[:, 0:n], ALU.add, [MG32, tb], [MGB])
            if DEBUG and l == 0:
                DMA('sp', d_mg[:, :, c0:c0 + n], mgb[:, :, 0:n], [MGB], [OUTB])
            for half in range(2):
                wo_, wob = loadw(wout_b[l][:, :, half * 512:(half + 1) * 512], 128, 8 * 512, a=8, R=[WSC[l]])
                wov = w3(wo_, 8)
                for mm_ in range(4):
                    m = half * 4 + mm_
                    ps, pb = nextps()
                    for kc in range(8):
                        MM(ps[:, 0:n], wov[:, kc, mm_ * 128:(mm_ + 1) * 128], mgb[:, kc, 0:n], kc == 0, kc == 7,
                           [wob, MGB], [pb])
                    STT(xt[:, m, 0:n], xt[:, m, 0:n], ALPHA, ps[:, 0:n], ALU.mult, ALU.add, [XT, pb], [XT])
            layernorm(MTP, [(xt[:, m, 0:n], [XT]) for m in range(8)], n, 1024.0,
                      [sp(SP_LNG + m) for m in range(8)], [sp(SP_LNB + m) for m in range(8)],
                      [(xt[:, m, 0:n], [XT]) for m in range(8)],
                      [(xb[:, m, c0:c0 + n], [XB[ti]]) for m in range(8)])
            DMA("sp", xs1[:, :, c0:c0 + n], xt[:, :, 0:n], [XT], [XS1[ti]])
            if DEBUG and l == 0:
                DMA('sp', d_ln1[:, :, c0:c0 + n], xt[:, :, 0:n], [XT], [OUTB])
        FENCE()
        for ti, (c0, n) in enumerate(MT):
            DMA("sp", xf[:, :, c0:c0 + n], xs1[:, :, c0:c0 + n], [XS1[ti]], [XF[ti]])
        HB = [B("hb", i) for i in range(4)]
        SG = [B("sg", i) for i in range(3)]
        CMB = [B("cmb", i) for i in range(1)]
        CMT = B("cmbT")
        hrr = [0]
        moe = l % 2 == 1
        if moe:
            LG, FM8, FSC, EX8 = B("lg"), B("fm8"), B("fsc"), B("ex8")
            chunks = [(c * 128, 128) for c in range(16)] + [(NPR, NS)]
            for (t0, tn) in chunks:
                xfb = tok_bufs(XF, t0, tn)
                ps, pb = nextps()
                for kc in range(8):
                    MM(ps[0:tn, 0:8], xf[:, kc, t0:t0 + tn], wrt[:, kc, :], kc == 0, False, xfb + [CSTB], [pb])
                MM(ps[0:tn, 0:8], ones_f[0:1, 0:tn], brt[0:1, :], False, True, [CSTB], [pb])
                CP(lg[0:tn, :], ps[0:tn, 0:8], [pb], [LG])
                MAX8(fm8[0:tn, :], lg[0:tn, :], [LG], [FM8])
                TS(fsc[0:tn, 0:1], fm8[0:tn, 0:1], -1.0, None, ALU.mult, None, [FM8], [FSC])
                ACT(ex8[0:tn, :], lg[0:tn, :], AF.Exp, [LG, FSC], [EX8], bias=fsc[0:tn, 0:1])
                TS(lg[0:tn, :], lg[0:tn, :], fm8[0:tn, 1:2], None, ALU.is_ge, None, [LG, FM8], [LG])
                TT(ex8[0:tn, :], ex8[0:tn, :], lg[0:tn, :], ALU.mult, [EX8, LG], [EX8])
                RED(fsc[0:tn, 1:2], ex8[0:tn, :], ALU.add, [EX8], [FSC])
                RCP(fsc[0:tn, 2:3], fsc[0:tn, 1:2], [FSC], [FSC])
                TS(ex8[0:tn, :], ex8[0:tn, :], fsc[0:tn, 2:3], None, ALU.mult, None, [EX8, FSC], [EX8])
                pt_, ptb = nextps()
                TR(pt_[0:8, 0:tn], ex8[0:tn, :], ident_f[0:tn, 0:tn], [EX8, CSTB], [ptb])
                CP(cmbT[:, t0:t0 + tn], pt_[0:8, 0:tn], [ptb], [CMT])
        for ti, (c0, n) in enumerate(MT):
            for m in range(8):
                ACT(xf[:, m, c0:c0 + n], xf[:, m, c0:c0 + n], AF.Copy, [XF[ti]], [XF[ti]], scale=ALPHA)
        ngroups = 8 * NMOE_G if moe else NFFN_G
        wsrc = moew if moe else ffnw
        psmod[0] = 8
        wcur = {}

        def emit_gu(gi, c0, n):
            wgv, wuv, wdv, wfb, e_ = wcur[gi]
            hs = []
            for hc in range(2):
                psg, pbg = nextps()
                proj(psg[:, 0:n], pbg, wgv, wfb, 8, (hc * 128, hc * 128 + 128), c0, n)
                psu, pbu = nextps()
                proj(psu[:, 0:n], pbu, wuv, wfb, 8, (hc * 128, hc * 128 + 128), c0, n)
                si = hrr[0] % 3
                hi = hrr[0] % 4
                hrr[0] += 1
                ACT(sgt[si][:, 0:n], psg[:, 0:n], AF.Sigmoid, [pbg], [SG[si]])
                TT(sgt[si][:, 0:n], sgt[si][:, 0:n], psg[:, 0:n], ALU.mult, [SG[si], pbg], [SG[si]])
                if moe:
                    TT(sgt[si][:, 0:n], sgt[si][:, 0:n], cmbbc[0][:, c0:c0 + n], ALU.mult,
                       [SG[si], CMB[0]], [SG[si]])
                TT(hb[hi][:, 0:n], sgt[si][:, 0:n], psu[:, 0:n], ALU.mult, [SG[si], pbu], [HB[hi]])
                hs.append((hb[hi], HB[hi]))
            return hs

        def emit_down(gi, c0, n, hs):
            wgv, wuv, wdv, wfb, e_ = wcur[gi]
            xfb = tok_bufs(XF, c0, n)
            for m in range(8):
                ps, pb = nextps()
                for hc in range(2):
                    MM(ps[:, 0:n], wdv[:, hc, m * 128:(m + 1) * 128], hs[hc][0][:, 0:n], hc == 0, hc == 1,
                       [wfb, hs[hc][1]], [pb])
                TT(xf[:, m, c0:c0 + n], xf[:, m, c0:c0 + n], ps[:, 0:n], ALU.add, xfb + [pb], xfb)

        prev = None
        for gi in range(ngroups):
            e_ = gi // NMOE_G
            if moe and gi % NMOE_G == 0:
                for (c0, n) in FT:
                    ps, pb = nextps()
                    MM(ps[:, 0:n], cst[0:8, C_SEL8 + e_ * 128:C_SEL8 + (e_ + 1) * 128], cmbT[0:8, c0:c0 + n], True, True,
                       [CSTB, CMT], [pb])
                    CP(cmbbc[0][:, c0:c0 + n], ps[:, 0:n], [pb], [CMB[0]], eng="act")
            wf_, wfb = loadw(wsrc[gi], 128, 6144)
            wcur[gi] = (w3(wf_[:, 0:2048], 8), w3(wf_[:, 2048:4096], 8), w3(wf_[:, 4096:6144], 2), wfb, e_)
            for (c0, n) in FT:
                hs = emit_gu(gi, c0, n)
                if prev is not None:
                    emit_down(*prev)
                prev = (gi, c0, n, hs)
        emit_down(*prev)
        for (c0, n) in FT:
            layernorm_x(l, 1, c0, n)
            if DEBUG and l == 0:
                DMA('sp', d_ln2[:, :, c0:c0 + n], xf[:, :, c0:c0 + n], tok_bufs(XF, c0, n), [OUTB])
        PTB = B("pTb")
        for (c0, n) in FT:
            DMA("pool", pTb[:, :, c0:c0 + n], p_in[l][:, :, c0:c0 + n], [], [PTB])
        wppb = B("wppt")
        DMA("pool", wppt[:, :], plep[l].rearrange("p a b -> p (a b)"), [], [wppb])
        wppv = w3(wppt[:, :], 2)
        for (c0, n) in FT:
            xfb = tok_bufs(XF, c0, n)
            for half in range(2):
                wpg_, wpgb = loadw(pleg[l][:, :, half * 512:(half + 1) * 512], 128, 8 * 512, a=8)
                wpgv = w3(wpg_, 8)
                for mm_ in range(4):
                    m = half * 4 + mm_
                    ps1, pb1 = nextps()
                    proj(ps1[:, 0:n], pb1, wpgv, wpgb, 8, (mm_ * 128, mm_ * 128 + 128), c0, n)
                    ps2, pb2 = nextps()
                    for c in range(2):
                        MM(ps2[:, 0:n], wppv[:, c, m * 128:(m + 1) * 128], pTb[:, c, c0:c0 + n], c == 0, c == 1,
                           [wppb, PTB], [pb2])
                    si = hrr[0] % 3
                    hrr[0] += 1
                    ACT(sgt[si][:, 0:n], ps1[:, 0:n], AF.Sigmoid, [pb1], [SG[si]])
                    TT(sgt[si][:, 0:n], sgt[si][:, 0:n], ps2[:, 0:n], ALU.mult, [SG[si], pb2], [SG[si]])
                    STT(xf[:, m, c0:c0 + n], xf[:, m, c0:c0 + n], ALPHA, sgt[si][:, 0:n], ALU.mult, ALU.add,
                        xfb + [SG[si]], xfb)
            layernorm_x(l, 2, c0, n)
        if DEBUG and l == 0:
            for ti, (c0, n) in enumerate(MT):
                DMA('sp', d_ln3[:, :, c0:c0 + n], xf[:, :, c0:c0 + n], [XF[ti]], [OUTB])
        psmod[0] = 6
        dst = o_y if l == DEPTH - 1 else xs2
        for ti, (c0, n) in enumerate(MT):
            DMA("sp", dst[:, :, c0:c0 + n], xf[:, :, c0:c0 + n], [XF[ti]], [OUTB if l == DEPTH - 1 else XS2[ti]])
        FENCE()

    emit(nc, P)
    return nc, len(P.ops)


def _fm(a, nch):
    return np.ascontiguousarray(a.reshape(nch, 128, -1).transpose(1, 0, 2))


_CACHE = {}


def _constants():
    c = np.zeros((128, NCST), np.float32)
    c[:, C_ID:C_ID + 128] = np.eye(128, dtype=np.float32)
    c[:, C_ONE:C_ONE + 128] = 1.0
    s = np.arange(128)
    c[:, C_TRI:C_TRI + 128] = (s[:, None] <= s[None, :]).astype(np.float32)
    pair = (s[:, None] // 2 == np.arange(64)[None, :]).astype(np.float32)
    c[:, C_PAIR:C_PAIR + 64] = pair
    c[0:64, C_PAIRT:C_PAIRT + 128] = pair.T
    for e in range(8):
        c[e, C_SEL8 + e * 128:C_SEL8 + (e + 1) * 128] = 1.0
    cb = np.zeros((128, NCSTB), np.float32)
    cb[:, CB_ID:CB_ID + 128] = np.eye(128, dtype=np.float32)
    q = np.arange(256)
    for j in range(2):
        key = j * 128 + s
        cb[:, CB_CAUS + j * 256:CB_CAUS + (j + 1) * 256] = np.where(key[:, None] <= q[None, :], 0.0, NEG)
    koh = np.zeros((40, 10240), np.float32)
    for b in range(40):
        koh[b, b * 256:(b + 1) * 256] = 1.0
    return c, cb, koh


def kernel(x_prompt, x_sample, p_prompt, p_sample, cache_k, cache_v, state_conv_b, state_conv_d, page_table,
           w_in, w_gate, b_gate, a_ln_g, a_ln_b, a_w_s, a_b_s, b_conv_w, d_conv_w, d_conv_b, d_ln_g, d_ln_b,
           w_branch, w_out, ln_g, ln_b, ffn_w_gate, ffn_w_up, ffn_w_down, moe_w_router, moe_b_router,
           moe_w_gate, moe_w_up, moe_w_down, ple_w_gate, ple_w_proj):
    f = lambda a: np.asarray(a, dtype=np.float32)
    x_prompt, x_sample, p_prompt, p_sample = f(x_prompt), f(x_sample), f(p_prompt), f(p_sample)
    cache_k, cache_v = f(cache_k), f(cache_v)
    if "nc" not in _CACHE:
        _CACHE["nc"] = build_program()
    nc, _ = _CACHE["nc"]

    sh = {}
    sh["win"] = np.stack([_fm(f(w_in[l]), 8) for l in range(2)])
    sh["wgate"] = np.stack([_fm(f(w_gate[l]), 8) for l in range(2)])
    wbr = f(w_branch)
    sh["wbrA"] = np.ascontiguousarray(wbr[:, 0].reshape(2, 4, 64, 1024).transpose(0, 2, 1, 3))
    sh["wbrB"] = np.stack([_fm(wbr[l, 1], 2) for l in range(2)])
    sh["wbrC"] = np.ascontiguousarray(wbr[:, 2].reshape(2, 4, 64, 1024).transpose(0, 2, 1, 3))
    sh["wbrD"] = np.stack([_fm(wbr[l, 3], 2) for l in range(2)])
    sh["wout"] = np.stack([_fm(f(w_out[l]), 8) for l in range(2)])
    aws = f(a_w_s)
    sh["awsT"] = np.ascontiguousarray(aws.transpose(0, 3, 1, 2))
    sh["absr"] = np.ascontiguousarray(f(a_b_s).reshape(2, 1, 4, 128))
    spa = np.zeros((2, 128, NSP), np.float32)
    for l in range(2):
        spa[l, :, SP_BGATE:SP_BGATE + 32] = f(b_gate[l]).reshape(32, 128).T
        spa[l, :, SP_LNG:SP_LNG + 24] = f(ln_g[l]).reshape(24, 128).T
        spa[l, :, SP_LNB:SP_LNB + 24] = f(ln_b[l]).reshape(24, 128).T
        spa[l, :, SP_ALNG:SP_ALNG + 2] = f(a_ln_g[l]).reshape(2, 128).T
        spa[l, :, SP_ALNB:SP_ALNB + 2] = f(a_ln_b[l]).reshape(2, 128).T
        spa[l, :, SP_DLNG:SP_DLNG + 2] = f(d_ln_g[l]).reshape(2, 128).T
        spa[l, :, SP_DLNB:SP_DLNB + 2] = f(d_ln_b[l]).reshape(2, 128).T
        spa[l, :, SP_DCB:SP_DCB + 2] = f(d_conv_b[l]).reshape(2, 128).T
        spa[l, :, SP_BCW:SP_BCW + 6] = f(b_conv_w[l]).reshape(3, 2, 128).transpose(2, 1, 0).reshape(128, 6)
        spa[l, :, SP_DCW:SP_DCW + 62] = f(d_conv_w[l]).reshape(31, 2, 128).transpose(2, 1, 0).reshape(128, 62)
        spa[l, :, SP_AW00:SP_AW00 + 4] = aws[l, :, 0, 0][None, :]
        spa[l, :, SP_ABS0:SP_ABS0 + 4] = f(a_b_s[l])[:, 0][None, :]
    sh["sp_in"] = spa

    def pack_ffn(wg, wu, wd, ng):
        H = wg.shape[1]
        g = _fm(wg, 8).reshape(128, 8, ng, 256).transpose(2, 0, 1, 3).reshape(ng, 128, 2048)
        u = _fm(wu, 8).reshape(128, 8, ng, 256).transpose(2, 0, 1, 3).reshape(ng, 128, 2048)
        d = wd.reshape(ng, 2, 128, 1024).transpose(0, 2, 1, 3).reshape(ng, 128, 2048)
        return np.ascontiguousarray(np.concatenate([g, u, d], axis=2))

    sh["ffnw"] = pack_ffn(f(ffn_w_gate[0]), f(ffn_w_up[0]), f(ffn_w_down[0]), NFFN_G)
    sh["moew"] = np.concatenate([pack_ffn(f(moe_w_gate[0, e]), f(moe_w_up[0, e]), f(moe_w_down[0, e]), NMOE_G)
                                 for e in range(8)], axis=0)
    sh["wr_in"] = _fm(f(moe_w_router[0]), 8)
    sh["br_in"] = f(moe_b_router).reshape(1, 8)
    sh["pleg"] = np.stack([_fm(f(ple_w_gate[l]), 8) for l in range(2)])
    sh["plep"] = np.stack([_fm(f(ple_w_proj[l]), 2) for l in range(2)])
    cst, cstb, koh = _constants()
    sh["cstb_in"] = cstb
    sh["koh_in"] = koh
    sh["ck_in"] = cache_k.reshape(2 * NPOOL * NCHK, PCH * 256)
    sh["cv_in"] = cache_v.reshape(2 * NPOOL * NCHK, PCH * 256)
    scb = f(state_conv_b)
    scd = f(state_conv_d)
    pt = np.asarray(page_table).astype(np.int32)

    in_maps = []
    for c in range(8):
        b, r = c // 4, c % 4
        m = dict(sh)
        xs = np.concatenate([x_prompt[b, r * NPR:(r + 1) * NPR], x_sample[4 * c:4 * c + 4, 0]], axis=0)
        m["x_in"] = _fm(np.ascontiguousarray(xs.T), 8)
        ps_ = [np.concatenate([p_prompt[l, b, r * NPR:(r + 1) * NPR], p_sample[l, 4 * c:4 * c + 4, 0]], axis=0)
               for l in range(2)]
        m["p_in"] = np.stack([_fm(np.ascontiguousarray(p.T), 2) for p in ps_])
        m["scb_in"] = np.ascontiguousarray(scb[:, 4 * c:4 * c + 4].reshape(2, NS, 2, 2, 128).transpose(0, 4, 3, 1, 2))
        m["scd_in"] = np.ascontiguousarray(scd[:, 4 * c:4 * c + 4].reshape(2, NS, 30, 2, 128).transpose(0, 4, 3, 1, 2))
        m["pt_in"] = np.ascontiguousarray(pt[4 * c:4 * c + 4].T)
        cc_ = cst.copy()
        for g in range(8):
            pb = np.full(40, -1e6, np.float32)
            pv = np.zeros(40, np.float32)
            po = np.zeros(40, np.float32)
            pb[0:8 * r] = 0.0
            pv[0:8 * r] = 1.0
            pb[32:32 + g] = 0.0
            pv[32:32 + g] = 1.0
            po[32 + g] = 1.0
            cc_[:, C_PB + g * 40:C_PB + (g + 1) * 40] = pb[None, :]
            cc_[:, C_PV + g * 40:C_PV + (g + 1) * 40] = pv[None, :]
            cc_[:, C_PO + g * 40:C_PO + (g + 1) * 40] = po[None, :]
        if r > 0:
            cc_[:, C_SELR + r - 1] = 1.0
        m["cst_in"] = cc_
        in_maps.append(m)

    res = run_bass_kernel_spmd(nc, in_maps, core_ids=list(range(8)))
    R = res.results
    if DEBUG:
        _CACHE['dbg'] = {k: np.asarray(v).astype(np.float32) for k, v in R[0].items() if k.startswith('d_')}

    y_prompt = np.zeros((2, 8192, 1024), np.float32)
    y_sample = np.zeros((32, 1, 1024), np.float32)
    k_prompt = np.zeros((2, 2, 8192, 4, 64), np.float32)
    v_prompt = np.zeros((2, 2, 8192, 4, 64), np.float32)
    k_sample = np.zeros((2, 32, 1, 4, 64), np.float32)
    v_sample = np.zeros((2, 32, 1, 4, 64), np.float32)
    conv_b_prompt = np.zeros((2, 2, 2, 256), np.float32)
    conv_b_sample = np.zeros((2, 32, 2, 256), np.float32)
    conv_d_prompt = np.zeros((2, 2, 30, 256), np.float32)
    conv_d_sample = np.zeros((2, 32, 30, 256), np.float32)
    chunk_v_sample = np.zeros((2, 32, 1, 256), np.float32)
    for c in range(8):
        b, r = c // 4, c % 4
        o = R[c]
        y = np.asarray(o["o_y"]).transpose(2, 1, 0).reshape(T, 1024)
        y_prompt[b, r * NPR:(r + 1) * NPR] = y[:NPR]
        y_sample[4 * c:4 * c + 4, 0] = y[NPR:]
        ok = np.asarray(o["o_k"]).transpose(0, 3, 2, 1)
        k_prompt[:, b, r * NPR:(r + 1) * NPR] = ok[:, :NPR]
        k_sample[:, 4 * c:4 * c + 4, 0] = ok[:, NPR:]
        ov = np.asarray(o["o_v"]).transpose(0, 3, 2, 1).reshape(2, T, 4, 64)
        v_prompt[:, b, r * NPR:(r + 1) * NPR] = ov[:, :NPR]
        v_sample[:, 4 * c:4 * c + 4, 0] = ov[:, NPR:]
        if r == 3:
            conv_b_prompt[:, b] = np.asarray(o["o_cb"]).transpose(0, 3, 2, 1).reshape(2, 2, 256)
            conv_d_prompt[:, b] = np.asarray(o["o_cd"]).transpose(0, 3, 2, 1).reshape(2, 30, 256)
        conv_b_sample[:, 4 * c:4 * c + 4] = np.asarray(o["o_cbs"]).transpose(0, 3, 4, 2, 1).reshape(2, NS, 2, 256)
        conv_d_sample[:, 4 * c:4 * c + 4] = np.asarray(o["o_cds"]).transpose(0, 3, 4, 2, 1).reshape(2, NS, 30, 256)
        chunk_v_sample[:, 4 * c:4 * c + 4, 0] = np.asarray(o["o_cv"]).transpose(0, 3, 2, 1).reshape(2, NS, 256)
    return (y_prompt, y_sample, k_prompt, v_prompt, k_sample, v_sample, conv_b_prompt, conv_b_sample,
            conv_d_prompt, conv_d_sample, chunk_v_sample)
```

```python
import numpy as np
import concourse.bass as bass
import concourse.mybir as mybir
from concourse.bass_utils import run_bass_kernel_spmd

F32 = mybir.dt.float32
BF16 = mybir.dt.bfloat16
I32 = mybir.dt.int32
AF = mybir.ActivationFunctionType
ALU = mybir.AluOpType
AX = mybir.AxisListType

NPR = 2048
NS = 4
T = NPR + NS
MT = [(i * 256, 256) for i in range(8)] + [(NPR, NS)]
FT = [(0, 512), (512, 512), (1024, 512), (1536, 512), (2048, 4)]
DEPTH = 2
ALPHA = (2 * DEPTH) ** 0.25
EPS = 1e-5
NEG = -30000.0
NPOOL = 5120
GROUPS = [[0, 1, 2, 3], [4, 5, 6, 7]]
NFFN_G = 11
NMOE_G = 14
PCH = 16
import os
DEBUG = bool(os.environ.get('KDBG'))
NCHK = 128 // PCH

SP_BGATE = 0
SP_LNG = 32
SP_LNB = 56
SP_ALNG = 80
SP_ALNB = 82
SP_DLNG = 84
SP_DLNB = 86
SP_DCB = 88
SP_BCW = 90
SP_DCW = 96
SP_AW00 = 158
SP_ABS0 = 162
NSP = 166
C_ID = 0
C_ONE = 128
C_TRI = 256
C_PAIR = 384
C_PAIRT = 448
C_SEL8 = 576
C_PB = 1600
C_PV = 1920
C_PO = 2240
C_SELR = 2560
NCST = 2564
CB_ID = 0
CB_CAUS = 128
NCSTB = 640


class Buf:
    __slots__ = ("w", "r", "name")

    def __init__(self, name=""):
        self.w = None
        self.r = {}
        self.name = name


class Op:
    __slots__ = ("eng", "fn", "deps", "kind", "sem", "val", "signal", "idx")


class Prog:
    def __init__(self):
        self.ops = []
        self.bufs = {}
        self.always = []

    def B(self, *key):
        b = self.bufs.get(key)
        if b is None:
            b = self.bufs[key] = Buf(str(key))
        return b

    def add(self, eng, fn, reads=(), writes=(), kind="c"):
        op = Op()
        op.eng, op.fn, op.kind = eng, fn, kind
        op.idx = len(self.ops)
        op.signal = False
        op.sem = None
        op.val = 0
        deps = {}
        reads = list(reads) + [b for b in self.always if b not in writes]
        for b in reads:
            if b.w is not None:
                deps[b.w.idx] = b.w
        for b in writes:
            for o in b.r.values():
                deps[o.idx] = o
            if b.w is not None:
                deps[b.w.idx] = b.w
        rkey = eng if kind == "c" else ("d", op.idx)
        for b in reads:
            b.r[rkey] = op
        for b in writes:
            b.w = op
            b.r = {}
        deps.pop(op.idx, None)
        dl = []
        for o in deps.values():
            if o.kind == "c" and kind == "c" and o.eng == "pe" and eng == "pe":
                continue
            dl.append(o)
        op.deps = dl
        self.ops.append(op)
        return op


def emit(nc, P):
    ops = P.ops
    for op in ops:
        for d in op.deps:
            d.signal = True
    NRING = 12
    csem = {e: nc.alloc_semaphore(name=f"c_{e}") for e in ("pe", "act", "dve", "pool")}
    rings = {q: [nc.alloc_semaphore(name=f"d_{q}{i}") for i in range(NRING)] for q in ("sp", "pool")}
    ccsem = nc.alloc_semaphore(name="ccs")
    ccount = {e: 0 for e in csem}
    rcount = {}
    rlast = {}
    rpos = {"sp": 0, "pool": 0}
    cccount = 0
    finals = {}
    for op in ops:
        if op.kind == "c":
            if op.signal:
                ccount[op.eng] += 1
                op.sem = csem[op.eng]
                op.val = ccount[op.eng]
        elif op.kind == "d":
            s = rings[op.eng][rpos[op.eng] % NRING]
            rpos[op.eng] += 1
            key = id(s)
            prev = rlast.get(key)
            if prev is not None:
                op.deps.append(prev)
            rcount[key] = rcount.get(key, 0) + 1
            rlast[key] = op
            op.sem = s
            op.val = 16 * rcount[key]
            finals[key] = (s, op.val)
        else:
            cccount += 1
            op.sem = ccsem
            op.val = cccount
            finals[id(ccsem)] = (ccsem, cccount)
    assert max(ccount.values()) < 60000, ccount
    queues = {"pe": [], "act": [], "dve": [], "pool": [], "sp": []}
    for op in ops:
        queues[op.eng].append(op)

    def run(eng_name, e):
        waited = {}
        for op in queues[eng_name]:
            need = {}
            for d in op.deps:
                k = id(d.sem)
                if d.val > need.get(k, (None, 0))[1]:
                    need[k] = (d.sem, d.val)
            for k, (s, v) in need.items():
                if waited.get(k, 0) >= v:
                    continue
                e.wait_ge(s, v)
                waited[k] = v
            ins = op.fn(e)
            if op.kind == "c":
                if op.signal:
                    ins.then_inc(op.sem, 1)
            elif op.kind == "d":
                ins.then_inc(op.sem, 16)
            else:
                ins.then_inc(op.sem, 1)
        if eng_name == "sp":
            for k, (s, v) in finals.items():
                e.wait_ge(s, v)

    with nc.Block() as block:
        @block.tensor
        def _(e):
            run("pe", e)

        @block.scalar
        def _(e):
            run("act", e)

        @block.vector
        def _(e):
            run("dve", e)

        @block.gpsimd
        def _(e):
            run("pool", e)

        @block.sync
        def _(e):
            run("sp", e)


def build_program():
    nc = bass.Bass("TRN2", target_bir_lowering=False)
    P = Prog()
    B = P.B

    def din(name, shape, dt=F32):
        return nc.dram_tensor(name, list(shape), dt, kind="ExternalInput").ap()

    def dout(name, shape, dt=F32):
        return nc.dram_tensor(name, list(shape), dt, kind="ExternalOutput").ap()

    def dint(name, shape, dt):
        return nc.dram_tensor(name, list(shape), dt, kind="Internal").ap()

    x_in = din("x_in", [128, 8, T])
    p_in = din("p_in", [2, 128, 2, T])
    scb_in = din("scb_in", [2, 128, 2, NS, 2])
    scd_in = din("scd_in", [2, 128, 2, NS, 30])
    pt_in = din("pt_in", [128, NS], I32)
    ck_in = din("ck_in", [2 * NPOOL * NCHK, PCH * 256])
    cv_in = din("cv_in", [2 * NPOOL * NCHK, PCH * 256])
    win = din("win", [2, 128, 8, 2560])
    wgate = din("wgate", [2, 128, 8, 4096])
    wbrA = din("wbrA", [2, 64, 4, 1024])
    wbrB = din("wbrB", [2, 128, 2, 1024])
    wbrC = din("wbrC", [2, 64, 4, 1024])
    wbrD = din("wbrD", [2, 128, 2, 1024])
    wout = din("wout", [2, 128, 8, 1024])
    awsT = din("awsT", [2, 128, 4, 128])
    absr = din("absr", [2, 1, 4, 128])
    sp_in = din("sp_in", [2, 128, NSP])
    ffnw = din("ffnw", [NFFN_G, 128, 6144])
    moew = din("moew", [8 * NMOE_G, 128, 6144])
    wr_in = din("wr_in", [128, 8, 8])
    br_in = din("br_in", [1, 8])
    pleg = din("pleg", [2, 128, 8, 1024])
    plep = din("plep", [2, 128, 2, 1024])
    cst_in = din("cst_in", [128, NCST])
    cstb_in = din("cstb_in", [128, NCSTB])
    koh_in = din("koh_in", [40, 10240])

    o_y = dout("o_y", [128, 8, T])
    o_k = dout("o_k", [2, 64, 4, T])
    o_v = dout("o_v", [2, 128, 2, T])
    o_cb = dout("o_cb", [2, 128, 2, 2])
    o_cd = dout("o_cd", [2, 128, 2, 30])
    o_cbs = dout("o_cbs", [2, 128, 2, NS, 2])
    o_cds = dout("o_cds", [2, 128, 2, NS, 30])
    o_cv = dout("o_cv", [2, 128, 2, NS])
    if DEBUG:
        d_ya = dout('d_ya', [64, 4, T], BF16)
        d_yb = dout('d_yb', [128, 2, T], BF16)
        d_yc = dout('d_yc', [64, 4, T], BF16)
        d_yd = dout('d_yd', [128, 2, T], BF16)
        d_mg = dout('d_mg', [128, 8, T], BF16)
        d_ln1 = dout('d_ln1', [128, 8, T])
        d_ln2 = dout('d_ln2', [128, 8, T])
        d_ln3 = dout('d_ln3', [128, 8, T])

    cc_kt = [dint(f"cc_kt{l}", [256, NPR], BF16) for l in range(2)]
    cc_ktg = [dint(f"cc_ktg{l}", [4 * 256, NPR], BF16) for l in range(2)]
    cc_v = [dint(f"cc_v{l}", [128, 4096], BF16) for l in range(2)]
    cc_vg = [dint(f"cc_vg{l}", [4 * 128, 4096], BF16) for l in range(2)]
    cc_t = [dint(f"cc_t{l}", [128, 64], F32) for l in range(2)]
    cc_tg = [dint(f"cc_tg{l}", [4 * 128, 64], F32) for l in range(2)]
    win_b = [dint(f"win_b{l}", [128, 8, 2560], BF16) for l in range(2)]
    wgate_b = [dint(f"wgate_b{l}", [128, 8, 4096], BF16) for l in range(2)]
    wbrA_b = [dint(f"wbrA_b{l}", [64, 4, 1024], BF16) for l in range(2)]
    wbrB_b = [dint(f"wbrB_b{l}", [128, 2, 1024], BF16) for l in range(2)]
    wbrC_b = [dint(f"wbrC_b{l}", [64, 4, 1024], BF16) for l in range(2)]
    wbrD_b = [dint(f"wbrD_b{l}", [128, 2, 1024], BF16) for l in range(2)]
    wout_b = [dint(f"wout_b{l}", [128, 8, 1024], BF16) for l in range(2)]
    xs1 = dint("xs1", [128, 8, T], F32)
    xs2 = dint("xs2", [128, 8, T], F32)

    ARENA_B = 208000
    arena = nc.alloc_sbuf_tensor("arena", [128, ARENA_B // 2], BF16)
    cur = [0]

    def carve(nbytes):
        off = (cur[0] + 63) // 64 * 64
        cur[0] = off + nbytes
        assert cur[0] <= ARENA_B, f"SBUF arena overflow {cur[0]}"
        return off

    def tile(shape, dt):
        esz = 4 if dt in (F32, I32) else 2
        n = 1
        for s in shape[1:]:
            n *= s
        off = carve(n * esz)
        v = arena[0:shape[0], off // 2: off // 2 + n * esz // 2]
        if dt != BF16:
            v = v.bitcast(dt)
        if len(shape) == 3:
            v = v.rearrange("p (a b) -> p a b", a=shape[1])
        elif len(shape) == 4:
            v = v.rearrange("p (a b c) -> p a b c", a=shape[1], b=shape[2])
        return v

    xb = tile([128, 8, T], BF16)
    WSLOT = 8192
    NW = 2
    wslots = [tile([128, WSLOT], BF16) for _ in range(NW)]
    cst = tile([128, NCST], F32)
    cstb = tile([128, NCSTB], BF16)
    spt = [tile([128, NSP], F32) for _ in range(2)]
    awsb = tile([128, 4, 128], BF16)
    awsf = tile([128, 4, 128], F32)
    absf = tile([1, 4, 128], F32)
    wrt = tile([128, 8, 8], F32)
    brt = tile([1, 8], F32)
    cbprev = tile([128, 2, 2], F32)
    cdprev = tile([128, 2, 30], F32)
    tails = tile([128, 64], F32)
    tailg = tile([128, 4, 64], F32)
    pti = tile([128, NS], I32)
    idx = tile([128, 2], I32)
    QTs = tile([64, 4, NS], F32)
    KTs = tile([64, 4, NS], F32)
    VTs = tile([64, 4, NS], F32)
    kmT = tile([64, 4, 40], BF16)
    ycs = tile([64, 4, NS], BF16)
    m8s = tile([4, 8], F32)
    fsc = tile([128, 4], F32)
    phase_base = cur[0]

    xt = tile([128, 8, 256], F32)
    ya = tile([64, 4, 256], BF16)
    yb = tile([128, 2, 256], BF16)
    yc = tile([64, 4, 256], BF16)
    yd = tile([128, 2, 256], BF16)
    mg32 = tile([128, 8, 256], F32)
    mgb = tile([128, 8, 256], BF16)
    QTa = tile([104, 4, 256], BF16)
    Kh = tile([104, 10240], BF16)
    Vh = tile([128, 80, 65], BF16)
    vtm = tile([128, 4, 16, 64], BF16)
    cbT = tile([128, 2, 2 + 256], F32)
    cdT = tile([128, 2, 30 + 256], F32)
    NTMP = 4
    mtmp = [tile([128, 256], F32) for _ in range(NTMP)]
    mlnm = tile([128, 256], F32)
    mlnr = tile([128, 256], F32)
    dacc = [tile([128, 256], F32) for _ in range(2)]
    gv = tile([128, 2, 256], F32)
    vn = tile([128, 2, 256], F32)
    vtA = tile([128, 256], BF16)
    ptile = [tile([128, 256], BF16) for _ in range(3)]
    accS = tile([65, 256], F32)
    small = tile([128, 4, 40], F32)
    small2 = [tile([128, 4, 104], F32) for _ in range(2)]
    m8 = tile([128, 8], F32)
    kTb = tile([64, 4, 256], BF16)
    cbs = tile([128, 2, NS, 3], F32)
    cds = tile([128, 2, NS, 31], F32)
    sm3 = tile([128, 2, NS, 31], F32)
    Kc = tile([128, PCH * 256], F32)
    Ssc = tile([128, 128, 4], F32)
    Psc = tile([128, 128, 4], F32)
    qbc = tile([128, 256], F32)
    pvp = tile([128, 256], F32)
    pvc = tile([128, 256], F32)
    sdg = tile([64, 64], F32)
    ssm = tile([128, 16], F32)
    mix_end = cur[0]

    cur[0] = phase_base
    xf = tile([128, 8, T], F32)
    cmbT = tile([8, T], F32)
    cmbbc = [tile([128, T], F32) for _ in range(1)]
    hb = [tile([128, 512], BF16) for _ in range(4)]
    sgt = [tile([128, 512], F32) for _ in range(3)]
    ftmp = [tile([128, 512], F32) for _ in range(NTMP)]
    flnm = tile([128, 512], F32)
    flnr = tile([128, 512], F32)
    pTb = tile([128, 2, T], BF16)
    wppt = tile([128, 2048], BF16)
    lg = tile([128, 8], F32)
    ex8 = tile([128, 8], F32)
    fm8 = tile([128, 8], F32)
    ffn_end = cur[0]
    cur[0] = max(mix_end, ffn_end)
    REG = B("region")
    P.always = [REG]

    psum = [nc.alloc_psum_tensor(f"ps{i}", [128, 512], F32) for i in range(8)]
    psb = [B("ps", i) for i in range(8)]
    psrr = [0]

    psmod = [6]

    def nextps():
        i = psrr[0] % psmod[0]
        psrr[0] += 1
        return psum[i], psb[i]

    def MM(out, lhsT, rhs, start, stop, R, W):
        P.add("pe", lambda e, o=out, a=lhsT, b=rhs, s=start, t=stop: e.matmul(o, a, b, start=s, stop=t), R, W)

    def TR(out, in_, ident, R, W):
        P.add("pe", lambda e, o=out, a=in_, i=ident: e.transpose(o, a, i), R, W)

    def ACT(out, in_, func, R, W, bias=0.0, scale=1.0):
        P.add("act", lambda e, o=out, a=in_, f=func, b=bias, s=scale: e.activation(o, a, f, bias=b, scale=s), R, W)

    def TT(out, a, b, op, R, W, eng="dve"):
        P.add(eng, lambda e, o=out, x=a, y=b, p=op: e.tensor_tensor(o, x, y, p), R, W)

    def TS(out, a, s1, s2, op0, op1, R, W, eng="dve"):
        if s2 is None:
            P.add(eng, lambda e, o=out, x=a, q=s1, p=op0: e.tensor_scalar(o, x, q, None, p), R, W)
        else:
            P.add(eng, lambda e, o=out, x=a, q=s1, r=s2, p=op0, p1=op1: e.tensor_scalar(o, x, q, r, p, p1), R, W)

    def STT(out, a, sc, b, op0, op1, R, W, eng="dve"):
        P.add(eng, lambda e, o=out, x=a, s=sc, y=b, p=op0, q=op1: e.scalar_tensor_tensor(o, x, s, y, p, q), R, W)

    def CP(out, in_, R, W, eng="dve"):
        if eng == "act":
            P.add("act", lambda e, o=out, a=in_: e.activation(o, a, AF.Copy), R, W)
        else:
            P.add(eng, lambda e, o=out, a=in_: e.tensor_copy(o, a), R, W)

    def RED(out, in_, op, R, W, eng="dve"):
        P.add(eng, lambda e, o=out, a=in_, p=op: e.tensor_reduce(o, a, AX.X, p), R, W)

    def MAX8(out, in_, R, W):
        P.add("dve", lambda e, o=out, a=in_: e.max(o, a), R, W)

    def RCP(out, in_, R, W):
        P.add("dve", lambda e, o=out, a=in_: e.reciprocal(o, a), R, W)

    def DMA(q, out, in_, R, W):
        P.add(q, lambda e, o=out, a=in_: e.dma_start(out=o, in_=a), R, W, kind="d")

    def FENCE():
        P.add("dve", lambda e: e.tensor_copy(fsc[0:1, 3:4], fsc[0:1, 3:4]), [], [REG])

    def w3(ap2d, a):
        return ap2d.rearrange("p (a b) -> p a b", a=a)

    wrr = [0]
    wsb = [B("wslot", i) for i in range(NW)]

    def loadw(src, parts, n, a=None, R=()):
        i = wrr[0] % NW
        wrr[0] += 1
        dst = wslots[i][0:parts, 0:n]
        DMA("pool", w3(dst, a) if a else dst, src, list(R), [wsb[i]])
        return dst, wsb[i]

    cs = lambda off, n, p=128: cst[0:p, off:off + n]
    ident_f = cs(C_ID, 128)
    ones_f = cs(C_ONE, 128)
    CSTB = B("cst")
    ident_b = cstb[:, CB_ID:CB_ID + 128]

    class TmpPool:
        def __init__(self, tiles, lnm, lnr, name):
            self.tiles = tiles
            self.bufs = [B(name, i) for i in range(len(tiles))]
            self.lnm, self.lnr = lnm, lnr
            self.lnmb, self.lnrb = B(name, "lnm"), B(name, "lnr")
            self.i = 0

        def next(self):
            i = self.i % len(self.tiles)
            self.i += 1
            return self.tiles[i], self.bufs[i]

    MTP = TmpPool(mtmp, mlnm, mlnr, "mtmp")
    FTP = TmpPool(ftmp, flnm, flnr, "ftmp")

    XB = [B("xb", i) for i in range(9)]
    XF = [B("xf", i) for i in range(9)]

    def tok_bufs(lst, c0, n):
        if c0 >= NPR:
            return [lst[8]]
        return [lst[i] for i in range(c0 // 256, (c0 + n - 1) // 256 + 1)]

    DMA("sp", cst[:, :], cst_in, [], [CSTB])
    DMA("pool", cstb[:, :], cstb_in, [], [CSTB])
    for l in range(2):
        DMA("sp", spt[l][:, :], sp_in[l], [], [CSTB])
    DMA("sp", wrt[:, :, :], wr_in, [], [CSTB])
    DMA("sp", brt[:, :], br_in, [], [CSTB])
    DMA("sp", pti[:, :], pt_in, [], [CSTB])
    for ti, (c0, n) in enumerate(MT):
        DMA("pool", xb[:, :, c0:c0 + n], x_in[:, :, c0:c0 + n], [], [XB[ti]])
    WSC = [B("wsc", l) for l in range(2)]
    for l in range(2):
        for src_, dst_ in ((win, win_b), (wgate, wgate_b), (wbrA, wbrA_b), (wbrB, wbrB_b), (wbrC, wbrC_b),
                           (wbrD, wbrD_b), (wout, wout_b)):
            DMA("pool", dst_[l], src_[l], [], [WSC[l]])
    KH = [B("Kh", 0), B("Kh", 1)]
    VH = [B("Vh", 0), B("Vh", 1)]

    def layernorm(tp, srcs, n, nfeat, gcols, bcols, outs32, outsb, silu=False):
        nch = len(srcs)
        ps1, pb1 = nextps()
        ps2, pb2 = nextps()
        for i, (s, sb) in enumerate(srcs):
            MM(ps1[:, 0:n], ones_f, s, i == 0, i == nch - 1, sb + [CSTB], [pb1])
        for i, (s, sb) in enumerate(srcs):
            t, tb = tp.next()
            ACT(t[:, 0:n], s, AF.Square, sb, [tb])
            MM(ps2[:, 0:n], ones_f, t[:, 0:n], i == 0, i == nch - 1, [tb, CSTB], [pb2])
        mean, mb, rstd, rb = tp.lnm, tp.lnmb, tp.lnr, tp.lnrb
        ACT(mean[:, 0:n], ps1[:, 0:n], AF.Copy, [pb1], [mb], scale=1.0 / nfeat)
        t, tb = tp.next()
        TT(t[:, 0:n], mean[:, 0:n], mean[:, 0:n], ALU.mult, [mb], [tb])
        STT(rstd[:, 0:n], ps2[:, 0:n], 1.0 / nfeat, t[:, 0:n], ALU.mult, ALU.subtract, [pb2, tb], [rb])
        TS(rstd[:, 0:n], rstd[:, 0:n], EPS, None, ALU.add, None, [rb], [rb])
        RCP(rstd[:, 0:n], rstd[:, 0:n], [rb], [rb])
        ACT(rstd[:, 0:n], rstd[:, 0:n], AF.Sqrt, [rb], [rb])
        for i, (s, sb) in enumerate(srcs):
            t, tb = tp.next()
            TT(t[:, 0:n], s, mean[:, 0:n], ALU.subtract, sb + [mb], [tb])
            TT(t[:, 0:n], t[:, 0:n], rstd[:, 0:n], ALU.mult, [tb, rb], [tb])
            if silu:
                ACT(t[:, 0:n], t[:, 0:n], AF.Identity, [tb, CSTB], [tb], bias=bcols[i], scale=gcols[i])
                t2, tb2 = tp.next()
                ACT(t2[:, 0:n], t[:, 0:n], AF.Sigmoid, [tb], [tb2])
                ob, obb = outsb[i]
                TT(ob, t[:, 0:n], t2[:, 0:n], ALU.mult, [tb, tb2], obb)
            else:
                o32, ob32 = outs32[i]
                ACT(o32, t[:, 0:n], AF.Identity, [tb, CSTB], ob32, bias=bcols[i], scale=gcols[i])
                if outsb is not None:
                    ob, obb = outsb[i]
                    CP(ob, o32, ob32, obb)

    def gelu_from(ps_ap, pbuf, out_ap, obufs, n, parts=128):
        x, xbf = MTP.next()
        t, tb = MTP.next()
        ACT(x[0:parts, 0:n], ps_ap, AF.Copy, [pbuf], [xbf])
        TT(t[0:parts, 0:n], x[0:parts, 0:n], x[0:parts, 0:n], ALU.mult, [xbf], [tb])
        TS(t[0:parts, 0:n], t[0:parts, 0:n], 0.044715, 1.0, ALU.mult, ALU.add, [tb], [tb])
        TT(t[0:parts, 0:n], t[0:parts, 0:n], x[0:parts, 0:n], ALU.mult, [tb, xbf], [tb])
        ACT(t[0:parts, 0:n], t[0:parts, 0:n], AF.Sigmoid, [tb], [tb], scale=1.5957691216057308)
        TT(out_ap, x[0:parts, 0:n], t[0:parts, 0:n], ALU.mult, [xbf, tb], obufs)

    def proj(ps_ap, pbuf, wv, wbuf, kcs, cols, c0, n):
        xbufs = tok_bufs(XB, c0, n)
        for kc in range(kcs):
            MM(ps_ap, wv[:, kc, cols[0]:cols[1]], xb[:, kc, c0:c0 + n], kc == 0, kc == kcs - 1,
               [wbuf] + xbufs, [pbuf])

    YA, YB, YC, YD = B("ya"), B("yb"), B("yc"), B("yd")
    MG32, MGB = B("mg32"), B("mgb")
    QTA = B("QTa")
    VTM = B("vtm")
    CBT, CDT = B("cbT"), B("cdT")
    CBP, CDP = B("cbprev"), B("cdprev")
    GV, VN = B("gv"), B("vn")
    SQ = B("sqkv")
    KMT = B("kmT")
    OUTB = B("outs")
    XT = B("xt")
    AWS = B("aws")

    def sample_attention(l):
        KC, SS, PS_, QBC, PVP, PVC, SDG, SSM, IDX = (B("Kc"), B("Ssc"), B("Psc"), B("qbc"), B("pvp"), B("pvc"),
                                                     B("sdg"), B("ssm"), B("idx"))
        for s_ in range(NS):
            psq, pbq = nextps()
            for h in range(4):
                TS(sdg[:, :], ident_f[0:64, 0:64], QTs[:, h, s_:s_ + 1], None, ALU.mult, None, [CSTB, SQ], [SDG])
                MM(psq[:, h * 64:(h + 1) * 64], ones_f[0:64, :], sdg[:, :], True, True, [CSTB, SDG], [pbq])
            CP(qbc[:, :], psq[:, 0:256], [pbq], [QBC])
            for ck in range(NCHK):
                TS(idx[:, 0:1], pti[:, s_:s_ + 1], NCHK, l * NPOOL * NCHK + ck, ALU.mult, ALU.add, [CSTB], [IDX])
                P.add("pool", lambda e: e.indirect_dma_start(
                    out=Kc[:, :], out_offset=None, in_=ck_in,
                    in_offset=bass.IndirectOffsetOnAxis(ap=idx[:, 0:1], axis=0)), [IDX], [KC], kind="d")
                kv = Kc[:, :].rearrange("p (a b) -> p a b", b=256)
                TT(kv, kv, qbc[:, :].rearrange("p (o b) -> p o b", o=1).to_broadcast([128, PCH, 256]), ALU.mult,
                   [KC, QBC], [KC])
                RED(Ssc[:, ck * PCH:(ck + 1) * PCH, :], Kc[:, :].rearrange("p (a h d) -> p a h d", h=4, d=64), ALU.add,
                    [KC], [SS])
                yield
            RED(ssm[:, 0:4], Ssc[:, :, :].rearrange("p a h -> p h a"), ALU.add, [SS], [SSM])
            psg, pbg = nextps()
            MM(psg[0:4, 0:64], ssm[:, 0:4], cs(C_PAIR, 64), True, True, [SSM, CSTB], [pbg])
            t, tb = MTP.next()
            CP(t[0:4, 0:64], psg[0:4, 0:64], [pbg], [tb])
            MAX8(m8s[0:4, :], t[0:4, 0:64], [tb], [B("m8s")])
            TS(t[0:4, 0:64], t[0:4, 0:64], m8s[0:4, 2:3], None, ALU.is_ge, None, [tb, B("m8s")], [tb])
            pst, pbt = nextps()
            TR(pst[0:64, 0:4], t[0:4, 0:64], ident_f[0:4, 0:4], [tb, CSTB], [pbt])
            t2, tb2 = MTP.next()
            CP(t2[0:64, 0:4], pst[0:64, 0:4], [pbt], [tb2])
            psm, pbm = nextps()
            MM(psm[:, 0:4], cs(C_PAIRT, 128, 64), t2[0:64, 0:4], True, True, [CSTB, tb2], [pbm])
            TS(ssm[:, 4:8], psm[:, 0:4], -1.0, -NEG, ALU.add, ALU.mult, [pbm], [SSM])
            for h in range(4):
                ACT(Psc[:, :, h], Ssc[:, :, h], AF.Exp, [SS, SSM], [PS_], bias=ssm[:, 4 + h:5 + h])
            RED(ssm[:, 8:12], Psc[:, :, :].rearrange("p a h -> p h a"), ALU.add, [PS_], [SSM])
            for ck in range(NCHK):
                TS(idx[:, 1:2], pti[:, s_:s_ + 1], NCHK, l * NPOOL * NCHK + ck, ALU.mult, ALU.add, [CSTB], [IDX])
                P.add("pool", lambda e: e.indirect_dma_start(
                    out=Kc[:, :], out_offset=None, in_=cv_in,
                    in_offset=bass.IndirectOffsetOnAxis(ap=idx[:, 1:2], axis=0)), [IDX], [KC], kind="d")
                kv4 = Kc[:, :].rearrange("p (a h d) -> p a h d", h=4, d=64)
                for h in range(4):
                    TT(kv4[:, :, h, :], kv4[:, :, h, :],
                       Psc[:, ck * PCH:(ck + 1) * PCH, h:h + 1].to_broadcast([128, PCH, 64]), ALU.mult, [KC, PS_], [KC])
                dst, dstb = (pvp, PVP) if ck == 0 else (pvc, PVC)
                RED(dst[:, :], Kc[:, :].rearrange("p (a c) -> p c a", c=256), ALU.add, [KC], [dstb])
                if ck > 0:
                    TT(pvp[:, :], pvp[:, :], pvc[:, :], ALU.add, [PVP, PVC], [PVP])
                yield
            psn, pbn = nextps()
            for h in range(4):
                MM(psn[0:64, h:h + 1], pvp[:, h * 64:(h + 1) * 64], ones_f[:, 0:1], True, True, [PVP, CSTB], [pbn])
            psd, pbd = nextps()
            MM(psd[0:64, 0:4], ones_f[:, 0:64], ssm[:, 8:12], True, True, [CSTB, SSM], [pbd])
            t, tb = MTP.next()
            TT(t[0:64, 0:4], QTs[:, :, s_], KTs[:, :, s_], ALU.mult, [SQ], [tb])
            pss, pbs = nextps()
            MM(pss[0:64, 0:4], ones_f[0:64, 0:64], t[0:64, 0:4], True, True, [CSTB, tb], [pbs])
            t2, tb2 = MTP.next()
            ACT(t2[0:64, 0:4], pss[0:64, 0:4], AF.Exp, [pbs], [tb2])
            t3, tb3 = MTP.next()
            TT(t3[0:64, 0:4], t2[0:64, 0:4], VTs[:, :, s_], ALU.mult, [tb2, SQ], [tb3])
            TT(t3[0:64, 0:4], t3[0:64, 0:4], psn[0:64, 0:4], ALU.add, [tb3, pbn], [tb3])
            TT(t2[0:64, 0:4], t2[0:64, 0:4], psd[0:64, 0:4], ALU.add, [tb2, pbd], [tb2])
            RCP(t2[0:64, 0:4], t2[0:64, 0:4], [tb2], [tb2])
            TT(ycs[:, :, s_], t3[0:64, 0:4], t2[0:64, 0:4], ALU.mult, [tb3, tb2], [B("ycs")])
            yield

    def layernorm_x(l, i, c0, n):
        bufs = tok_bufs(XF, c0, n)
        bufsb = tok_bufs(XB, c0, n)
        layernorm(FTP, [(xf[:, m, c0:c0 + n], bufs) for m in range(8)], n, 1024.0,
                  [spt[l][:, SP_LNG + i * 8 + m:SP_LNG + i * 8 + m + 1] for m in range(8)],
                  [spt[l][:, SP_LNB + i * 8 + m:SP_LNB + i * 8 + m + 1] for m in range(8)],
                  [(xf[:, m, c0:c0 + n], bufs) for m in range(8)],
                  [(xb[:, m, c0:c0 + n], bufsb) for m in range(8)])

    for l in range(DEPTH):
        sp = lambda off, n=1, p=128, l=l: spt[l][0:p, off:off + n]
        xsrc = x_in if l == 0 else xs2
        XSRC = [B("xsrc", l, i) for i in range(9)] if l == 0 else [B("xs2", i) for i in range(9)]
        XS1 = [B("xs1", i) for i in range(9)]
        XS2 = [B("xs2", i) for i in range(9)]
        CCB = B("cc", l)
        CG = B("ccg", l)
        DMA("pool", Kh[64:104, :], koh_in, [], KH)
        P.add("dve", lambda e: e.memset(Vh[:, :, 64:65], 1.0), [], VH)
        wC, wCb = loadw(win_b[l][:, :, 1280:2048], 128, 8 * 768, a=8, R=[WSC[l]])
        wCv = w3(wC, 8)
        for ti, (c0, n) in enumerate(MT):
            smp = ti == 8
            for h in range(4):
                ps, pb = nextps()
                proj(ps[0:64, 0:n], pb, wCv, wCb, 8, (256 + h * 64, 256 + h * 64 + 64), c0, n)
                t, tb = MTP.next()
                ACT(t[0:64, 0:n], ps[0:64, 0:n], AF.Copy, [pb], [tb])
                DMA("sp", o_k[l][:, h, c0:c0 + n], t[0:64, 0:n], [tb], [OUTB])
                if smp:
                    CP(KTs[:, h, :], t[0:64, 0:n], [tb], [SQ])
                else:
                    CP(kTb[:, h, 0:n], t[0:64, 0:n], [tb], [B("kTb")])
            if not smp:
                for h in range(4):
                    DMA("sp", cc_kt[l][h * 64:(h + 1) * 64, c0:c0 + n], kTb[:, h, 0:n], [B("kTb")], [CCB])
            for vc in range(2):
                ps, pb = nextps()
                proj(ps[:, 0:n], pb, wCv, wCb, 8, (512 + vc * 128, 512 + vc * 128 + 128), c0, n)
                t, tb = MTP.next()
                ACT(t[:, 0:n], ps[:, 0:n], AF.Copy, [pb], [tb])
                DMA("sp", o_v[l][:, vc, c0:c0 + n], t[:, 0:n], [tb], [OUTB])
                if not smp:
                    for c in range(2):
                        pt_, ptb = nextps()
                        TR(pt_[:, 0:128], t[:, c * 128:(c + 1) * 128], ident_f, [tb, CSTB], [ptb])
                        kt = ti * 2 + c
                        CP(vtm[:, 2 * vc:2 * vc + 2, kt, :],
                           pt_[:, 0:128].rearrange("p (a b) -> p a b", a=2), [ptb], [VTM],
                           eng="act" if c % 2 else "dve")
            if smp:
                for h in range(4):
                    ps, pb = nextps()
                    proj(ps[0:64, 0:n], pb, wCv, wCb, 8, (h * 64, h * 64 + 64), c0, n)
                    ACT(QTs[:, h, :], ps[0:64, 0:n], AF.Copy, [pb], [SQ], scale=0.125)
                    ps, pb = nextps()
                    proj(ps[0:64, 0:n], pb, wCv, wCb, 8, (512 + h * 64, 512 + h * 64 + 64), c0, n)
                    ACT(VTs[:, h, :], ps[0:64, 0:n], AF.Copy, [pb], [SQ])
        DMA("sp", cc_v[l], vtm.rearrange("p a b c -> p (a b c)"), [VTM], [CCB])
        wBt, wBtb = loadw(win_b[l][:, :, 768:1280], 128, 8 * 512, a=8, R=[WSC[l]])
        wBtv = w3(wBt, 8)
        wDt, wDtb = loadw(win_b[l][:, :, 2048:2560], 128, 8 * 512, a=8, R=[WSC[l]])
        wDtv = w3(wDt, 8)
        c0t, nt = NPR - 32, 32
        TL = B("tails")
        for cc in range(2):
            psc, pbc = nextps()
            proj(psc[:, 0:nt], pbc, wBtv, wBtb, 8, (cc * 128, cc * 128 + 128), c0t, nt)
            psx, pbx = nextps()
            proj(psx[:, 0:nt], pbx, wBtv, wBtb, 8, (256 + cc * 128, 256 + cc * 128 + 128), c0t, nt)
            t, tb = MTP.next()
            ACT(t[:, 0:nt], psc[:, 0:nt], AF.Copy, [pbc], [tb])
            TT(tails[:, cc * 32:cc * 32 + 2], t[:, 30:32], psx[:, 30:32], ALU.mult, [tb, pbx], [TL])
            psa, pba = nextps()
            proj(psa[:, 0:nt], pba, wDtv, wDtb, 8, (cc * 128, cc * 128 + 128), c0t, nt)
            psg, pbg = nextps()
            proj(psg[:, 0:nt], pbg, wDtv, wDtb, 8, (256 + cc * 128, 256 + cc * 128 + 128), c0t, nt)
            t, tb = MTP.next()
            ACT(t[:, 0:nt], psg[:, 0:nt], AF.Sigmoid, [pbg], [tb])
            TT(tails[:, cc * 32 + 2:cc * 32 + 32], t[:, 2:32], psa[:, 2:32], ALU.mult, [tb, pba], [TL])
        DMA("sp", cc_t[l], tails[:, :], [TL], [CCB])
        for src, dst in ((cc_kt[l], cc_ktg[l]), (cc_v[l], cc_vg[l]), (cc_t[l], cc_tg[l])):
            P.add("pool", lambda e, s=src, d=dst: e.collective_compute(
                "AllGather", ALU.bypass, replica_groups=GROUPS, ins=[s], outs=[d]), [CCB], [CG], kind="cc")
        TG = B("tailg")
        DMA("sp", tailg[:, :, :], cc_tg[l].rearrange("(r p) c -> p r c", p=128), [CG], [TG])
        for rk in range(4):
            selc = cst[:, C_SELR + rk:C_SELR + rk + 1]
            for cc in range(2):
                if rk == 0:
                    TS(cbprev[:, cc, :], tailg[:, rk, cc * 32:cc * 32 + 2], selc, None, ALU.mult, None,
                       [TG, CSTB], [CBP])
                    TS(cdprev[:, cc, :], tailg[:, rk, cc * 32 + 2:cc * 32 + 32], selc, None, ALU.mult, None,
                       [TG, CSTB], [CDP])
                else:
                    STT(cbprev[:, cc, :], tailg[:, rk, cc * 32:cc * 32 + 2], selc, cbprev[:, cc, :],
                        ALU.mult, ALU.add, [TG, CSTB, CBP], [CBP])
                    STT(cdprev[:, cc, :], tailg[:, rk, cc * 32 + 2:cc * 32 + 32], selc, cdprev[:, cc, :],
                        ALU.mult, ALU.add, [TG, CSTB, CDP], [CDP])

        def load_KV(h, half, l=l, CG=CG, CCB=CCB):
            ktg = cc_ktg[l].rearrange("(r hh d) t -> d r hh t", r=4, hh=4)
            r0 = 2 * half
            DMA("sp", Kh[0:64, r0 * 2048:(r0 + 2) * 2048].rearrange("p (r t) -> p r t", r=2),
                ktg[:, r0:r0 + 2, h, :], [CG], [KH[half]])
            for r_ in (r0, r0 + 1):
                DMA("sp", Vh[:, r_ * 16:(r_ + 1) * 16, 0:64],
                    cc_vg[l][r_ * 128:(r_ + 1) * 128, :].rearrange("p (hh k d) -> p hh k d", hh=4, k=16)[:, h, :, :],
                    [CG], [VH[half]])
            if half == 1:
                DMA("sp", Kh[0:64, 8192:10240], cc_kt[l][h * 64:(h + 1) * 64, :], [CCB], [KH[1]])
                DMA("sp", Vh[:, 64:80, 0:64],
                    cc_v[l].rearrange("p (hh k d) -> p hh k d", hh=4, k=16)[:, h, :, :], [CCB], [VH[1]])

        for h in range(4):
            load_KV(h, 0)
            load_KV(h, 1)
            t, tb = MTP.next()
            RED(t[0:64, 0:40], Kh[0:64, :].rearrange("p (b k) -> p b k", k=256), ALU.add, KH, [tb])
            CP(kmT[:, h, :], t[0:64, 0:40], [tb], [KMT])
        sa_gen = sample_attention(l)
        DMA("sp", awsf[:, :, :], awsT[l], [], [AWS])
        DMA("sp", absf[:, :, :], absr[l], [], [AWS])
        for h in range(4):
            TT(awsb[:, h, :], awsf[:, h, :], cs(C_TRI, 128), ALU.mult, [AWS, CSTB], [B("awsb")])

        for ti, (c0, n) in enumerate(MT):
            smp = ti == 8
            DMA("sp", xt[:, :, 0:n], xsrc[:, :, c0:c0 + n], [XSRC[ti]], [XT])
            if not smp:
                g = ti
                wq, wqb = loadw(win_b[l][:, :, 1280:1536], 128, 8 * 256, a=8, R=[WSC[l]])
                wqv = w3(wq, 8)
                for h in range(4):
                    ps, pb = nextps()
                    proj(ps[0:64, 0:n], pb, wqv, wqb, 8, (h * 64, h * 64 + 64), c0, n)
                    ACT(QTa[0:64, h, 0:n], ps[0:64, 0:n], AF.Copy, [pb], [QTA], scale=0.125)
                SM, M8 = B("small"), B("m8")
                SM2 = [B("small2", 0), B("small2", 1)]
                PBv = cst[:, C_PB + g * 40:C_PB + g * 40 + 40]
                PVv = cst[:, C_PV + g * 40:C_PV + g * 40 + 40]
                POv = cst[:, C_PO + g * 40:C_PO + g * 40 + 40]
                bc3 = lambda v: v.rearrange("p (o b) -> p o b", o=1).to_broadcast([128, 4, 40])
                for c in range(2):
                    psg, pbg = nextps()
                    for h in range(4):
                        MM(psg[:, h * 40:h * 40 + 40], QTa[0:64, h, c * 128:(c + 1) * 128], kmT[:, h, :], True, True,
                           [QTA, KMT], [pbg])
                    TT(small[:, :, :], psg[:, 0:160].rearrange("p (h b) -> p h b", h=4), bc3(PBv), ALU.add,
                       [pbg, CSTB], [SM])
                    s2 = small2[c]
                    for h in range(4):
                        MAX8(m8[:, :], small[:, h, :], [SM], [M8])
                        TS(s2[:, h, 64:104], small[:, h, :], m8[:, 2:3], None, ALU.is_ge, None, [SM, M8], [SM2[c]])
                    TT(s2[:, :, 64:104], s2[:, :, 64:104], bc3(PVv), ALU.mult, [SM2[c], CSTB], [SM2[c]])
                    TT(s2[:, :, 64:104], s2[:, :, 64:104], bc3(POv), ALU.add, [SM2[c], CSTB], [SM2[c]])
                    TS(s2[:, :, 64:104], s2[:, :, 64:104], -1.0, -NEG, ALU.add, ALU.mult, [SM2[c]], [SM2[c]])
            wA, wAb = loadw(win_b[l][:, :, 0:512], 128, 8 * 512, a=8, R=[WSC[l]])
            wAv = w3(wA, 8)
            for h in range(4):
                ps, pb = nextps()
                proj(ps[0:64, 0:n], pb, wAv, wAb, 8, (h * 64, h * 64 + 64), c0, n)
                gelu_from(ps[0:64, 0:n], pb, ya[:, h, 0:n], [YA], n, parts=64)
            for vc in range(2):
                ps, pb = nextps()
                proj(ps[:, 0:n], pb, wAv, wAb, 8, (256 + vc * 128, 256 + vc * 128 + 128), c0, n)
                gelu_from(ps[:, 0:n], pb, gv[:, vc, 0:n], [GV], n)
            layernorm(MTP, [(gv[:, vc, 0:n], [GV]) for vc in range(2)], n, 256.0,
                      [sp(SP_ALNG + vc) for vc in range(2)], [sp(SP_ALNB + vc) for vc in range(2)],
                      [(vn[:, vc, 0:n], [VN]) for vc in range(2)], None)
            if not smp:
                for c in range(2):
                    for vc in range(2):
                        pt_, ptb = nextps()
                        TR(pt_[:, 0:128], vn[:, vc, c * 128:(c + 1) * 128], ident_f, [VN, CSTB], [ptb])
                        CP(vtA[:, vc * 128:(vc + 1) * 128], pt_[:, 0:128], [ptb], [B("vtA")],
                           eng="act" if vc else "dve")
                    for h in range(4):
                        ps, pb = nextps()
                        MM(ps[0:64, 0:128], vtA[:, h * 64:(h + 1) * 64], awsb[:, h, :], True, False,
                           [B("vtA"), B("awsb")], [pb])
                        MM(ps[0:64, 0:128], ones_f[0:1, 0:64], absf[0:1, h, :], False, True,
                           [CSTB, AWS], [pb])
                        TT(ya[:, h, c * 128:(c + 1) * 128], ya[:, h, c * 128:(c + 1) * 128], ps[0:64, 0:128],
                           ALU.mult, [YA, pb], [YA])
            else:
                DMA("sp", o_cv[l], vn[:, :, 0:NS], [VN], [OUTB])
                for h in range(4):
                    ps, pb = nextps()
                    MM(ps[0:64, 0:n], ident_f[:, (h % 2) * 64:(h % 2) * 64 + 64], vn[:, h // 2, 0:n], True, True,
                       [VN, CSTB], [pb])
                    t, tb = MTP.next()
                    TS(t[0:64, 0:n], ps[0:64, 0:n], sp(SP_AW00 + h, 1, 64), sp(SP_ABS0 + h, 1, 64),
                       ALU.mult, ALU.add, [pb, CSTB], [tb])
                    TT(ya[:, h, 0:n], ya[:, h, 0:n], t[0:64, 0:n], ALU.mult, [YA, tb], [YA])
            wB, wBb = loadw(win_b[l][:, :, 512:1280], 128, 8 * 768, a=8, R=[WSC[l]])
            wBv = w3(wB, 8)
            CBS, CDS, SM3 = B("cbs"), B("cds"), B("sm3")
            if smp:
                DMA("sp", cbs[:, :, :, 0:2], scb_in[l], [], [CBS])
            for cc in range(2):
                psc, pbc = nextps()
                proj(psc[:, 0:n], pbc, wBv, wBb, 8, (256 + cc * 128, 256 + cc * 128 + 128), c0, n)
                psx, pbx = nextps()
                proj(psx[:, 0:n], pbx, wBv, wBb, 8, (512 + cc * 128, 512 + cc * 128 + 128), c0, n)
                psb_, pbb = nextps()
                proj(psb_[:, 0:n], pbb, wBv, wBb, 8, (cc * 128, cc * 128 + 128), c0, n)
                t, tb = MTP.next()
                ACT(t[:, 0:n], psc[:, 0:n], AF.Copy, [pbc], [tb])
                a, ab = MTP.next()
                if not smp:
                    CP(cbT[:, cc, 0:2], cbprev[:, cc, :], [CBP], [CBT])
                    TT(cbT[:, cc, 2:2 + n], t[:, 0:n], psx[:, 0:n], ALU.mult, [tb, pbx], [CBT])
                    TS(a[:, 0:n], cbT[:, cc, 0:n], sp(SP_BCW + cc * 3), None, ALU.mult, None, [CBT, CSTB], [ab])
                    for k in (1, 2):
                        STT(a[:, 0:n], cbT[:, cc, k:k + n], sp(SP_BCW + cc * 3 + k), a[:, 0:n], ALU.mult, ALU.add,
                            [CBT, CSTB, ab], [ab])
                    TT(yb[:, cc, 0:n], a[:, 0:n], psb_[:, 0:n], ALU.mult, [ab, pbb], [YB])
                    CP(cbprev[:, cc, :], cbT[:, cc, n:n + 2], [CBT], [CBP])
                else:
                    TT(cbs[:, cc, :, 2], t[:, 0:n], psx[:, 0:n], ALU.mult, [tb, pbx], [CBS])
                    wv_ = spt[l][:, SP_BCW + cc * 3:SP_BCW + cc * 3 + 3]
                    TT(sm3[:, cc, :, 0:3], cbs[:, cc, :, :],
                       wv_.rearrange("p (o k) -> p o k", o=1).to_broadcast([128, NS, 3]),
                       ALU.mult, [CBS, CSTB], [SM3])
                    RED(a[:, 0:n], sm3[:, cc, :, 0:3], ALU.add, [SM3], [ab])
                    TT(yb[:, cc, 0:n], a[:, 0:n], psb_[:, 0:n], ALU.mult, [ab, pbb], [YB])
            if ti == 7:
                DMA("sp", o_cb[l], cbprev[:, :, :], [CBP], [OUTB])
            if smp:
                DMA("sp", o_cbs[l], cbs[:, :, :, 1:3], [CBS], [OUTB])
            wD, wDb = loadw(win_b[l][:, :, 2048:2560], 128, 8 * 512, a=8, R=[WSC[l]])
            wDv = w3(wD, 8)
            if smp:
                DMA("sp", cds[:, :, :, 0:30], scd_in[l], [], [CDS])
            dsrc = []
            for cc in range(2):
                psa, pba = nextps()
                proj(psa[:, 0:n], pba, wDv, wDb, 8, (cc * 128, cc * 128 + 128), c0, n)
                psg, pbg = nextps()
                proj(psg[:, 0:n], pbg, wDv, wDb, 8, (256 + cc * 128, 256 + cc * 128 + 128), c0, n)
                t, tb = MTP.next()
                ACT(t[:, 0:n], psg[:, 0:n], AF.Sigmoid, [pbg], [tb])
                a, ab = dacc[cc], B("dacc", cc)
                if not smp:
                    CP(cdT[:, cc, 0:30], cdprev[:, cc, :], [CDP], [CDT])
                    TT(cdT[:, cc, 30:30 + n], t[:, 0:n], psa[:, 0:n], ALU.mult, [tb, pba], [CDT])
                    TS(a[:, 0:n], cdT[:, cc, 0:n], sp(SP_DCW + cc * 31), sp(SP_DCB + cc), ALU.mult, ALU.add,
                       [CDT, CSTB], [ab])
                    for k in range(1, 31):
                        STT(a[:, 0:n], cdT[:, cc, k:k + n], sp(SP_DCW + cc * 31 + k), a[:, 0:n], ALU.mult, ALU.add,
                            [CDT, CSTB, ab], [ab])
                    CP(cdprev[:, cc, :], cdT[:, cc, n:n + 30], [CDT], [CDP])
                else:
                    TT(cds[:, cc, :, 30], t[:, 0:n], psa[:, 0:n], ALU.mult, [tb, pba], [CDS])
                    wv_ = spt[l][:, SP_DCW + cc * 31:SP_DCW + cc * 31 + 31]
                    TT(sm3[:, cc, :, :], cds[:, cc, :, :],
                       wv_.rearrange("p (o k) -> p o k", o=1).to_broadcast([128, NS, 31]),
                       ALU.mult, [CDS, CSTB], [SM3])
                    RED(a[:, 0:n], sm3[:, cc, :, :], ALU.add, [SM3], [ab])
                    TS(a[:, 0:n], a[:, 0:n], sp(SP_DCB + cc), None, ALU.add, None, [ab, CSTB], [ab])
                dsrc.append((a[:, 0:n], [ab]))
            layernorm(MTP, dsrc, n, 256.0, [sp(SP_DLNG + cc) for cc in range(2)],
                      [sp(SP_DLNB + cc) for cc in range(2)],
                      None, [(yd[:, cc, 0:n], [YD]) for cc in range(2)], silu=True)
            if ti == 7:
                DMA("sp", o_cd[l], cdprev[:, :, :], [CDP], [OUTB])
            if smp:
                DMA("sp", o_cds[l], cds[:, :, :, 1:31], [CDS], [OUTB])
            if not smp:
                for _ in range(9):
                    next(sa_gen, None)
                g = ti
                for c in range(2):
                    for h in range(4):
                        pt_, ptb = nextps()
                        TR(pt_[0:104, 0:128], small2[c][:, h, :], ident_f, [SM2[c], CSTB], [ptb])
                        CP(QTa[64:104, h, c * 128:(c + 1) * 128], pt_[64:104, 0:128], [ptb], [QTA],
                           eng="act" if h % 2 else "dve")
                for h in range(4):
                    load_KV(h, 0)
                    load_KV(h, 1)
                    kts = list(range(64)) + [64 + j for j in range(2 * (g + 1))]
                    acc, accb = psum[6 + h % 2], psb[6 + h % 2]
                    pend = None

                    def emit_pv(i, kt, hf, pp, ppb, acc=acc, accb=accb, nk=len(kts)):
                        MM(acc[0:65, 0:256], Vh[:, kt, 0:65], pp[:, :], i == 0, i == nk - 1, [VH[hf], ppb], [accb])

                    for i, kt in enumerate(kts):
                        ps, pb = nextps()
                        own = kt >= 64 + 2 * g
                        hf = 0 if kt < 32 else 1
                        MM(ps[:, 0:256], Kh[0:104, kt * 128:(kt + 1) * 128], QTa[0:104, h, 0:256], True, not own,
                           [KH[hf], QTA], [pb])
                        if own:
                            j = kt - (64 + 2 * g)
                            MM(ps[:, 0:256], ident_b, cstb[:, CB_CAUS + j * 256:CB_CAUS + (j + 1) * 256], False, True,
                               [CSTB], [pb])
                        pp, ppb = ptile[i % 3], B("ptile", i % 3)
                        ACT(pp[:, :], ps[:, 0:256], AF.Exp, [pb], [ppb])
                        if pend is not None:
                            emit_pv(*pend)
                        pend = (i, kt, hf, pp, ppb)
                    emit_pv(*pend)
                    ACS = B("accS")
                    CP(accS[:, :], acc[0:65, 0:256], [accb], [ACS])
                    ps, pb = nextps()
                    MM(ps[0:64, 0:256], ones_f[64:65, 0:64], accS[64:65, :], True, True, [CSTB, ACS], [pb])
                    t, tb = MTP.next()
                    RCP(t[0:64, 0:256], ps[0:64, 0:256], [pb], [tb])
                    TT(yc[:, h, 0:256], accS[0:64, :], t[0:64, 0:256], ALU.mult, [ACS, tb], [YC])
            else:
                for _ in sa_gen:
                    pass
                CP(yc[:, :, 0:NS], ycs[:, :, :], [B("ycs")], [YC])
            if DEBUG and l == 0:
                DMA('sp', d_ya[:, :, c0:c0 + n], ya[:, :, 0:n], [YA], [OUTB])
                DMA('sp', d_yb[:, :, c0:c0 + n], yb[:, :, 0:n], [YB], [OUTB])
                DMA('sp', d_yc[:, :, c0:c0 + n], yc[:, :, 0:n], [YC], [OUTB])
                DMA('sp', d_yd[:, :, c0:c0 + n], yd[:, :, 0:n], [YD], [OUTB])
            ysrc = [(ya, YA, 64, 4, wbrA_b), (yb, YB, 128, 2, wbrB_b), (yc, YC, 64, 4, wbrC_b), (yd, YD, 128, 2, wbrD_b)]
            for j, (yt, ybuf, kp, nk, wsrc) in enumerate(ysrc):
                for half in range(2):
                    wb_, wbb = loadw(wsrc[l][:, :, half * 512:(half + 1) * 512], kp, nk * 512, a=nk, R=[WSC[l]])
                    wbv = w3(wb_, nk)
                    wg_, wgb = loadw(wgate_b[l][:, :, j * 1024 + half * 512:j * 1024 + (half + 1) * 512], 128, 8 * 512, a=8, R=[WSC[l]])
                    wgv = w3(wg_, 8)
                    for mm_ in range(4):
                        m = half * 4 + mm_
                        psb_, pbb = nextps()
                        for k in range(nk):
                            MM(psb_[:, 0:n], wbv[:, k, mm_ * 128:(mm_ + 1) * 128], yt[:, k, 0:n], k == 0, k == nk - 1,
                               [wbb, ybuf], [pbb])
                        psg, pbg = nextps()
                        proj(psg[:, 0:n], pbg, wgv, wgb, 8, (mm_ * 128, mm_ * 128 + 128), c0, n)
                        t, tb = MTP.next()
                        ACT(t[:, 0:n], psg[:, 0:n], AF.Sigmoid, [pbg, CSTB], [tb], bias=sp(SP_BGATE + j * 8 + m))
                        if j == 0:
                            TT(mg32[:, m, 0:n], t[:, 0:n], psb_[:, 0:n], ALU.mult, [tb, pbb], [MG32])
                        else:
                            TT(t[:, 0:n], t[:, 0:n], psb_[:, 0:n], ALU.mult, [tb, pbb], [tb])
                            if j < 3:
                                TT(mg32[:, m, 0:n], mg32[:, m, 0:n], t[:, 0:n], ALU.add, [MG32, tb], [MG32])
                            else:
                                TT(mgb[:, m, 0:n], mg32[:, m, 0:n], t[:, 0:n], ALU.add, [MG32, tb], [MGB])
            if DEBUG and l == 0:
                DMA('sp', d_mg[:, :, c0:c0 + n], mgb[:, :, 0:n], [MGB], [OUTB])
            for half in range(2):
                wo_, wob = loadw(wout_b[l][:, :, half * 512:(half + 1) * 512], 128, 8 * 512, a=8, R=[WSC[l]])
                wov = w3(wo_, 8)
                for mm_ in range(4):
                    m = half * 4 + mm_
                    ps, pb = nextps()
                    for kc in range(8):
                        MM(ps[:, 0:n], wov[:, kc, mm_ * 128:(mm_ + 1) * 128], mgb[:, kc, 0:n], kc == 0, kc == 7,
                           [wob, MGB], [pb])
                    STT(xt[:, m, 0:n], xt[:, m, 0:n], ALPHA, ps[:, 0:n], ALU.mult, ALU.add, [XT, pb], [XT])
            layernorm(MTP, [(xt[:, m, 0:n], [XT]) for m in range(8)], n, 1024.0,
                      [sp(SP_LNG + m) for m in range(8)], [sp(SP_LNB + m) for m in range(8)],
                      [(xt[:, m, 0:n], [XT]) for m in range(8)],
                      [(xb[:, m, c0:c0 + n], [XB[ti]]) for m in range(8)])
            DMA("sp", xs1[:, :, c0:c0 + n], xt[:, :, 0:n], [XT], [XS1[ti]])
            if DEBUG and l == 0:
                DMA('sp', d_ln1[:, :, c0:c0 + n], xt[:, :, 0:n], [XT], [OUTB])
        FENCE()
        for ti, (c0, n) in enumerate(MT):
            DMA("sp", xf[:, :, c0:c0 + n], xs1[:, :, c0:c0 + n], [XS1[ti]], [XF[ti]])
        HB = [B("hb", i) for i in range(4)]
        SG = [B("sg", i) for i in range(3)]
        CMB = [B("cmb", i) for i in range(1)]
        CMT = B("cmbT")
        hrr = [0]
        moe = l % 2 == 1
        if moe:
            LG, FM8, FSC, EX8 = B("lg"), B("fm8"), B("fsc"), B("ex8")
            chunks = [(c * 128, 128) for c in range(16)] + [(NPR, NS)]
            for (t0, tn) in chunks:
                xfb = tok_bufs(XF, t0, tn)
                ps, pb = nextps()
                for kc in range(8):
                    MM(ps[0:tn, 0:8], xf[:, kc, t0:t0 + tn], wrt[:, kc, :], kc == 0, False, xfb + [CSTB], [pb])
                MM(ps[0:tn, 0:8], ones_f[0:1, 0:tn], brt[0:1, :], False, True, [CSTB], [pb])
                CP(lg[0:tn, :], ps[0:tn, 0:8], [pb], [LG])
                MAX8(fm8[0:tn, :], lg[0:tn, :], [LG], [FM8])
                TS(fsc[0:tn, 0:1], fm8[0:tn, 0:1], -1.0, None, ALU.mult, None, [FM8], [FSC])
                ACT(ex8[0:tn, :], lg[0:tn, :], AF.Exp, [LG, FSC], [EX8], bias=fsc[0:tn, 0:1])
                TS(lg[0:tn, :], lg[0:tn, :], fm8[0:tn, 1:2], None, ALU.is_ge, None, [LG, FM8], [LG])
                TT(ex8[0:tn, :], ex8[0:tn, :], lg[0:tn, :], ALU.mult, [EX8, LG], [EX8])
                RED(fsc[0:tn, 1:2], ex8[0:tn, :], ALU.add, [EX8], [FSC])
                RCP(fsc[0:tn, 2:3], fsc[0:tn, 1:2], [FSC], [FSC])
                TS(ex8[0:tn, :], ex8[0:tn, :], fsc[0:tn, 2:3], None, ALU.mult, None, [EX8, FSC], [EX8])
                pt_, ptb = nextps()
                TR(pt_[0:8, 0:tn], ex8[0:tn, :], ident_f[0:tn, 0:tn], [EX8, CSTB], [ptb])
                CP(cmbT[:, t0:t0 + tn], pt_[0:8, 0:tn], [ptb], [CMT])
        for ti, (c0, n) in enumerate(MT):
            for m in range(8):
                ACT(xf[:, m, c0:c0 + n], xf[:, m, c0:c0 + n], AF.Copy, [XF[ti]], [XF[ti]], scale=ALPHA)
        ngroups = 8 * NMOE_G if moe else NFFN_G
        wsrc = moew if moe else ffnw
        psmod[0] = 8
        wcur = {}

        def emit_gu(gi, c0, n):
            wgv, wuv, wdv, wfb, e_ = wcur[gi]
            hs = []
            for hc in range(2):
                psg, pbg = nextps()
                proj(psg[:, 0:n], pbg, wgv, wfb, 8, (hc * 128, hc * 128 + 128), c0, n)
                psu, pbu = nextps()
                proj(psu[:, 0:n], pbu, wuv, wfb, 8, (hc * 128, hc * 128 + 128), c0, n)
                si = hrr[0] % 3
                hi = hrr[0] % 4
                hrr[0] += 1
                ACT(sgt[si][:, 0:n], psg[:, 0:n], AF.Silu, [pbg], [SG[si]])
                if moe:
                    TT(sgt[si][:, 0:n], sgt[si][:, 0:n], cmbbc[0][:, c0:c0 + n], ALU.mult,
                       [SG[si], CMB[0]], [SG[si]])
                TT(hb[hi][:, 0:n], sgt[si][:, 0:n], psu[:, 0:n], ALU.mult, [SG[si], pbu], [HB[hi]])
                hs.append((hb[hi], HB[hi]))
            return hs

        def emit_down(gi, c0, n, hs):
            wgv, wuv, wdv, wfb, e_ = wcur[gi]
            xfb = tok_bufs(XF, c0, n)
            for m in range(8):
                ps, pb = nextps()
                for hc in range(2):
                    MM(ps[:, 0:n], wdv[:, hc, m * 128:(m + 1) * 128], hs[hc][0][:, 0:n], hc == 0, hc == 1,
                       [wfb, hs[hc][1]], [pb])
                TT(xf[:, m, c0:c0 + n], xf[:, m, c0:c0 + n], ps[:, 0:n], ALU.add, xfb + [pb], xfb)

        prev = None
        for gi in range(ngroups):
            e_ = gi // NMOE_G
            if moe and gi % NMOE_G == 0:
                for (c0, n) in FT:
                    ps, pb = nextps()
                    MM(ps[:, 0:n], cst[0:8, C_SEL8 + e_ * 128:C_SEL8 + (e_ + 1) * 128], cmbT[0:8, c0:c0 + n], True, True,
                       [CSTB, CMT], [pb])
                    CP(cmbbc[0][:, c0:c0 + n], ps[:, 0:n], [pb], [CMB[0]], eng="act")
            wf_, wfb = loadw(wsrc[gi], 128, 6144)
            wcur[gi] = (w3(wf_[:, 0:2048], 8), w3(wf_[:, 2048:4096], 8), w3(wf_[:, 4096:6144], 2), wfb, e_)
            for (c0, n) in FT:
                hs = emit_gu(gi, c0, n)
                if prev is not None:
                    emit_down(*prev)
                prev = (gi, c0, n, hs)
        emit_down(*prev)
        for (c0, n) in FT:
            layernorm_x(l, 1, c0, n)
            if DEBUG and l == 0:
                DMA('sp', d_ln2[:, :, c0:c0 + n], xf[:, :, c0:c0 + n], tok_bufs(XF, c0, n), [OUTB])
        PTB = B("pTb")
        for (c0, n) in FT:
            DMA("pool", pTb[:, :, c0:c0 + n], p_in[l][:, :, c0:c0 + n], [], [PTB])
        wppb = B("wppt")
        DMA("pool", wppt[:, :], plep[l].rearrange("p a b -> p (a b)"), [], [wppb])
        wppv = w3(wppt[:, :], 2)
        for (c0, n) in FT:
            xfb = tok_bufs(XF, c0, n)
            for half in range(2):
                wpg_, wpgb = loadw(pleg[l][:, :, half * 512:(half + 1) * 512], 128, 8 * 512, a=8)
                wpgv = w3(wpg_, 8)
                for mm_ in range(4):
                    m = half * 4 + mm_
                    ps1, pb1 = nextps()
                    proj(ps1[:, 0:n], pb1, wpgv, wpgb, 8, (mm_ * 128, mm_ * 128 + 128), c0, n)
                    ps2, pb2 = nextps()
                    for c in range(2):
                        MM(ps2[:, 0:n], wppv[:, c, m * 128:(m + 1) * 128], pTb[:, c, c0:c0 + n], c == 0, c == 1,
                           [wppb, PTB], [pb2])
                    si = hrr[0] % 3
                    hrr[0] += 1
                    ACT(sgt[si][:, 0:n], ps1[:, 0:n], AF.Sigmoid, [pb1], [SG[si]])
                    TT(sgt[si][:, 0:n], sgt[si][:, 0:n], ps2[:, 0:n], ALU.mult, [SG[si], pb2], [SG[si]])
                    STT(xf[:, m, c0:c0 + n], xf[:, m, c0:c0 + n], ALPHA, sgt[si][:, 0:n], ALU.mult, ALU.add,
                        xfb + [SG[si]], xfb)
            layernorm_x(l, 2, c0, n)
        if DEBUG and l == 0:
            for ti, (c0, n) in enumerate(MT):
                DMA('sp', d_ln3[:, :, c0:c0 + n], xf[:, :, c0:c0 + n], [XF[ti]], [OUTB])
        psmod[0] = 6
        dst = o_y if l == DEPTH - 1 else xs2
        for ti, (c0, n) in enumerate(MT):
            DMA("sp", dst[:, :, c0:c0 + n], xf[:, :, c0:c0 + n], [XF[ti]], [OUTB if l == DEPTH - 1 else XS2[ti]])
        FENCE()

    emit(nc, P)
    return nc, len(P.ops)


def _fm(a, nch):
    return np.ascontiguousarray(a.reshape(nch, 128, -1).transpose(1, 0, 2))


_CACHE = {}


def _constants():
    c = np.zeros((128, NCST), np.float32)
    c[:, C_ID:C_ID + 128] = np.eye(128, dtype=np.float32)
    c[:, C_ONE:C_ONE + 128] = 1.0
    s = np.arange(128)
    c[:, C_TRI:C_TRI + 128] = (s[:, None] <= s[None, :]).astype(np.float32)
    pair = (s[:, None] // 2 == np.arange(64)[None, :]).astype(np.float32)
    c[:, C_PAIR:C_PAIR + 64] = pair
    c[0:64, C_PAIRT:C_PAIRT + 128] = pair.T
    for e in range(8):
        c[e, C_SEL8 + e * 128:C_SEL8 + (e + 1) * 128] = 1.0
    cb = np.zeros((128, NCSTB), np.float32)
    cb[:, CB_ID:CB_ID + 128] = np.eye(128, dtype=np.float32)
    q = np.arange(256)
    for j in range(2):
        key = j * 128 + s
        cb[:, CB_CAUS + j * 256:CB_CAUS + (j + 1) * 256] = np.where(key[:, None] <= q[None, :], 0.0, NEG)
    koh = np.zeros((40, 10240), np.float32)
    for b in range(40):
        koh[b, b * 256:(b + 1) * 256] = 1.0
    return c, cb, koh


def kernel(x_prompt, x_sample, p_prompt, p_sample, cache_k, cache_v, state_conv_b, state_conv_d, page_table,
           w_in, w_gate, b_gate, a_ln_g, a_ln_b, a_w_s, a_b_s, b_conv_w, d_conv_w, d_conv_b, d_ln_g, d_ln_b,
           w_branch, w_out, ln_g, ln_b, ffn_w_gate, ffn_w_up, ffn_w_down, moe_w_router, moe_b_router,
           moe_w_gate, moe_w_up, moe_w_down, ple_w_gate, ple_w_proj):
    f = lambda a: np.asarray(a, dtype=np.float32)
    x_prompt, x_sample, p_prompt, p_sample = f(x_prompt), f(x_sample), f(p_prompt), f(p_sample)
    cache_k, cache_v = f(cache_k), f(cache_v)
    if "nc" not in _CACHE:
        _CACHE["nc"] = build_program()
    nc, _ = _CACHE["nc"]

    sh = {}
    sh["win"] = np.stack([_fm(f(w_in[l]), 8) for l in range(2)])
    sh["wgate"] = np.stack([_fm(f(w_gate[l]), 8) for l in range(2)])
    wbr = f(w_branch)
    sh["wbrA"] = np.ascontiguousarray(wbr[:, 0].reshape(2, 4, 64, 1024).transpose(0, 2, 1, 3))
    sh["wbrB"] = np.stack([_fm(wbr[l, 1], 2) for l in range(2)])
    sh["wbrC"] = np.ascontiguousarray(wbr[:, 2].reshape(2, 4, 64, 1024).transpose(0, 2, 1, 3))
    sh["wbrD"] = np.stack([_fm(wbr[l, 3], 2) for l in range(2)])
    sh["wout"] = np.stack([_fm(f(w_out[l]), 8) for l in range(2)])
    aws = f(a_w_s)
    sh["awsT"] = np.ascontiguousarray(aws.transpose(0, 3, 1, 2))
    sh["absr"] = np.ascontiguousarray(f(a_b_s).reshape(2, 1, 4, 128))
    spa = np.zeros((2, 128, NSP), np.float32)
    for l in range(2):
        spa[l, :, SP_BGATE:SP_BGATE + 32] = f(b_gate[l]).reshape(32, 128).T
        spa[l, :, SP_LNG:SP_LNG + 24] = f(ln_g[l]).reshape(24, 128).T
        spa[l, :, SP_LNB:SP_LNB + 24] = f(ln_b[l]).reshape(24, 128).T
        spa[l, :, SP_ALNG:SP_ALNG + 2] = f(a_ln_g[l]).reshape(2, 128).T
        spa[l, :, SP_ALNB:SP_ALNB + 2] = f(a_ln_b[l]).reshape(2, 128).T
        spa[l, :, SP_DLNG:SP_DLNG + 2] = f(d_ln_g[l]).reshape(2, 128).T
        spa[l, :, SP_DLNB:SP_DLNB + 2] = f(d_ln_b[l]).reshape(2, 128).T
        spa[l, :, SP_DCB:SP_DCB + 2] = f(d_conv_b[l]).reshape(2, 128).T
        spa[l, :, SP_BCW:SP_BCW + 6] = f(b_conv_w[l]).reshape(3, 2, 128).transpose(2, 1, 0).reshape(128, 6)
        spa[l, :, SP_DCW:SP_DCW + 62] = f(d_conv_w[l]).reshape(31, 2, 128).transpose(2, 1, 0).reshape(128, 62)
        spa[l, :, SP_AW00:SP_AW00 + 4] = aws[l, :, 0, 0][None, :]
        spa[l, :, SP_ABS0:SP_ABS0 + 4] = f(a_b_s[l])[:, 0][None, :]
    sh["sp_in"] = spa

    def pack_ffn(wg, wu, wd, ng):
        H = wg.shape[1]
        g = _fm(wg, 8).reshape(128, 8, ng, 256).transpose(2, 0, 1, 3).reshape(ng, 128, 2048)
        u = _fm(wu, 8).reshape(128, 8, ng, 256).transpose(2, 0, 1, 3).reshape(ng, 128, 2048)
        d = wd.reshape(ng, 2, 128, 1024).transpose(0, 2, 1, 3).reshape(ng, 128, 2048)
        return np.ascontiguousarray(np.concatenate([g, u, d], axis=2))

    sh["ffnw"] = pack_ffn(f(ffn_w_gate[0]), f(ffn_w_up[0]), f(ffn_w_down[0]), NFFN_G)
    sh["moew"] = np.concatenate([pack_ffn(f(moe_w_gate[0, e]), f(moe_w_up[0, e]), f(moe_w_down[0, e]), NMOE_G)
                                 for e in range(8)], axis=0)
    sh["wr_in"] = _fm(f(moe_w_router[0]), 8)
    sh["br_in"] = f(moe_b_router).reshape(1, 8)
    sh["pleg"] = np.stack([_fm(f(ple_w_gate[l]), 8) for l in range(2)])
    sh["plep"] = np.stack([_fm(f(ple_w_proj[l]), 2) for l in range(2)])
    cst, cstb, koh = _constants()
    sh["cstb_in"] = cstb
    sh["koh_in"] = koh
    sh["ck_in"] = cache_k.reshape(2 * NPOOL * NCHK, PCH * 256)
    sh["cv_in"] = cache_v.reshape(2 * NPOOL * NCHK, PCH * 256)
    scb = f(state_conv_b)
    scd = f(state_conv_d)
    pt = np.asarray(page_table).astype(np.int32)

    in_maps = []
    for c in range(8):
        b, r = c // 4, c % 4
        m = dict(sh)
        xs = np.concatenate([x_prompt[b, r * NPR:(r + 1) * NPR], x_sample[4 * c:4 * c + 4, 0]], axis=0)
        m["x_in"] = _fm(np.ascontiguousarray(xs.T), 8)
        ps_ = [np.concatenate([p_prompt[l, b, r * NPR:(r + 1) * NPR], p_sample[l, 4 * c:4 * c + 4, 0]], axis=0)
               for l in range(2)]
        m["p_in"] = np.stack([_fm(np.ascontiguousarray(p.T), 2) for p in ps_])
        m["scb_in"] = np.ascontiguousarray(scb[:, 4 * c:4 * c + 4].reshape(2, NS, 2, 2, 128).transpose(0, 4, 3, 1, 2))
        m["scd_in"] = np.ascontiguousarray(scd[:, 4 * c:4 * c + 4].reshape(2, NS, 30, 2, 128).transpose(0, 4, 3, 1, 2))
        m["pt_in"] = np.ascontiguousarray(pt[4 * c:4 * c + 4].T)
        cc_ = cst.copy()
        for g in range(8):
            pb = np.full(40, -1e6, np.float32)
            pv = np.zeros(40, np.float32)
            po = np.zeros(40, np.float32)
            pb[0:8 * r] = 0.0
            pv[0:8 * r] = 1.0
            pb[32:32 + g] = 0.0
            pv[32:32 + g] = 1.0
            po[32 + g] = 1.0
            cc_[:, C_PB + g * 40:C_PB + (g + 1) * 40] = pb[None, :]
            cc_[:, C_PV + g * 40:C_PV + (g + 1) * 40] = pv[None, :]
            cc_[:, C_PO + g * 40:C_PO + (g + 1) * 40] = po[None, :]
        if r > 0:
            cc_[:, C_SELR + r - 1] = 1.0
        m["cst_in"] = cc_
        in_maps.append(m)

    res = run_bass_kernel_spmd(nc, in_maps, core_ids=list(range(8)))
    R = res.results
    if DEBUG:
        _CACHE['dbg'] = {k: np.asarray(v).astype(np.float32) for k, v in R[0].items() if k.startswith('d_')}

    y_prompt = np.zeros((2, 8192, 1024), np.float32)
    y_sample = np.zeros((32, 1, 1024), np.float32)
    k_prompt = np.zeros((2, 2, 8192, 4, 64), np.float32)
    v_prompt = np.zeros((2, 2, 8192, 4, 64), np.float32)
    k_sample = np.zeros((2, 32, 1, 4, 64), np.float32)
    v_sample = np.zeros((2, 32, 1, 4, 64), np.float32)
    conv_b_prompt = np.zeros((2, 2, 2, 256), np.float32)
    conv_b_sample = np.zeros((2, 32, 2, 256), np.float32)
    conv_d_prompt = np.zeros((2, 2, 30, 256), np.float32)
    conv_d_sample = np.zeros((2, 32, 30, 256), np.float32)
    chunk_v_sample = np.zeros((2, 32, 1, 256), np.float32)
    for c in range(8):
        b, r = c // 4, c % 4
        o = R[c]
        y = np.asarray(o["o_y"]).transpose(2, 1, 0).reshape(T, 1024)
        y_prompt[b, r * NPR:(r + 1) * NPR] = y[:NPR]
        y_sample[4 * c:4 * c + 4, 0] = y[NPR:]
        ok = np.asarray(o["o_k"]).transpose(0, 3, 2, 1)
        k_prompt[:, b, r * NPR:(r + 1) * NPR] = ok[:, :NPR]
        k_sample[:, 4 * c:4 * c + 4, 0] = ok[:, NPR:]
        ov = np.asarray(o["o_v"]).transpose(0, 3, 2, 1).reshape(2, T, 4, 64)
        v_prompt[:, b, r * NPR:(r + 1) * NPR] = ov[:, :NPR]
        v_sample[:, 4 * c:4 * c + 4, 0] = ov[:, NPR:]
        if r == 3:
            conv_b_prompt[:, b] = np.asarray(o["o_cb"]).transpose(0, 3, 2, 1).reshape(2, 2, 256)
            conv_d_prompt[:, b] = np.asarray(o["o_cd"]).transpose(0, 3, 2, 1).reshape(2, 30, 256)
        conv_b_sample[:, 4 * c:4 * c + 4] = np.asarray(o["o_cbs"]).transpose(0, 3, 4, 2, 1).reshape(2, NS, 2, 256)
        conv_d_sample[:, 4 * c:4 * c + 4] = np.asarray(o["o_cds"]).transpose(0, 3, 4, 2, 1).reshape(2, NS, 30, 256)
        chunk_v_sample[:, 4 * c:4 * c + 4, 0] = np.asarray(o["o_cv"]).transpose(0, 3, 2, 1).reshape(2, NS, 256)
    return (y_prompt, y_sample, k_prompt, v_prompt, k_sample, v_sample, conv_b_prompt, conv_b_sample,
            conv_d_prompt, conv_d_sample, chunk_v_sample)
```
